# Optimizing a Trainium2 kernel written in Bass

```python
import math
import jax
import jax.numpy as jnp
from jax import lax
import numpy as np

D_MODEL = 2048
BATCH = 4
SEQ = 4096
DEPTH = 4

GRID_W = 64
CTX_LEN = 256
HEAD_DIM = 128
NA_HEADS = 4
NA_WIN_R = 8
NA_WIN_C = 16
NA_QBLOCK_C = 16
NA_KBLOCK_C = 32
GQA_Q_HEADS = 8
GQA_KV_HEADS = 2
ROPE_THETA = 10000.0
Q_BLOCK = 128
DN_HEADS = 4
DN_CONV = 5
DN_CHUNK = 64
W_A = NA_HEADS * HEAD_DIM
W_B = GQA_Q_HEADS * HEAD_DIM
W_KV = GQA_KV_HEADS * HEAD_DIM
W_C = DN_HEADS * HEAD_DIM
MIX_WIDTH = W_A + W_B + W_C
IN_SPLITS = (W_A, W_A, W_A, W_B, W_KV, W_KV, W_C, W_C, W_C, W_C, 2 * DN_HEADS, 2 * DN_HEADS)
D_IN = sum(IN_SPLITS)
IN_OFFSETS = tuple(sum(IN_SPLITS[:i + 1]) for i in range(len(IN_SPLITS) - 1))
N_EXPERTS = 16
EC_FACTOR = 2
D_EXPERT = 1024
N_MOD = 6
NORM_EPS = 1e-6
NEG_INF = -1e30
F32 = jnp.float32

kernel_name = 'hybrid_diffusion_trunk'


def rms_norm(x, gain):
    xf = x.astype(F32)
    y = xf * lax.rsqrt(jnp.mean(xf * xf, axis=-1, keepdims=True) + NORM_EPS)
    return (y * gain.astype(F32)).astype(x.dtype)


def ada_norm(x, gain, shift, scale):
    return rms_norm(x, gain) * (1 + scale) + shift


def l2_normalize(x):
    xf = x.astype(F32)
    return (xf * lax.rsqrt(jnp.sum(xf * xf, axis=-1, keepdims=True) + NORM_EPS)).astype(x.dtype)


def split_heads(t, n_heads):
    return t.reshape(t.shape[0], t.shape[1], n_heads, t.shape[-1] // n_heads)


def axial_rope_tables(n_tok):
    t = jnp.arange(n_tok, dtype=jnp.int32)
    pos = jnp.stack([t // GRID_W, t % GRID_W], axis=-1).astype(F32)
    n_freq = HEAD_DIM // 4
    freqs = ROPE_THETA ** (-jnp.arange(n_freq, dtype=F32) / n_freq)
    ang = pos[:, :, None] * freqs
    return jnp.cos(ang), jnp.sin(ang)


def apply_axial_rope(x, rope):
    cos, sin = rope
    B, N, H, Dh = x.shape
    xf = x.astype(F32).reshape(B, N, H, 2, 2, Dh // 4)
    x1, x2 = xf[..., 0, :], xf[..., 1, :]
    c, s = cos[None, :, None], sin[None, :, None]
    out = jnp.stack([x1 * c - x2 * s, x1 * s + x2 * c], axis=-2)
    return out.reshape(B, N, H, Dh).astype(x.dtype)


def attend(q, k, v):
    B, Q, Hq, Dh = q.shape
    Hkv = k.shape[2]
    qg = q.reshape(B, Q, Hkv, Hq // Hkv, Dh)
    s = jnp.einsum('bqhgd,bkhd->bhgqk', qg, k).astype(F32) * (Dh ** -0.5)
    p = jax.nn.softmax(s, axis=-1).astype(v.dtype)
    return jnp.einsum('bhgqk,bkhd->bqhgd', p, v).reshape(B, Q, Hq * Dh)


def blocked_attention(q, k, v):
    B, N, Hq, Dh = q.shape
    qb = jnp.moveaxis(q.reshape(B, N // Q_BLOCK, Q_BLOCK, Hq, Dh), 1, 0)
    o = lax.map(lambda q_blk: attend(q_blk, k, v), qb)
    return jnp.moveaxis(o, 0, 1).reshape(B, N, Hq * Dh)


def neighbourhood_attention(q, k, v, k_ctx, v_ctx, rpb):
    B, N, H, Dh = q.shape
    rows = N // GRID_W
    wr = min(NA_WIN_R, rows)
    scale = Dh ** -0.5
    grid = lambda t: t.reshape(B, rows, GRID_W, H, Dh)
    qg, kg, vg = grid(q), grid(k), grid(v)
    r = jnp.arange(rows)
    row_idx = jnp.clip(r - wr // 2, 0, rows - wr)[:, None] + jnp.arange(wr)[None, :]
    dr_idx = row_idx - r[:, None] + (NA_WIN_R - 1)
    n_loc = wr * NA_KBLOCK_C
    outs = []
    for c0 in range(0, GRID_W, NA_QBLOCK_C):
        kc0 = min(max(c0 - NA_WIN_C // 2, 0), GRID_W - NA_KBLOCK_C)
        qcol = jnp.arange(c0, c0 + NA_QBLOCK_C)
        kcol = jnp.arange(kc0, kc0 + NA_KBLOCK_C)
        cstart = jnp.clip(qcol - NA_WIN_C // 2, 0, GRID_W - NA_WIN_C)
        in_win = (kcol[None, :] >= cstart[:, None]) & (kcol[None, :] < cstart[:, None] + NA_WIN_C)
        dc_idx = jnp.clip(kcol[None, :] - qcol[:, None] + (NA_WIN_C - 1), 0, 2 * NA_WIN_C - 2)
        bias = rpb[:, dr_idx[:, None, :, None], dc_idx[None, :, None, :]]
        q_blk = qg[:, :, c0:c0 + NA_QBLOCK_C]
        k_blk = kg[:, :, kc0:kc0 + NA_KBLOCK_C][:, row_idx]
        v_blk = vg[:, :, kc0:kc0 + NA_KBLOCK_C][:, row_idx]
        s_loc = jnp.einsum('brqhd,brwkhd->bhrqwk', q_blk, k_blk).astype(F32) * scale + bias[None].astype(F32)
        s_loc = jnp.where(in_win[:, None, :], s_loc, NEG_INF).reshape(B, H, rows, NA_QBLOCK_C, n_loc)
        s_ctx = jnp.einsum('brqhd,bkhd->bhrqk', q_blk, k_ctx).astype(F32) * scale
        p = jax.nn.softmax(jnp.concatenate([s_loc, s_ctx], axis=-1), axis=-1).astype(v.dtype)
        o = (jnp.einsum('bhrqn,brnhd->brqhd', p[..., :n_loc], v_blk.reshape(B, rows, n_loc, H, Dh))
             + jnp.einsum('bhrqk,bkhd->brqhd', p[..., n_loc:], v_ctx))
        outs.append(o)
    return jnp.concatenate(outs, axis=2).reshape(B, N, H * Dh)


def short_conv(x, w):
    return lax.conv_general_dilated(x, w[:, None, :].astype(x.dtype), window_strides=(1,),
                                    padding=[(DN_CONV // 2, DN_CONV // 2)],
                                    dimension_numbers=('NWC', 'WIO', 'NWC'),
                                    feature_group_count=x.shape[-1])


def deltanet_inputs(q, k, v, beta_raw, dec_raw, conv_w, a_log, dt_bias, with_q):
    B, N, _ = k.shape
    wq, wk, wv = jnp.split(conv_w, 3, axis=-1)
    conv_heads = lambda t, w: split_heads(jax.nn.silu(short_conv(t, w)), DN_HEADS)
    k = l2_normalize(conv_heads(k, wk))
    v = conv_heads(v, wv)
    q = l2_normalize(conv_heads(q, wq)) * (HEAD_DIM ** -0.5) if with_q else None
    beta = jax.nn.sigmoid(beta_raw.astype(F32)).reshape(B, N, 2, DN_HEADS)
    g = -jnp.exp(a_log.astype(F32)) * jax.nn.softplus(
        dec_raw.astype(F32).reshape(B, N, 2, DN_HEADS) + dt_bias.astype(F32))
    return q, k, v, beta, g


def delta_rule_chunked(q, k, v, beta, g, s0, with_output):
    B, N, H, Dk = k.shape
    Dv = v.shape[-1]
    v_dtype = v.dtype
    L = DN_CHUNK

    def chunks(t):
        t = t.astype(F32).reshape(B, N // L, L, H, *t.shape[3:])
        return jnp.moveaxis(t, 3, 1)

    k, v, beta, g = chunks(k), chunks(v), chunks(beta), chunks(g)
    G = jnp.cumsum(g, axis=-1)
    incl = jnp.tril(jnp.ones((L, L), dtype=bool))
    decay = jnp.exp(jnp.where(incl, G[..., :, None] - G[..., None, :], -jnp.inf))
    k_beta = k * beta[..., None]
    a_strict = jnp.tril(jnp.einsum('bhnid,bhnjd->bhnij', k_beta, k) * decay, -1)
    rhs = jnp.concatenate([v * beta[..., None], k_beta * jnp.exp(G)[..., None]], axis=-1)
    sol = lax.linalg.triangular_solve(a_strict + jnp.eye(L, dtype=F32), rhs,
                                      left_side=True, lower=True, unit_diagonal=True)
    u, w = sol[..., :Dv], sol[..., Dv:]
    g_last = G[..., -1]
    k_tail = k * jnp.exp(g_last[..., None] - G)[..., None]
    xs = [u, w, k_tail, jnp.exp(g_last)]
    if with_output:
        qf = chunks(q)
        xs = xs + [qf * jnp.exp(G)[..., None], jnp.einsum('bhnid,bhnjd->bhnij', qf, k) * decay]
    xs = [jnp.moveaxis(t, 2, 0) for t in xs]

    def step(S, xc):
        u_c, w_c, kt_c, gl_c = xc[:4]
        v_new = u_c - jnp.einsum('bhld,bhde->bhle', w_c, S)
        S_next = S * gl_c[..., None, None] + jnp.einsum('bhld,bhle->bhde', kt_c, v_new)
        if not with_output:
            return S_next, None
        qh_c, qk_c = xc[4:]
        o = jnp.einsum('bhld,bhde->bhle', qh_c, S) + jnp.einsum('bhlm,bhme->bhle', qk_c, v_new)
        return S_next, o

    s_final, o = lax.scan(step, s0, xs)
    if with_output:
        o = jnp.transpose(o, (1, 0, 3, 2, 4)).reshape(B, N, H, Dv).astype(v_dtype)
    return s_final, o


def deltanet_bidir(q, k, v, beta, g, s0_f, s0_b, with_output):
    rev = lambda t: None if t is None else jnp.flip(t, axis=1)
    s_f, o_f = delta_rule_chunked(q, k, v, beta[:, :, 0], g[:, :, 0], s0_f, with_output)
    s_b, o_b = delta_rule_chunked(rev(q), rev(k), rev(v), rev(beta[:, :, 1]), rev(g[:, :, 1]), s0_b, with_output)
    o = o_f + rev(o_b) if with_output else None
    return s_f, s_b, o


def deltanet_output(o, gate, out_gain):
    o = rms_norm(o, out_gain) * jax.nn.silu(split_heads(gate, DN_HEADS))
    return o.reshape(o.shape[0], o.shape[1], -1)


def expert_choice_ffn(h, router_w, w_gate, w_up, w_down):
    B, N, _ = h.shape
    cap = (EC_FACTOR * N) // N_EXPERTS
    aff = jax.nn.softmax(jnp.einsum('bnd,de->bne', h, router_w).astype(F32), axis=-1)
    gate, idx = lax.top_k(jnp.swapaxes(aff, 1, 2), cap)
    b_idx = jnp.arange(B)[:, None, None]
    xe = h[b_idx, idx]
    hid = jax.nn.silu(jnp.einsum('becd,edf->becf', xe, w_gate)) * jnp.einsum('becd,edf->becf', xe, w_up)
    ye = jnp.einsum('becf,efd->becd', hid, w_down) * gate[..., None].astype(h.dtype)
    return jnp.zeros_like(h).at[b_idx, idx].add(ye)


def hybrid_layer(x, ctx, c, c_ctx, rope, ada_w, ada_b, g_mix, g_ffn, w_in, w_out, na_qk_gain, na_rpb,
                 gqa_qk_gain, dn_conv, dn_a_log, dn_dt_bias, dn_out_gain, router_w, w_gate, w_up, w_down,
                 need_ctx_out):
    sh_l, sc_l, gt_l, sh2_l, sc2_l, gt2_l = jnp.split((jax.nn.silu(c) @ ada_w + ada_b)[:, None, :], N_MOD, axis=-1)
    sh_c, sc_c, gt_c, sh2_c, sc2_c, gt2_c = jnp.split(jax.nn.silu(c_ctx) @ ada_w + ada_b, N_MOD, axis=-1)

    p_lat = jnp.split(ada_norm(x, g_mix, sh_l, sc_l) @ w_in, IN_OFFSETS, axis=-1)
    p_ctx = jnp.split(ada_norm(ctx, g_mix, sh_c, sc_c) @ w_in, IN_OFFSETS, axis=-1)
    qa_l, ka_l, va_l, qb_l, kb_l, vb_l, qc_l, kc_l, vc_l, gc_l, beta_l, dec_l = p_lat
    qa_c, ka_c, va_c, qb_c, kb_c, vb_c, qc_c, kc_c, vc_c, gc_c, beta_c, dec_c = p_ctx

    k_na_c = rms_norm(split_heads(ka_c, NA_HEADS), na_qk_gain[1])
    v_na_c = split_heads(va_c, NA_HEADS)
    oa_lat = neighbourhood_attention(rms_norm(split_heads(qa_l, NA_HEADS), na_qk_gain[0]),
                                     rms_norm(split_heads(ka_l, NA_HEADS), na_qk_gain[1]),
                                     split_heads(va_l, NA_HEADS), k_na_c, v_na_c, na_rpb)

    k_g_c = rms_norm(split_heads(kb_c, GQA_KV_HEADS), gqa_qk_gain[1])
    v_g_c = split_heads(vb_c, GQA_KV_HEADS)
    q_g = apply_axial_rope(rms_norm(split_heads(qb_l, GQA_Q_HEADS), gqa_qk_gain[0]), rope)
    k_g = apply_axial_rope(rms_norm(split_heads(kb_l, GQA_KV_HEADS), gqa_qk_gain[1]), rope)
    ob_lat = blocked_attention(q_g, jnp.concatenate([k_g_c, k_g], axis=1),
                               jnp.concatenate([v_g_c, split_heads(vb_l, GQA_KV_HEADS)], axis=1))

    ctx_dn = deltanet_inputs(qc_c, kc_c, vc_c, beta_c, dec_c, dn_conv, dn_a_log, dn_dt_bias, need_ctx_out)
    lat_dn = deltanet_inputs(qc_l, kc_l, vc_l, beta_l, dec_l, dn_conv, dn_a_log, dn_dt_bias, True)
    s0 = jnp.zeros((x.shape[0], DN_HEADS, HEAD_DIM, HEAD_DIM), F32)
    s_cf, s_cb, o_dn_c = deltanet_bidir(*ctx_dn, s0, s0, need_ctx_out)
    _, _, o_dn_l = deltanet_bidir(*lat_dn, s_cf, s_cb, True)
    oc_lat = deltanet_output(o_dn_l, gc_l, dn_out_gain)

    x = x + gt_l * (jnp.concatenate([oa_lat, ob_lat, oc_lat], axis=-1) @ w_out)
    x = x + gt2_l * expert_choice_ffn(ada_norm(x, g_ffn, sh2_l, sc2_l), router_w, w_gate, w_up, w_down)
    if not need_ctx_out:
        return x, None

    oa_ctx = attend(rms_norm(split_heads(qa_c, NA_HEADS), na_qk_gain[0]), k_na_c, v_na_c)
    ob_ctx = attend(rms_norm(split_heads(qb_c, GQA_Q_HEADS), gqa_qk_gain[0]), k_g_c, v_g_c)
    oc_ctx = deltanet_output(o_dn_c, gc_c, dn_out_gain)
    ctx = ctx + gt_c * (jnp.concatenate([oa_ctx, ob_ctx, oc_ctx], axis=-1) @ w_out)
    ctx = ctx + gt2_c * expert_choice_ffn(ada_norm(ctx, g_ffn, sh2_c, sc2_c), router_w, w_gate, w_up, w_down)
    return x, ctx


def setup_inputs(seed: int = 0) -> dict:
    key = jax.random.key(seed)
    ks = jax.random.split(key, 24)
    nrm = lambda k, shape, s: jax.random.normal(k, shape, F32) * s
    dt = jnp.exp(jax.random.uniform(ks[15], (DEPTH, 2, DN_HEADS), F32, math.log(1e-3), math.log(1e-1)))
    return {
        'x': nrm(ks[0], (BATCH, SEQ, D_MODEL), 1.0),
        'c': nrm(ks[1], (BATCH, D_MODEL), 1.0),
        'ctx': nrm(ks[2], (BATCH, CTX_LEN, D_MODEL), 1.0),
        'c_ctx': nrm(ks[3], (D_MODEL,), 1.0),
        'ada_w': nrm(ks[4], (DEPTH, D_MODEL, N_MOD * D_MODEL), 0.5 * D_MODEL ** -0.5),
        'ada_b': nrm(ks[5], (DEPTH, N_MOD * D_MODEL), 0.02),
        'norm_mix': 1.0 + nrm(ks[6], (DEPTH, D_MODEL), 0.02),
        'norm_ffn': 1.0 + nrm(ks[7], (DEPTH, D_MODEL), 0.02),
        'w_in': nrm(ks[8], (DEPTH, D_MODEL, D_IN), D_MODEL ** -0.5),
        'w_out': nrm(ks[9], (DEPTH, MIX_WIDTH, D_MODEL), MIX_WIDTH ** -0.5),
        'na_qk_gain': 1.0 + nrm(ks[10], (DEPTH, 2, HEAD_DIM), 0.02),
        'na_rpb': nrm(ks[11], (DEPTH, NA_HEADS, 2 * NA_WIN_R - 1, 2 * NA_WIN_C - 1), 0.1),
        'gqa_qk_gain': 1.0 + nrm(ks[12], (DEPTH, 2, HEAD_DIM), 0.02),
        'dn_conv': nrm(ks[13], (DEPTH, DN_CONV, 3 * W_C), DN_CONV ** -0.5),
        'dn_a_log': jnp.log(jax.random.uniform(ks[14], (DEPTH, 2, DN_HEADS), F32, 1.0, 16.0)),
        'dn_dt_bias': dt + jnp.log(-jnp.expm1(-dt)),
        'dn_out_gain': 1.0 + nrm(ks[16], (DEPTH, HEAD_DIM), 0.02),
        'router_w': nrm(ks[17], (DEPTH, D_MODEL, N_EXPERTS), D_MODEL ** -0.5),
        'exp_w_gate': nrm(ks[18], (DEPTH, N_EXPERTS, D_MODEL, D_EXPERT), D_MODEL ** -0.5),
        'exp_w_up': nrm(ks[19], (DEPTH, N_EXPERTS, D_MODEL, D_EXPERT), D_MODEL ** -0.5),
        'exp_w_down': nrm(ks[20], (DEPTH, N_EXPERTS, D_EXPERT, D_MODEL), D_EXPERT ** -0.5),
    }


def reference(x, c, ctx, c_ctx, ada_w, ada_b, norm_mix, norm_ffn, w_in, w_out, na_qk_gain, na_rpb,
              gqa_qk_gain, dn_conv, dn_a_log, dn_dt_bias, dn_out_gain, router_w, exp_w_gate, exp_w_up,
              exp_w_down):
    rope = axial_rope_tables(x.shape[1])
    for i in range(DEPTH):
        x, ctx = hybrid_layer(
            x, ctx, c, c_ctx, rope,
            ada_w=ada_w[i], ada_b=ada_b[i], g_mix=norm_mix[i], g_ffn=norm_ffn[i],
            w_in=w_in[i], w_out=w_out[i], na_qk_gain=na_qk_gain[i], na_rpb=na_rpb[i],
            gqa_qk_gain=gqa_qk_gain[i], dn_conv=dn_conv[i], dn_a_log=dn_a_log[i],
            dn_dt_bias=dn_dt_bias[i], dn_out_gain=dn_out_gain[i], router_w=router_w[i],
            w_gate=exp_w_gate[i], w_up=exp_w_up[i], w_down=exp_w_down[i],
            need_ctx_out=(i < DEPTH - 1))
    return x
```

```python
import math
import numpy as np
from contextlib import ExitStack
import concourse.bass as bass
import concourse.mybir as mybir
from concourse.bass_utils import run_bass_kernel_spmd

F32 = mybir.dt.float32
BF16 = mybir.dt.bfloat16
I32 = mybir.dt.int32
U32 = mybir.dt.uint32
AF = mybir.ActivationFunctionType
ALU = mybir.AluOpType
AX = mybir.AxisListType

SEM_ROLL = 30000

D = 2048
NCTX = 256
NLAT = 4096
T = NCTX + NLAT
NT = T // 128
DIN = 5136
NE = 16
DEXP = 1024
EPS = 1e-6


class Tk:
    __slots__ = ("w", "r")

    def __init__(self):
        self.w = None
        self.r = {}


class Tile:
    def __init__(self, t, is_psum=False):
        self.t = t
        self.k = Tk()
        self.is_psum = is_psum

    def __getitem__(self, idx):
        return self.t[idx]


class Eng:
    def __init__(self, name, e):
        self.name = name
        self.e = e
        self.sem = None
        self.cnt = 0
        self.seen = {}
        self.same_sync = name in ("act", "dve", "pool")
        self.last_tok = None
        self.pending = False


class FW:
    def __init__(self, nc, stack):
        self.nc = nc
        self.stack = stack
        self.nsem = 0
        self.pe = Eng("pe", nc.tensor)
        self.act = Eng("act", nc.scalar)
        self.dve = Eng("dve", nc.vector)
        self.pool = Eng("pool", nc.gpsimd)
        self.sp = Eng("sp", nc.sync)
        self.engs = [self.pe, self.act, self.dve, self.pool, self.sp]
        for e in self.engs:
            e.sem = self.new_sem(e.name)
        self.dma_rings = {"sp": [[self.new_sem("dmah%d" % i), 0] for i in range(32)],
                          "pool": [[self.new_sem("dmas%d" % i), 0] for i in range(24)]}
        self.dma_is = {"sp": 0, "pool": 0}
        self.n_inst = 0

    def new_sem(self, name):
        self.nsem += 1
        return self.stack.enter_context(self.nc.semaphore("s_%s_%d" % (name, self.nsem)))

    def _wait(self, eng, tok):
        if tok is None:
            return
        sem, val = tok
        if eng.seen.get(sem.num, 0) >= val:
            return
        if sem is eng.sem and not eng.same_sync:
            return
        eng.e.wait_ge(sem, val)
        self.n_inst += 1
        eng.seen[sem.num] = val

    def deps(self, eng, reads, writes):
        for t in reads:
            self._wait(eng, t.k.w)
            if getattr(t, "is_psum", False):
                for tok in t.k.r.values():
                    if tok[0] is not eng.sem:
                        self._wait(eng, tok)
        for t in writes:
            self._wait(eng, t.k.w)
            for tok in t.k.r.values():
                self._wait(eng, tok)

    def commit(self, tok, reads, writes):
        for t in reads:
            t.k.r[tok[0].num] = tok
        for t in writes:
            t.k.w = tok
            t.k.r = {}

    def op(self, eng, reads, writes, fn, signal=True):
        if signal and eng.cnt >= SEM_ROLL and not eng.pending:
            eng.sem = self.new_sem(eng.name)
            eng.cnt = 0
        self.deps(eng, reads, writes)
        ins = fn()
        self.n_inst += 1
        eng.pending = not signal
        if signal:
            eng.cnt += 1
            ins.then_inc(eng.sem, 1)
            tok = (eng.sem, eng.cnt)
            eng.last_tok = tok
        else:
            tok = (eng.sem, eng.cnt + 1)
        self.commit(tok, reads, writes)
        return ins

    def dma(self, q, reads, writes, fn):
        ring = self.dma_rings[q.name]
        slot = ring[self.dma_is[q.name]]
        self.dma_is[q.name] = (self.dma_is[q.name] + 1) % len(ring)
        sem = slot[0]
        if slot[1] > 0:
            self._wait(q, (sem, slot[1]))
        self.deps(q, reads, writes)
        ins = fn(q.e)
        self.n_inst += 1
        slot[1] += 16
        ins.then_inc(sem, 16)
        tok = (sem, slot[1])
        self.commit(tok, reads, writes)
        return tok

    def barrier(self):
        toks = [e.last_tok for e in self.engs if e.last_tok is not None]
        toks += [(s[0], s[1]) for ring in self.dma_rings.values() for s in ring if s[1] > 0]
        for e in self.engs:
            for tok in toks:
                if tok[0] is e.sem:
                    continue
                self._wait(e, tok)


class Ring:
    def __init__(self, tiles):
        self.tiles = tiles
        self.i = -1

    def next(self):
        self.i = (self.i + 1) % len(self.tiles)
        return self.tiles[self.i]


class KB:
    def __init__(self, nc, stack, n_layers, debug=()):
        self.nc = nc
        self.top = stack
        self.fw = FW(nc, stack)
        self.L = n_layers
        self.debug = set(debug)
        self.uid = 0
        self.ph = None

    def _name(self, p):
        self.uid += 1
        return "%s_%d" % (p, self.uid)

    def sb(self, shape, dt, name="sb", perm=False):
        st = self.top if perm else self.ph
        return Tile(st.enter_context(self.nc.sbuf_tensor(self._name(name), list(shape), dt)))

    def ps(self, shape, dt, name="ps"):
        return Tile(self.ph.enter_context(self.nc.psum_tensor(self._name(name), list(shape), dt)), is_psum=True)

    def dram(self, name, shape, dt, kind="Internal"):
        return Tile(self.nc.dram_tensor(name, list(shape), dt, kind=kind).ap())

    def begin_phase(self):
        self.ph = ExitStack()

    def end_phase(self):
        self.fw.barrier()
        self.ph.close()
        self.ph = None

    def mm(self, out, lhsT, rhs, R, W, start=True, stop=True, signal=None):
        if signal is None:
            signal = stop
        return self.fw.op(self.fw.pe, R, W, lambda: self.nc.tensor.matmul(out, lhsT=lhsT, rhs=rhs, start=start, stop=stop), signal=signal)

    def tr(self, out, in_, ident, R, W, signal=True):
        return self.fw.op(self.fw.pe, R, W, lambda: self.nc.tensor.transpose(out=out, in_=in_, identity=ident), signal=signal)

    def _e(self, eng):
        return {"act": self.fw.act, "dve": self.fw.dve, "pool": self.fw.pool}[eng]

    def _ne(self, eng):
        return {"act": self.nc.scalar, "dve": self.nc.vector, "pool": self.nc.gpsimd}[eng]

    def act(self, out, in_, func, R, W, **kw):
        return self.fw.op(self.fw.act, R, W, lambda: self.nc.scalar.activation(out=out, in_=in_, func=func, **kw))

    def tt(self, eng, out, in0, in1, op, R, W):
        return self.fw.op(self._e(eng), R, W, lambda: self._ne(eng).tensor_tensor(out=out, in0=in0, in1=in1, op=op))

    def ts(self, eng, out, in0, s1, s2, op0, op1, R, W):
        if s2 is None:
            return self.fw.op(self._e(eng), R, W, lambda: self._ne(eng).tensor_scalar(out=out, in0=in0, scalar1=s1, scalar2=None, op0=op0))
        return self.fw.op(self._e(eng), R, W, lambda: self._ne(eng).tensor_scalar(out=out, in0=in0, scalar1=s1, scalar2=s2, op0=op0, op1=op1))

    def cp(self, eng, out, in_, R, W):
        if eng == "act":
            return self.act(out, in_, AF.Copy, R, W)
        return self.fw.op(self._e(eng), R, W, lambda: self._ne(eng).tensor_copy(out=out, in_=in_))

    def memset(self, eng, out, val, W):
        return self.fw.op(self._e(eng), [], W, lambda: self._ne(eng).memset(out, val))

    def dma(self, q, out, in_, R, W):
        qe = {"sp": self.fw.sp, "pool": self.fw.pool}[q]
        return self.fw.dma(qe, R, W, lambda e: e.dma_start(out=out, in_=in_))

    def rsqrt_inplace(self, ap, tile, mul):
        self.act(ap, ap, AF.Ln, [tile, self.epsc], [tile], scale=mul, bias=self.epsc[:ap.shape[0], 0:1])
        self.act(ap, ap, AF.Exp, [tile], [tile], scale=-0.5)


class Sub:
    def __init__(self):
        self.k = Tk()


IN_BLOCKS = [
    (0, 512, "qk", dict(H=4, gain="na_q", rope=False, dst="QT", h0=0)),
    (512, 512, "qk", dict(H=4, gain="na_k", rope=False, dst="KT", h0=0)),
    (1024, 512, "v", dict(h0=0)),
    (1536, 512, "qk", dict(H=4, gain="gq_q", rope=True, dst="QT", h0=4)),
    (2048, 512, "qk", dict(H=4, gain="gq_q", rope=True, dst="QT", h0=8)),
    (2560, 512, "kvb", dict()),
    (3072, 512, "fm", dict(c0=0)),
    (3584, 512, "fm", dict(c0=4)),
    (4096, 512, "fm", dict(c0=8)),
    (4608, 512, "gate", dict()),
    (5120, 16, "bd", dict()),
]


def declare_io(kb):
    nc = kb.nc
    L = kb.L
    I = {}

    def inp(name, shape, dt=F32):
        I[name] = nc.dram_tensor(name, list(shape), dt, kind="ExternalInput").ap()

    inp("xin", [T, D])
    inp("cT", [128, 16, 2])
    inp("ident", [128, 128])
    inp("ropec", [128, NT, 64])
    inp("ropes", [128, NT, 64])
    inp("ada_w", [L, D, 6 * D])
    inp("ada_b", [L, 6 * D])
    inp("ada_b_fm", [L, 128, 96])
    inp("gmix_fm", [L, 128, 16])
    inp("gffn_fm", [L, 128, 16])
    inp("w_in", [L, D, DIN])
    inp("w_out", [L, D, D])
    inp("na_gain", [L, 2, 128])
    inp("gq_gain", [L, 2, 128])
    kb.I = I
    kb.xout = nc.dram_tensor("xout", [T, D], F32, kind="ExternalOutput").ap()
    kb.xs_k = [Sub() for _ in range(NT)]
    kb.QT = kb.dram("QT", [12, 128, T], BF16)
    kb.KT = kb.dram("KT", [6, 128, T], BF16)
    kb.V = kb.dram("V", [T, 6, 128], BF16)
    kb.CQ = kb.dram("CQ", [12, 128, T], F32)
    kb.SG = kb.dram("SG", [T, 512], BF16)
    kb.dbg = {}

    def dbg(name, shape, dt=F32):
        if name in kb.debug:
            kb.dbg[name] = nc.dram_tensor("dbg_" + name, list(shape), dt, kind="ExternalOutput").ap()

    dbg("modfm", [128, 2 * 4 * 16])
    dbg("gates", [4, 128, D])
    dbg("QT", [12, 128, T], BF16)
    dbg("KT", [6, 128, T], BF16)
    dbg("V", [T, 6, 128], BF16)
    dbg("CQ", [12, 128, T])
    dbg("SG", [T, 512], BF16)
    dbg("BD", [128, NT * 16])


def setup_consts(kb):
    I = kb.I
    kb.begin_phase()
    kb.epsc = kb.sb([128, 1], F32, "eps", perm=True)
    kb.memset("dve", kb.epsc[:], EPS, [kb.epsc])
    kb.identf = kb.sb([128, 128], F32, "identf", perm=True)
    kb.identb = kb.sb([128, 128], BF16, "identb", perm=True)
    kb.dma("sp", kb.identf[:], I["ident"], [], [kb.identf])
    kb.cp("dve", kb.identb[:], kb.identf[:], [kb.identf], [kb.identb])
    kb.modfm = kb.sb([128, 2, 4, 16], F32, "modfm", perm=True)
    kb.gs = kb.sb([128, 2, 2, 16], F32, "gs", perm=True)
    kb.GTS = kb.dram("GTS", [4, 128, D], F32)
    kb.AFFT = kb.dram("AFFT", [NE, T], F32)
    kb.BD = kb.sb([128, NT, 16], F32, "BD", perm=True)
    kb.end_phase()


def phase0(kb, l):
    I = kb.I
    kb.begin_phase()
    cT = kb.sb([128, 16, 2], F32)
    kb.dma("sp", cT[:], I["cT"], [], [cT])
    scT = kb.sb([128, 16, 2], F32)
    kb.act(scT[:], cT[:], AF.Silu, [cT], [scT])
    screp = kb.sb([128, 16, 2, 128], F32)
    for r in range(2):
        kb.cp("dve", screp[:, :, r, :], scT[:, :, r:r + 1].to_broadcast([128, 16, 128]), [scT], [screp])
    adabfm = kb.sb([128, 96], F32)
    kb.dma("sp", adabfm[:], I["ada_b_fm"][l], [], [adabfm])
    gfm = kb.sb([128, 2, 16], F32)
    kb.dma("sp", gfm[:, 0, :], I["gmix_fm"][l], [], [gfm])
    kb.dma("sp", gfm[:, 1, :], I["gffn_fm"][l], [], [gfm])
    kb.gtb = [[kb.sb([128, D], F32, "gtb") for _ in range(2)] for _ in range(2)]
    wring = Ring([kb.sb([128, 16, 512], F32) for _ in range(2)])
    bring = Ring([kb.sb([128, 512], F32) for _ in range(2)])
    pg = Ring([kb.ps([128, 512], F32) for _ in range(2)])
    pf = Ring([kb.ps([128, 512], F32) for _ in range(2)])

    def load(blk):
        w = wring.next()
        kb.dma("sp", w[:], I["ada_w"][l, :, blk * 512:(blk + 1) * 512].rearrange("(k p) c -> p k c", p=128), [], [w])
        return w

    import os
    CUT = os.environ.get("CUT", "")
    nblk = 0 if CUT == "pre" else 24
    wn = load(0)
    for blk in range(nblk):
        w = wn
        if blk + 1 < 24:
            wn = load(blk + 1)
        m, cb = blk // 4, blk % 4
        if (CUT == "fm" and m in (2, 5)) or (CUT == "gate" and m not in (2, 5)):
            continue
        if m in (2, 5):
            gi = 0 if m == 2 else 1
            bb = bring.next()
            kb.dma("sp", bb[:], I["ada_b"][l, blk * 512:(blk + 1) * 512].partition_broadcast(128), [], [bb])
            for r in range(2):
                p = pg.next()
                for k in range(16):
                    kb.mm(p[:], screp[:, k, r, :], w[:, k, :], [screp, w], [p], start=(k == 0), stop=(k == 15))
                kb.tt("dve", kb.gtb[r][gi][:, cb * 512:(cb + 1) * 512], p[:], bb[:], ALU.add, [p, bb], [kb.gtb[r][gi]])
        else:
            mi = {0: 0, 1: 1, 3: 2, 4: 3}[m]
            p = pf.next()
            pv = p[:, 0:8].rearrange("p (j r) -> p j r", r=2)
            for j in range(4):
                for k in range(16):
                    kb.mm(pv[:, j, :], w[:, k, j * 128:(j + 1) * 128], scT[:, k, :], [scT, w], [p], start=(k == 0), stop=(k == 15))
            c0 = m * 16 + cb * 4
            kb.tt("dve", kb.modfm[:, :, mi, cb * 4:cb * 4 + 4], pv.rearrange("p j r -> p r j"),
                  adabfm[:, c0:c0 + 4].unsqueeze(1).to_broadcast([128, 2, 4]), ALU.add, [p, adabfm], [kb.modfm])
    for r in range(2):
        for wi, mi in ((0, 1), (1, 3)):
            kb.ts("dve", kb.gs[:, r, wi, :], kb.modfm[:, r, mi, :], 1.0, None, ALU.add, None, [kb.modfm], [kb.gs])
            kb.tt("dve", kb.gs[:, r, wi, :], kb.gs[:, r, wi, :], gfm[:, wi, :], ALU.mult, [kb.gs, gfm], [kb.gs])
    for r in range(2):
        for gi in range(2):
            kb.dma("sp", kb.GTS[r * 2 + gi], kb.gtb[r][gi][:], [kb.gtb[r][gi]], [kb.GTS])
    if "modfm" in kb.dbg:
        kb.dma("sp", kb.dbg["modfm"], kb.modfm[:].rearrange("p a b c -> p (a b c)"), [kb.modfm], [])
    if "gates" in kb.dbg:
        for r in range(2):
            for gi in range(2):
                kb.dma("sp", kb.dbg["gates"][r * 2 + gi], kb.gtb[r][gi][:], [kb.gtb[r][gi]], [])
    kb.end_phase()


def phaseA(kb, l):
    I = kb.I
    nc = kb.nc
    kb.begin_phase()
    xsrc = I["xin"] if l == 0 else kb.xout
    kb.ropec = kb.sb([128, NT, 64], F32, "ropec")
    kb.ropes = kb.sb([128, NT, 64], F32, "ropes")
    kb.dma("sp", kb.ropec[:], I["ropec"], [], [kb.ropec])
    kb.dma("sp", kb.ropes[:], I["ropes"], [], [kb.ropes])
    gains = {}
    gq = kb.sb([128, 4, 128], F32)
    kb.dma("sp", gq[:, 0, :], I["na_gain"][l, 0].partition_broadcast(128), [], [gq])
    kb.dma("sp", gq[:, 1, :], I["na_gain"][l, 1].partition_broadcast(128), [], [gq])
    kb.dma("sp", gq[:, 2, :], I["gq_gain"][l, 0].partition_broadcast(128), [], [gq])
    kb.dma("sp", gq[:, 3, :], I["gq_gain"][l, 1].partition_broadcast(128), [], [gq])
    qs = 128.0 ** -0.5
    kb.ts("dve", gq[:, 0, :], gq[:, 0, :], qs, None, ALU.mult, None, [gq], [gq])
    kb.ts("dve", gq[:, 2, :], gq[:, 2, :], qs, None, ALU.mult, None, [gq], [gq])
    gidx = {"na_q": 0, "na_k": 1, "gq_q": 2, "gq_k": 3}

    xt_ring = Ring([kb.sb([128, D], F32) for _ in range(2)])
    xs_ring = Ring([kb.sb([128, D], BF16) for _ in range(2)])
    sqj = kb.sb([128, D], BF16)
    ssq_ring = Ring([kb.sb([128, 1], F32) for _ in range(2)])
    xnT_ring = Ring([kb.sb([128, 16, 512], BF16) for _ in range(2)])
    tmpf_ring = Ring([kb.sb([128, 8, 128], F32) for _ in range(2)])
    w_ring = Ring([kb.sb([128, 16, 512], BF16) for _ in range(2)])
    psT = Ring([kb.ps([128, 8, 128], BF16) for _ in range(2)])
    pP = Ring([kb.ps([128, 512], F32) for _ in range(2)])
    pQ = Ring([kb.ps([128, 8, 128], BF16) for _ in range(2)])
    sq4 = kb.sb([128, 512], F32)
    ssq4_ring = Ring([kb.sb([128, 4], F32) for _ in range(2)])
    xn4_ring = Ring([kb.sb([128, 512], F32) for _ in range(2)])
    xg4_ring = Ring([kb.sb([128, 512], F32) for _ in range(2)])
    xr4_ring = Ring([kb.sb([128, 512], BF16) for _ in range(2)])
    rt_ring = Ring([kb.sb([128, 4, 2, 32], F32) for _ in range(4)])
    qst_ring = Ring([kb.sb([128, 4, 512], BF16) for _ in range(3)])
    vst_ring = Ring([kb.sb([128, 512], BF16) for _ in range(3)])
    cst_ring = Ring([kb.sb([128, 512], F32) for _ in range(3)])

    flip = [0]

    def alt(a="act", b="dve"):
        flip[0] ^= 1
        return a if flip[0] else b

    def qk_post(P, pcols, H, gname, rope, t, stage, h_off, ti):
        Pv = P[:, pcols:pcols + H * 128]
        kb.act(sq4[:, 0:H * 128], Pv, AF.Square, [P], [sq4])
        s4 = ssq4_ring.next()
        kb.fw.op(kb.fw.dve, [sq4], [s4], lambda: nc.vector.tensor_reduce(
            out=s4[:, 0:H], in_=sq4[:, 0:H * 128].rearrange("p (h d) -> p h d", h=H), axis=AX.X, op=ALU.add))
        kb.rsqrt_inplace(s4[:, 0:H], s4, 1.0 / 128)
        xn = xn4_ring.next()
        kb.tt("dve", xn[:, 0:H * 128].rearrange("p (h d) -> p h d", h=H), Pv.rearrange("p (h d) -> p h d", h=H),
              s4[:, 0:H].unsqueeze(2).to_broadcast([128, H, 128]), ALU.mult, [P, s4], [xn])
        xr = xr4_ring.next()
        gv = gq[:, gidx[gname], :].unsqueeze(1).to_broadcast([128, H, 128])
        if not (rope and t >= 2):
            kb.tt("pool", xr[:, 0:H * 128].rearrange("p (h d) -> p h d", h=H),
                  xn[:, 0:H * 128].rearrange("p (h d) -> p h d", h=H), gv, ALU.mult, [xn, gq], [xr])
        else:
            xg = xg4_ring.next()
            kb.tt("pool", xg[:, 0:H * 128].rearrange("p (h d) -> p h d", h=H),
                  xn[:, 0:H * 128].rearrange("p (h d) -> p h d", h=H), gv, ALU.mult, [xn, gq], [xg])
            v5 = xg[:, 0:H * 128].rearrange("p (h a b f) -> p h a b f", h=H, a=2, b=2)
            o5 = xr[:, 0:H * 128].rearrange("p (h a b f) -> p h a b f", h=H, a=2, b=2)
            x1, x2 = v5[:, :, :, 0, :], v5[:, :, :, 1, :]
            cb = kb.ropec[:, t, :].rearrange("p (a f) -> p a f", a=2).unsqueeze(1).to_broadcast([128, H, 2, 32])
            sb_ = kb.ropes[:, t, :].rearrange("p (a f) -> p a f", a=2).unsqueeze(1).to_broadcast([128, H, 2, 32])
            t1, t2, t3, t4 = rt_ring.next(), rt_ring.next(), rt_ring.next(), rt_ring.next()
            kb.tt("dve", t1[:, 0:H], x1, cb, ALU.mult, [xg, kb.ropec], [t1])
            kb.tt("pool", t2[:, 0:H], x2, sb_, ALU.mult, [xg, kb.ropes], [t2])
            kb.tt("dve", o5[:, :, :, 0, :], t1[:, 0:H], t2[:, 0:H], ALU.subtract, [t1, t2], [xr])
            kb.tt("pool", t3[:, 0:H], x1, sb_, ALU.mult, [xg, kb.ropes], [t3])
            kb.tt("dve", t4[:, 0:H], x2, cb, ALU.mult, [xg, kb.ropec], [t4])
            kb.tt("pool", o5[:, :, :, 1, :], t3[:, 0:H], t4[:, 0:H], ALU.add, [t3, t4], [xr])
        pq = pQ.next()
        for h in range(H):
            kb.tr(pq[:, h, :], xr[:, h * 128:(h + 1) * 128], kb.identb[:], [xr, kb.identb], [pq], signal=(h == H - 1))
        kb.cp(alt(), stage[:, h_off:h_off + H, ti * 128:(ti + 1) * 128], pq[:, 0:H, :], [pq], [stage])

    def load_x(t):
        xt = xt_ring.next()
        kb.dma("sp", xt[:], xsrc[t * 128:(t + 1) * 128, :], [kb.xs_k[t]], [xt])
        return xt

    def load_w(bi):
        c0, n, kind, info = IN_BLOCKS[bi]
        w = w_ring.next()
        kb.dma("pool", w[:, :, 0:n], I["w_in"][l, :, c0:c0 + n].rearrange("(k p) c -> p k c", p=128), [], [w])
        return w

    ngroups = (NT + 3) // 4
    xt_next = load_x(0)
    for g in range(ngroups):
        tiles = list(range(4 * g, min(4 * g + 4, NT)))
        nt = len(tiles)
        ntok = nt * 128
        tok0 = tiles[0] * 128
        xnT = xnT_ring.next()
        for ti, t in enumerate(tiles):
            r = 1 if t < 2 else 0
            xt = xt_next
            if t + 1 < NT:
                xt_next = load_x(t + 1)
            ssq = ssq_ring.next()
            kb.act(sqj[:], xt[:], AF.Square, [xt], [sqj, ssq], accum_out=ssq[:, 0:1])
            kb.rsqrt_inplace(ssq[:, 0:1], ssq, 1.0 / D)
            xs = xs_ring.next()
            kb.ts("dve", xs[:], xt[:], ssq[:, 0:1], None, ALU.mult, None, [xt, ssq], [xs])
            for hf in range(2):
                p = psT.next()
                for k8 in range(8):
                    k = hf * 8 + k8
                    kb.tr(p[:, k8, :], xs[:, k * 128:(k + 1) * 128], kb.identb[:], [xs, kb.identb], [p], signal=(k8 == 7))
                tf = tmpf_ring.next()
                kb.tt("dve", tf[:], p[:], kb.gs[:, r, 0, hf * 8:hf * 8 + 8].unsqueeze(2).to_broadcast([128, 8, 128]), ALU.mult, [p, kb.gs], [tf])
                kb.tt("pool", xnT[:, hf * 8:hf * 8 + 8, ti * 128:(ti + 1) * 128], tf[:],
                      kb.modfm[:, r, 0, hf * 8:hf * 8 + 8].unsqueeze(2).to_broadcast([128, 8, 128]), ALU.add, [tf, kb.modfm], [xnT])
        import os
        CUTA = os.environ.get("CUTA", "")
        if CUTA == "A1":
            continue
        w_next = load_w(0)
        for bi, (c0, n, kind, info) in enumerate(IN_BLOCKS):
            w = w_next
            if bi + 1 < len(IN_BLOCKS):
                w_next = load_w(bi + 1)
            if CUTA and kind not in CUTA.split(","):
                continue
            if kind == "fm":
                for j in range(4):
                    P = pP.next()
                    for k in range(16):
                        kb.mm(P[:, 0:ntok], w[:, k, j * 128:(j + 1) * 128], xnT[:, k, 0:ntok], [w, xnT], [P], start=(k == 0), stop=(k == 15))
                    cst = cst_ring.next()
                    kb.cp(alt(), cst[:, 0:ntok], P[:, 0:ntok], [P], [cst])
                    kb.dma("sp", kb.CQ[info["c0"] + j, :, tok0:tok0 + ntok], cst[:, 0:ntok], [cst], [kb.CQ])
                continue
            stage = None
            if kind in ("qk", "kvb"):
                stage = qst_ring.next()
            for ti, t in enumerate(tiles):
                P = pP.next()
                for k in range(16):
                    kb.mm(P[:, 0:n], xnT[:, k, ti * 128:(ti + 1) * 128], w[:, k, 0:n], [w, xnT], [P], start=(k == 0), stop=(k == 15))
                if kind == "qk":
                    qk_post(P, 0, info["H"], info["gain"], info["rope"], t, stage, 0, ti)
                elif kind == "kvb":
                    KVB = os.environ.get("KVB", "ab")
                    if "a" in KVB:
                        qk_post(P, 0, 2, "gq_k", True, t, stage, 0, ti)
                    if "b" in KVB:
                        vst = vst_ring.next()
                        kb.cp("dve", vst[:, 0:256], P[:, 256:512], [P], [vst])
                        kb.dma("sp", kb.V[t * 128:(t + 1) * 128, 4:6, :], vst[:, 0:256].rearrange("p (h d) -> p h d", h=2), [vst], [kb.V])
                elif kind == "v":
                    vst = vst_ring.next()
                    kb.cp("act", vst[:], P[:], [P], [vst])
                    kb.dma("sp", kb.V[t * 128:(t + 1) * 128, 0:4, :], vst[:].rearrange("p (h d) -> p h d", h=4), [vst], [kb.V])
                elif kind == "gate":
                    vst = vst_ring.next()
                    kb.act(vst[:], P[:], AF.Silu, [P], [vst])
                    kb.dma("sp", kb.SG[t * 128:(t + 1) * 128, :], vst[:], [vst], [kb.SG])
                elif kind == "bd":
                    kb.cp("dve", kb.BD[:, t, :], P[:, 0:16], [P], [kb.BD])
            if kind == "qk":
                dst = kb.QT if info["dst"] == "QT" else kb.KT
                h0 = info["h0"]
                kb.dma("sp", dst[h0:h0 + 4, :, tok0:tok0 + ntok].rearrange("h d t -> d h t"), stage[:, 0:4, 0:ntok], [stage], [dst])
            elif kind == "kvb":
                kb.dma("sp", kb.KT[4:6, :, tok0:tok0 + ntok].rearrange("h d t -> d h t"), stage[:, 0:2, 0:ntok], [stage], [kb.KT])
    for nm, tl in (("QT", kb.QT), ("KT", kb.KT), ("V", kb.V), ("CQ", kb.CQ), ("SG", kb.SG)):
        if nm in kb.dbg:
            kb.dma("sp", kb.dbg[nm], tl[:], [tl], [])
    if "BD" in kb.dbg:
        kb.dma("sp", kb.dbg["BD"], kb.BD[:].rearrange("p t c -> p (t c)"), [kb.BD], [])
    kb.end_phase()


def host_consts():
    t = np.arange(NLAT)
    pos = np.stack([t // 64, t % 64], axis=-1).astype(np.float32)
    nf = 32
    freqs = (10000.0 ** (-np.arange(nf, dtype=np.float32) / nf)).astype(np.float32)
    ang = pos[:, :, None] * freqs
    cos = np.zeros((T, 64), np.float32)
    sin = np.zeros((T, 64), np.float32)
    cos[NCTX:] = np.cos(ang).reshape(NLAT, 64)
    sin[NCTX:] = np.sin(ang).reshape(NLAT, 64)
    cos[:NCTX] = 1.0
    out = {
        "ident": np.eye(128, dtype=np.float32),
        "ropec": np.ascontiguousarray(cos.reshape(NT, 128, 64).transpose(1, 0, 2)),
        "ropes": np.ascontiguousarray(sin.reshape(NT, 128, 64).transpose(1, 0, 2)),
    }
    ii = np.arange(128)
    trif = (ii[:, None] <= ii[None, :]).astype(np.float32)
    trib = (ii[:, None] >= ii[None, :]).astype(np.float32)
    out["tri"] = np.stack([trif, trib])
    out["negstrict"] = -(out["tri"] - np.eye(128, dtype=np.float32)[None])
    return out


def fm(v):
    s = v.shape
    return np.ascontiguousarray(np.swapaxes(v.reshape(s[:-1] + (s[-1] // 128, 128)), -1, -2))


def core_inputs(inp, b, layers):
    ls = list(layers)
    d = dict(host_consts())
    d["xin"] = np.ascontiguousarray(np.concatenate([inp["ctx"][b], inp["x"][b]], axis=0))
    cv = np.stack([inp["c"][b], inp["c_ctx"]], axis=0)
    d["cT"] = np.ascontiguousarray(cv.reshape(2, 16, 128).transpose(2, 1, 0))
    d["ada_w"] = inp["ada_w"][ls]
    d["ada_b"] = inp["ada_b"][ls]
    d["ada_b_fm"] = fm(inp["ada_b"][ls])
    d["gmix_fm"] = fm(inp["norm_mix"][ls])
    d["gffn_fm"] = fm(inp["norm_ffn"][ls])
    d["w_in"] = inp["w_in"][ls]
    d["w_out"] = inp["w_out"][ls]
    d["na_gain"] = inp["na_qk_gain"][ls]
    d["gq_gain"] = inp["gqa_qk_gain"][ls]
    d["nab"] = na_bias_tables(inp["na_rpb"][ls])
    cw = inp["dn_conv"][ls]
    d["convw_fm"] = np.ascontiguousarray(cw.reshape(len(ls), 5, 12, 128).transpose(0, 3, 2, 1))
    d["alog_bc"] = np.ascontiguousarray(np.broadcast_to(inp["dn_a_log"][ls].reshape(len(ls), 1, 8), (len(ls), 128, 8)))
    d["dtb_bc"] = np.ascontiguousarray(np.broadcast_to(inp["dn_dt_bias"][ls].reshape(len(ls), 1, 8), (len(ls), 128, 8)))
    d["ogain"] = inp["dn_out_gain"][ls]
    d["router_fm"] = np.ascontiguousarray(inp["router_w"][ls].reshape(len(ls), 16, 128, NE).transpose(0, 2, 1, 3))
    d["w_gate"] = inp["exp_w_gate"][ls]
    d["w_up"] = inp["exp_w_up"][ls]
    d["w_down"] = inp["exp_w_down"][ls]
    return d


def build_program(n_layers, debug=(), phases="0A"):
    nc = bass.Bass("TRN2", target_bir_lowering=False)
    with ExitStack() as st:
        kb = KB(nc, st, n_layers, debug)
        declare_io(kb)
        declare_io_B(kb)
        declare_io_C(kb)
        declare_io_DE(kb)
        setup_consts(kb)
        for l in range(n_layers):
            if "0" in phases:
                phase0(kb, l)
            if "A" in phases:
                phaseA(kb, l)
            if "B" in phases or "a" in phases or "b" in phases:
                phaseB(kb, l, do_a=("B" in phases or "a" in phases), do_b=("B" in phases or "b" in phases))
            if "C" in phases:
                phaseC(kb, l)
            if "D" in phases:
                phaseD(kb, l)
            if "E" in phases:
                phaseE(kb, l)
        kb.fw.barrier()
        print("n_inst", kb.fw.n_inst, "nsem", kb.fw.nsem)
    return nc


_NA_CACHE = {}


def na_patterns():
    if "p" in _NA_CACHE:
        return _NA_CACHE["p"]
    rows, W = 64, 64
    pats = {}
    plist = []
    pairs = {}
    for qt in range(32):
        qr = np.repeat(np.arange(2 * qt, 2 * qt + 2), W)
        qc = np.tile(np.arange(W), 2)
        rstart = np.clip(qr - 4, 0, rows - 8)
        cstart = np.clip(qc - 8, 0, W - 16)
        lst = []
        for kt in range(32):
            kr = np.repeat(np.arange(2 * kt, 2 * kt + 2), W)[:, None]
            kc = np.tile(np.arange(W), 2)[:, None]
            valid = (kr >= rstart[None]) & (kr < rstart[None] + 8) & (kc >= cstart[None]) & (kc < cstart[None] + 16)
            if not valid.any():
                continue
            dr = np.clip(kr - qr[None] + 7, 0, 14)
            dc = np.clip(kc - qc[None] + 15, 0, 30)
            dr = np.where(valid, dr, 0).astype(np.int16)
            dc = np.where(valid, dc, 0).astype(np.int16)
            key = (valid.tobytes(), dr.tobytes(), dc.tobytes())
            if key not in pats:
                pats[key] = len(plist)
                plist.append((valid, dr, dc))
            lst.append((kt, pats[key]))
        pairs[qt] = lst
    _NA_CACHE["p"] = (pairs, plist)
    return pairs, plist


def na_bias_tables(rpb):
    pairs, plist = na_patterns()
    L, H = rpb.shape[0], rpb.shape[1]
    out = np.empty((L, H, len(plist), 128, 128), np.float32)
    for pi, (valid, dr, dc) in enumerate(plist):
        g = rpb[:, :, dr, dc]
        out[:, :, pi] = np.where(valid[None, None], g, np.float32(-30000.0))
    return out


def declare_io_B(kb):
    nc = kb.nc
    pairs, plist = na_patterns()
    kb.NP = len(plist)
    kb.I["nab"] = nc.dram_tensor("nab", [kb.L, 4, kb.NP, 128, 128], F32, kind="ExternalInput").ap()
    kb.OT = kb.dram("OT", [16, 128, T], BF16)
    if "OT" in kb.debug:
        kb.dbg["OT"] = nc.dram_tensor("dbg_OT", [16, 128, T], BF16, kind="ExternalOutput").ap()


def phaseB(kb, l, do_a=True, do_b=True):
    I = kb.I
    nc = kb.nc
    pairs, plist = na_patterns()
    kb.begin_phase()
    kT_ring = Ring([kb.sb([128, T], BF16) for _ in range(2)])
    v1_ring = Ring([kb.sb([128, NT, 130], BF16) for _ in range(2)])
    for v1 in v1_ring.tiles:
        kb.memset("pool", v1[:, :, 128:130], 1.0, [v1])
    qT_ring = Ring([kb.sb([128, 512], BF16) for _ in range(3)])
    pT_ring = Ring([kb.sb([128, 512], BF16) for _ in range(3)])
    sb_ring = Ring([kb.sb([128, 128], F32) for _ in range(3)])
    pS = Ring([kb.ps([128, 512], F32) for _ in range(2)])
    pO = Ring([kb.ps([128, 512], F32) for _ in range(4)])
    pTr = Ring([kb.ps([128, 8, 128], BF16) for _ in range(1)])
    rinv_ring = Ring([kb.sb([128, 4], F32) for _ in range(2)])
    on_ring = Ring([kb.sb([128, 128], BF16) for _ in range(3)])
    ost_ring = Ring([kb.sb([128, 512], BF16) for _ in range(3)])
    nab = None
    if do_a:
        nab = kb.sb([128, kb.NP, 128], F32)

    def load_kv(kvh):
        kT = kT_ring.next()
        v1 = v1_ring.next()
        kb.dma("sp", kT[:], kb.KT[kvh], [kb.KT], [kT])
        kb.dma("sp", v1[:, :, 0:128], kb.V[:, kvh, :].rearrange("(t p) d -> p t d", p=128), [kb.V], [v1])
        return kT, v1

    def attn(qh, kT, v1, chunk, qtiles, keys):
        nq = len(qtiles)
        nqt = nq * 128
        q0 = qtiles[0] * 128
        qT = qT_ring.next()
        kb.dma("sp", qT[:, 0:nqt], kb.QT[qh, :, q0:q0 + nqt], [kb.QT], [qT])
        acc = [pO.next() for _ in range(nq)]
        nk = len(keys)
        for ki, (kt, bias) in enumerate(keys):
            S = pS.next()
            kb.mm(S[:, 0:nqt], kT[:, kt * 128:(kt + 1) * 128], qT[:, 0:nqt], [kT, qT], [S])
            pT = pT_ring.next()
            if bias is None:
                kb.act(pT[:, 0:nqt], S[:, 0:nqt], AF.Exp, [S], [pT])
            else:
                sbt = sb_ring.next()
                kb.tt("dve", sbt[:, 0:nqt], S[:, 0:nqt], bias, ALU.add, [S, nab], [sbt])
                kb.act(pT[:, 0:nqt], sbt[:, 0:nqt], AF.Exp, [sbt], [pT])
            for j in range(nq):
                a = acc[j]
                kb.mm(a[:, 0:129], pT[:, j * 128:(j + 1) * 128], v1[:, kt, 0:129], [pT, v1], [a],
                      start=(ki == 0), stop=(ki == nk - 1), signal=(ki == nk - 1))
        ost = ost_ring.next()
        ptr = pTr.next()
        rinv = rinv_ring.next()
        for j in range(nq):
            a = acc[j]
            off = 0
            kb.fw.op(kb.fw.dve, [a], [rinv], lambda: nc.vector.reciprocal(out=rinv[:, j:j + 1], in_=a[:, off + 128:off + 129]))
            on = on_ring.next()
            kb.act(on[:], a[:, off:off + 128], AF.Copy, [a, rinv], [on], scale=rinv[:, j:j + 1])
            kb.tr(ptr[:, j, :], on[:], kb.identb[:], [on, kb.identb], [ptr])
        kb.cp("dve", ost[:, 0:nqt], ptr[:, 0:nq, :].rearrange("p j q -> p (j q)"), [ptr], [ost])
        kb.dma("sp", kb.OT[chunk, :, q0:q0 + nqt], ost[:, 0:nqt], [ost], [kb.OT])

    if do_a:
        for h in range(4):
            kT, v1 = load_kv(h)
            kb.dma("sp", nab[:], I["nab"][l, h].rearrange("n k q -> k n q"), [], [nab])
            attn(h, kT, v1, h, [0, 1], [(0, None), (1, None)])
            for qt in range(32):
                keys = [(0, None), (1, None)] + [(kt + 2, nab[:, pid, :]) for kt, pid in pairs[qt]]
                attn(h, kT, v1, h, [qt + 2], keys)
    if do_b:
        for kvh in range(2):
            kT, v1 = load_kv(4 + kvh)
            for qi in range(4):
                qh = 4 + kvh * 4 + qi
                attn(qh, kT, v1, qh, [0, 1], [(0, None), (1, None)])
                for g in range(8):
                    attn(qh, kT, v1, qh, [2 + 4 * g + j for j in range(4)], [(kt, None) for kt in range(NT)])
    if "OT" in kb.dbg:
        kb.dma("sp", kb.dbg["OT"][0:12], kb.OT[0:12], [kb.OT], [])
    kb.end_phase()


def declare_io_DE(kb):
    nc = kb.nc
    L = kb.L
    kb.I["router_fm"] = nc.dram_tensor("router_fm", [L, 128, 16, NE], F32, kind="ExternalInput").ap()
    kb.I["w_gate"] = nc.dram_tensor("w_gate", [L, NE, D, DEXP], F32, kind="ExternalInput").ap()
    kb.I["w_up"] = nc.dram_tensor("w_up", [L, NE, D, DEXP], F32, kind="ExternalInput").ap()
    kb.I["w_down"] = nc.dram_tensor("w_down", [L, NE, DEXP, D], F32, kind="ExternalInput").ap()
    kb.H2 = kb.dram("H2", [T, D], BF16)
    kb.xs_c = [Sub() for _ in range(4)]
    for nm, shape, dt in (("xmid", [T, D], F32), ("H2", [T, D], BF16), ("affT", [NE, T], F32), ("idx", [128, 5 * NE], I32), ("gate", [128, 5 * NE], F32)):
        if nm in kb.debug:
            kb.dbg[nm] = nc.dram_tensor("dbg_" + nm, shape, dt, kind="ExternalOutput").ap()


def phaseD(kb, l):
    I = kb.I
    nc = kb.nc
    kb.begin_phase()
    xsrc = I["xin"] if l == 0 else kb.xout
    kb.affT = kb.sb([NE, T], F32, "affT")
    kb.gtb = [[None, None], [None, None]]
    for r in range(2):
        kb.gtb[r][0] = kb.sb([128, D], F32, "gtb")
        kb.dma("sp", kb.gtb[r][0][:], kb.GTS[r * 2 + 0], [kb.GTS], [kb.gtb[r][0]])
    wo = kb.sb([128, 16, D], BF16)
    for q in range(4):
        kb.dma("pool", wo[:, :, q * 512:(q + 1) * 512], I["w_out"][l, :, q * 512:(q + 1) * 512].rearrange("(k p) c -> p k c", p=128), [], [wo])
    rw = kb.sb([128, 16, NE], F32)
    kb.dma("sp", rw[:], I["router_fm"][l], [], [rw])
    ot_ring = Ring([kb.sb([128, 16, 512], BF16) for _ in range(2)])
    xt_ring = Ring([kb.sb([128, D], F32) for _ in range(2)])
    xn_ring = Ring([kb.sb([128, D], F32) for _ in range(1)])
    tmp_ring = Ring([kb.sb([128, 512], F32) for _ in range(3)])
    sqj = kb.sb([128, D], BF16)
    ssq_ring = Ring([kb.sb([128, 1], F32) for _ in range(2)])
    xs2_ring = Ring([kb.sb([128, D], F32) for _ in range(1)])
    xs2b_ring = Ring([kb.sb([128, D], BF16) for _ in range(1)])
    h2T_ring = Ring([kb.sb([128, 16, 128], F32) for _ in range(1)])
    tf_ring = Ring([kb.sb([128, 4, 128], F32) for _ in range(2)])
    sm_ring = Ring([kb.sb([128, 4], F32) for _ in range(2)])
    lg_ring = Ring([kb.sb([128, NE], F32) for _ in range(2)])
    pP = Ring([kb.ps([128, 512], F32) for _ in range(3)])
    pT = Ring([kb.ps([128, 4, 128], F32) for _ in range(2)])
    pL = Ring([kb.ps([128, 512], F32) for _ in range(2)])

    def load_ot(g):
        tiles = list(range(4 * g, min(4 * g + 4, NT)))
        ot = ot_ring.next()
        n = len(tiles) * 128
        kb.dma("sp", ot[:, :, 0:n], kb.OT[:, :, tiles[0] * 128:tiles[0] * 128 + n].rearrange("k d t -> d k t"), [kb.OT], [ot])
        return ot

    def load_x(t):
        xt = xt_ring.next()
        kb.dma("sp", xt[:], xsrc[t * 128:(t + 1) * 128, :], [kb.xs_k[t]], [xt])
        return xt

    ngroups = (NT + 3) // 4
    ot_next = load_ot(0)
    xt_next = load_x(0)
    for g in range(ngroups):
        tiles = list(range(4 * g, min(4 * g + 4, NT)))
        ot = ot_next
        if g + 1 < ngroups:
            ot_next = load_ot(g + 1)
        for ti, t in enumerate(tiles):
            r = 1 if t < 2 else 0
            xt = xt_next
            if t + 1 < NT:
                xt_next = load_x(t + 1)
            xn = xn_ring.next()
            for cb in range(4):
                P = pP.next()
                for k in range(16):
                    kb.mm(P[:], ot[:, k, ti * 128:(ti + 1) * 128], wo[:, k, cb * 512:(cb + 1) * 512], [ot, wo], [P], start=(k == 0), stop=(k == 15))
                tmp = tmp_ring.next()
                kb.tt("dve", tmp[:], P[:], kb.gtb[r][0][:, cb * 512:(cb + 1) * 512], ALU.mult, [P, kb.gtb[r][0]], [tmp])
                kb.tt("pool", xn[:, cb * 512:(cb + 1) * 512], tmp[:], xt[:, cb * 512:(cb + 1) * 512], ALU.add, [tmp, xt], [xn])
            kb.dma("sp", kb.xout[t * 128:(t + 1) * 128, :], xn[:], [xn], [kb.xs_k[t]])
            ssq = ssq_ring.next()
            kb.act(sqj[:], xn[:], AF.Square, [xn], [sqj, ssq], accum_out=ssq[:, 0:1])
            kb.rsqrt_inplace(ssq[:, 0:1], ssq, 1.0 / D)
            xs2 = xs2_ring.next()
            kb.ts("dve", xs2[:], xn[:], ssq[:, 0:1], None, ALU.mult, None, [xn, ssq], [xs2])
            xs2b = xs2b_ring.next()
            kb.cp("pool", xs2b[:], xs2[:], [xs2], [xs2b])
            kb.dma("sp", kb.H2[t * 128:(t + 1) * 128, :], xs2b[:], [xs2b], [kb.H2])
            h2T = h2T_ring.next()
            for q in range(4):
                p = pT.next()
                for j in range(4):
                    k = q * 4 + j
                    kb.tr(p[:, j, :], xs2[:, k * 128:(k + 1) * 128], kb.identf[:], [xs2, kb.identf], [p], signal=(j == 3))
                tf = tf_ring.next()
                kb.tt("dve", tf[:], p[:], kb.gs[:, r, 1, q * 4:q * 4 + 4].unsqueeze(2).to_broadcast([128, 4, 128]), ALU.mult, [p, kb.gs], [tf])
                kb.tt("pool", h2T[:, q * 4:q * 4 + 4, :], tf[:], kb.modfm[:, r, 2, q * 4:q * 4 + 4].unsqueeze(2).to_broadcast([128, 4, 128]), ALU.add, [tf, kb.modfm], [h2T])
            PL = pL.next()
            for k in range(16):
                kb.mm(PL[:, 0:NE], h2T[:, k, :], rw[:, k, :], [h2T, rw], [PL], start=(k == 0), stop=(k == 15))
            sm = sm_ring.next()
            kb.fw.op(kb.fw.dve, [PL], [sm], lambda: nc.vector.tensor_reduce(out=sm[:, 0:1], in_=PL[:, 0:NE], axis=AX.X, op=ALU.max))
            kb.ts("dve", sm[:, 1:2], sm[:, 0:1], -1.0, None, ALU.mult, None, [sm], [sm])
            lg = lg_ring.next()
            kb.act(lg[:], PL[:, 0:NE], AF.Exp, [PL, sm], [lg, sm], bias=sm[:, 1:2], accum_out=sm[:, 2:3])
            kb.fw.op(kb.fw.dve, [sm], [sm], lambda: nc.vector.reciprocal(out=sm[:, 3:4], in_=sm[:, 2:3]))
            kb.ts("dve", lg[:], lg[:], sm[:, 3:4], None, ALU.mult, None, [lg, sm], [lg])
            kb.tr(PL[0:NE, 128:256], lg[:], kb.identf[:], [lg, kb.identf], [PL])
            kb.cp("act", kb.affT[:, t * 128:(t + 1) * 128], PL[0:NE, 128:256], [PL], [kb.affT])
    kb.dma("sp", kb.AFFT[:], kb.affT[:], [kb.affT], [kb.AFFT])
    if "xmid" in kb.dbg:
        kb.dma("sp", kb.dbg["xmid"], kb.xout, [], kb.xs_k)
    if "H2" in kb.dbg:
        kb.dma("sp", kb.dbg["H2"], kb.H2[:], [kb.H2], [])
    if "affT" in kb.dbg:
        kb.dma("sp", kb.dbg["affT"], kb.affT[:], [kb.affT], [])
    kb.end_phase()


def phaseE(kb, l):
    I = kb.I
    nc = kb.nc
    kb.begin_phase()
    NS = 544
    kb.affT = kb.sb([NE, T], F32, "affT")
    kb.dma("sp", kb.affT[:], kb.AFFT[:], [kb.AFFT], [kb.affT])
    kb.gtb = [[None, None], [None, None]]
    for r in range(2):
        kb.gtb[r][1] = kb.sb([128, D], F32, "gtb")
        kb.dma("sp", kb.gtb[r][1][:], kb.GTS[r * 2 + 1], [kb.GTS], [kb.gtb[r][1]])
    work = kb.sb([NE, NLAT], F32)
    workc = kb.sb([NE, NCTX], F32)
    vals = kb.sb([NE, NS], F32)
    idxu = kb.sb([NE, NS], U32)
    idxf = kb.sb([NE, NS], F32)
    kb.cp("dve", work[:], kb.affT[:, NCTX:T], [kb.affT], [work])
    kb.cp("pool", workc[:], kb.affT[:, 0:NCTX], [kb.affT], [workc])
    for (wk, base, nround) in ((work, 0, 64), (workc, 512, 4)):
        for i in range(nround):
            c0 = base + i * 8
            kb.fw.op(kb.fw.dve, [wk], [vals], lambda: nc.vector.max(out=vals[:, c0:c0 + 8], in_=wk[:]))
            kb.fw.op(kb.fw.dve, [wk, vals], [idxu], lambda: nc.vector.max_index(out=idxu[:, c0:c0 + 8], in_max=vals[:, c0:c0 + 8], in_values=wk[:]))
            if i + 1 < nround:
                kb.fw.op(kb.fw.dve, [vals, wk], [wk], lambda: nc.vector.match_replace(out=wk[:], in_to_replace=vals[:, c0:c0 + 8], in_values=wk[:], imm_value=-1.0))
    kb.cp("dve", idxf[:], idxu[:], [idxu], [idxf])
    kb.ts("dve", idxf[:, 0:512], idxf[:, 0:512], float(NCTX), None, ALU.add, None, [idxf], [idxf])
    idxT = kb.sb([128, 5, NE], I32)
    gateT = kb.sb([128, 5, NE], F32)
    kb.memset("dve", idxT[:], 0, [idxT])
    kb.memset("dve", gateT[:], 0.0, [gateT])
    pX = kb.ps([128, 512], F32)
    for s in range(5):
        n = 128 if s < 4 else 32
        kb.tr(pX[0:n, 0:NE], idxf[:, s * 128:s * 128 + n], kb.identf[0:NE, 0:NE], [idxf, kb.identf], [pX])
        kb.cp("dve", idxT[0:n, s, :], pX[0:n, 0:NE], [pX], [idxT])
        kb.tr(pX[0:n, 64:64 + NE], vals[:, s * 128:s * 128 + n], kb.identf[0:NE, 0:NE], [vals, kb.identf], [pX])
        kb.cp("dve", gateT[0:n, s, :], pX[0:n, 64:64 + NE], [pX], [gateT])
    if "idx" in kb.dbg:
        kb.dma("sp", kb.dbg["idx"], idxT[:].rearrange("p s e -> p (s e)"), [idxT], [])
        kb.dma("sp", kb.dbg["gate"], gateT[:].rearrange("p s e -> p (s e)"), [gateT], [])
    xe_ring = Ring([kb.sb([128, D], BF16) for _ in range(3)])
    xeT = kb.sb([128, 16, NS], BF16)
    tf_ring = Ring([kb.sb([128, 8, 128], F32) for _ in range(2)])
    wg_ring = Ring([kb.sb([128, 16, 256], BF16) for _ in range(2)])
    wu_ring = Ring([kb.sb([128, 16, 256], BF16) for _ in range(2)])
    wd_ring = Ring([kb.sb([128, 8, 512], BF16) for _ in range(2)])
    hidT = kb.sb([128, 8, NS], BF16)
    sg_ring = Ring([kb.sb([128, NS], F32) for _ in range(2)])
    yes = [kb.sb([128, D], F32) for _ in range(5)]
    pTr = Ring([kb.ps([128, 8, 128], BF16) for _ in range(2)])
    pG = Ring([kb.ps([128, 512], F32) for _ in range(1)])
    pGc = Ring([kb.ps([128, 512], F32) for _ in range(1)])
    pU = Ring([kb.ps([128, 512], F32) for _ in range(1)])
    pY = Ring([kb.ps([128, 512], F32) for _ in range(2)])
    for e in range(NE):
        for s in range(5):
            n = 128 if s < 4 else 32
            r = 0 if s < 4 else 1
            xe = xe_ring.next()
            kb.fw.dma(kb.fw.pool, [idxT, kb.H2], [xe], lambda q: q.indirect_dma_start(
                out=xe[0:n, :], out_offset=None, in_=kb.H2[:], in_offset=bass.IndirectOffsetOnAxis(ap=idxT[0:n, s, e:e + 1], axis=0)))
            for hf in range(2):
                p = pTr.next()
                for k8 in range(8):
                    k = hf * 8 + k8
                    kb.tr(p[:, k8, 0:n], xe[0:n, k * 128:(k + 1) * 128], kb.identb[0:n, 0:n], [xe, kb.identb], [p], signal=(k8 == 7))
                tf = tf_ring.next()
                kb.tt("dve", tf[:, :, 0:n], p[:, :, 0:n], kb.gs[:, r, 1, hf * 8:hf * 8 + 8].unsqueeze(2).to_broadcast([128, 8, n]), ALU.mult, [p, kb.gs], [tf])
                kb.tt("pool", xeT[:, hf * 8:hf * 8 + 8, s * 128:s * 128 + n], tf[:, :, 0:n],
                      kb.modfm[:, r, 2, hf * 8:hf * 8 + 8].unsqueeze(2).to_broadcast([128, 8, n]), ALU.add, [tf, kb.modfm], [xeT])
        for cq in range(4):
            wg = wg_ring.next()
            wu = wu_ring.next()
            kb.dma("pool", wg[:], I["w_gate"][l, e, :, cq * 256:(cq + 1) * 256].rearrange("(k p) c -> p k c", p=128), [], [wg])
            kb.dma("pool", wu[:], I["w_up"][l, e, :, cq * 256:(cq + 1) * 256].rearrange("(k p) c -> p k c", p=128), [], [wu])
            for cj in range(2):
                c = cq * 2 + cj
                G, U, Gc = pG.next(), pU.next(), pGc.next()
                for k in range(16):
                    kb.mm(G[:], wg[:, k, cj * 128:(cj + 1) * 128], xeT[:, k, 0:512], [wg, xeT], [G], start=(k == 0), stop=(k == 15))
                for k in range(16):
                    kb.mm(Gc[:, 0:32], wg[:, k, cj * 128:(cj + 1) * 128], xeT[:, k, 512:544], [wg, xeT], [Gc], start=(k == 0), stop=(k == 15))
                for k in range(16):
                    kb.mm(U[:], wu[:, k, cj * 128:(cj + 1) * 128], xeT[:, k, 0:512], [wu, xeT], [U], start=(k == 0), stop=(k == 15))
                for k in range(16):
                    kb.mm(Gc[:, 32:64], wu[:, k, cj * 128:(cj + 1) * 128], xeT[:, k, 512:544], [wu, xeT], [Gc], start=(k == 0), stop=(k == 15))
                sg = sg_ring.next()
                kb.act(sg[:, 0:512], G[:], AF.Silu, [G], [sg])
                kb.act(sg[:, 512:544], Gc[:, 0:32], AF.Silu, [Gc], [sg])
                kb.tt("dve", hidT[:, c, 0:512], sg[:, 0:512], U[:], ALU.mult, [sg, U], [hidT])
                kb.tt("dve", hidT[:, c, 512:544], sg[:, 512:544], Gc[:, 32:64], ALU.mult, [sg, Gc], [hidT])
        for cb in range(4):
            wd = wd_ring.next()
            kb.dma("pool", wd[:], I["w_down"][l, e, :, cb * 512:(cb + 1) * 512].rearrange("(c p) n -> p c n", p=128), [], [wd])
            for s in range(5):
                n = 128 if s < 4 else 32
                r = 0 if s < 4 else 1
                Y = pY.next()
                for c in range(8):
                    kb.mm(Y[0:n, :], hidT[:, c, s * 128:s * 128 + n], wd[:, c, :], [hidT, wd], [Y], start=(c == 0), stop=(c == 7))
                ye = yes[s]
                kb.fw.op(kb.fw.dve, [Y, gateT, kb.gtb[r][1]], [ye], lambda: nc.vector.scalar_tensor_tensor(
                    out=ye[0:n, cb * 512:(cb + 1) * 512], in0=Y[0:n, :], scalar=gateT[0:n, s, e:e + 1], in1=kb.gtb[r][1][0:n, cb * 512:(cb + 1) * 512],
                    op0=ALU.mult, op1=ALU.mult))
        for s in range(5):
            n = 128 if s < 4 else 32
            ye = yes[s]
            kb.fw.dma(kb.fw.pool, [ye, idxT], [kb.xs_c[0]], lambda q: q.indirect_dma_start(
                out=kb.xout, out_offset=bass.IndirectOffsetOnAxis(ap=idxT[0:n, s, e:e + 1], axis=0),
                in_=ye[0:n, :], in_offset=None, compute_op=ALU.add))
    kb.end_phase()


def declare_io_C(kb):
    nc = kb.nc
    L = kb.L
    for nm, shape in (("convw_fm", [L, 128, 12, 5]), ("alog_bc", [L, 128, 8]), ("dtb_bc", [L, 128, 8]),
                      ("ogain", [L, 128]), ("tri", [2, 128, 128]), ("negstrict", [2, 128, 128])):
        kb.I[nm] = nc.dram_tensor(nm, shape, F32, kind="ExternalInput").ap()
    if "OTC" in kb.debug:
        kb.dbg["OTC"] = nc.dram_tensor("dbg_OTC", [4, 128, T], BF16, kind="ExternalOutput").ap()
    if "odn" in kb.debug:
        kb.dbg["odn"] = nc.dram_tensor("dbg_odn", [4, 128, NT * 128], F32, kind="ExternalOutput").ap()


def phaseC(kb, l):
    I = kb.I
    nc = kb.nc
    kb.begin_phase()
    SEGS = ((0, NCTX), (NCTX, T))
    tri = kb.sb([128, 2, 128], F32)
    nst = kb.sb([128, 2, 128], F32)
    for d in range(2):
        kb.dma("sp", tri[:, d, :], I["tri"][d], [], [tri])
        kb.dma("sp", nst[:, d, :], I["negstrict"][d], [], [nst])
    ones = kb.sb([128, 128], F32)
    kb.memset("dve", ones[:], 1.0, [ones])
    cw = kb.sb([128, 12, 5], F32)
    kb.dma("sp", cw[:], I["convw_fm"][l], [], [cw])
    ab = kb.sb([128, 2, 8], F32)
    kb.dma("sp", ab[:, 0, :], I["alog_bc"][l], [], [ab])
    kb.dma("sp", ab[:, 1, :], I["dtb_bc"][l], [], [ab])
    og = kb.sb([128, 128], F32)
    kb.dma("sp", og[:], I["ogain"][l].partition_broadcast(128), [], [og])
    beta = kb.sb([128, NT, 8], F32)
    gg = kb.sb([128, NT, 8], F32)
    tmp8 = kb.sb([128, NT, 8], F32)
    kb.act(beta[:], kb.BD[:, :, 0:8], AF.Sigmoid, [kb.BD], [beta])
    kb.tt("dve", tmp8[:], kb.BD[:, :, 8:16], ab[:, 1, :].unsqueeze(1).to_broadcast([128, NT, 8]), ALU.add, [kb.BD, ab], [tmp8])
    kb.act(tmp8[:], tmp8[:], AF.Exp, [tmp8], [tmp8])
    kb.act(tmp8[:], tmp8[:], AF.Ln, [tmp8], [tmp8], bias=1.0)
    kb.act(ab[:, 0, :], ab[:, 0, :], AF.Exp, [ab], [ab])
    kb.tt("dve", gg[:], tmp8[:], ab[:, 0, :].unsqueeze(1).to_broadcast([128, NT, 8]), ALU.mult, [tmp8, ab], [gg])
    kb.ts("dve", gg[:], gg[:], -1.0, None, ALU.mult, None, [gg], [gg])

    qT = kb.sb([128, T], F32)
    kT = kb.sb([128, T], F32)
    ktm = kb.sb([128, NT, 128], F32)
    vtm = kb.sb([128, NT, 128], F32)
    otot = kb.sb([128, NT, 128], F32)
    sgt = kb.sb([128, NT, 128], BF16)
    xc_ring = Ring([kb.sb([128, T], F32) for _ in range(1)])
    yc = kb.sb([128, T], F32)
    sc_ring = Ring([kb.sb([128, 512], F32) for _ in range(2)])
    sm = {nm: kb.sb([128, 2, NT], F32) for nm in ("Gcol", "glast", "expG", "bg", "etail", "eglast")}
    S = [kb.sb([128, 128], F32) for _ in range(2)]
    pPre = Ring([kb.ps([128, 4, 128], F32) for _ in range(2)])
    pSol = [kb.ps([128, 4, 128], F32) for _ in range(4)]
    pRec = Ring([kb.ps([128, 4, 128], F32) for _ in range(2)])
    W = {}

    def wt(name, n, shape=(128, 128), dt=F32):
        W[name] = Ring([kb.sb(list(shape), dt) for _ in range(n)])

    for nm in ("rhsA", "rhsB", "E", "DT", "DTb", "M", "Rv", "Rk", "kt", "vnew", "tmpo", "on", "onb"):
        wt(nm, 2)
    for nm in ("Q", "Qt", "Tt", "QKD", "wT", "u"):
        wt(nm, 4)
    wt("ssq", 4, (128, 1))
    ostage = Ring([kb.sb([128, 512], BF16) for _ in range(2)])
    pTrb = None

    for h in range(4):
        for which, c in (("q", h), ("k", 4 + h), ("v", 8 + h)):
            xc = xc_ring.next()
            kb.dma("sp", xc[:], kb.CQ[c], [kb.CQ], [xc])
            kb.ts("dve", yc[:], xc[:], cw[:, c, 2:3], None, ALU.mult, None, [xc, cw], [yc])
            for j in (0, 1, 3, 4):
                s = j - 2
                for (lo, hi) in SEGS:
                    a, b = max(lo, lo - s), min(hi, hi - s)
                    eng = "dve"
                    kb.fw.op(kb._e(eng), [xc, cw, yc], [yc], lambda: kb._ne(eng).scalar_tensor_tensor(
                        out=yc[:, a:b], in0=xc[:, a + s:b + s], scalar=cw[:, c, j:j + 1], in1=yc[:, a:b], op0=ALU.mult, op1=ALU.add))
            dst = {"q": qT, "k": kT, "v": xc}[which]
            if which == "v":
                kb.act(xc[:], yc[:], AF.Silu, [yc], [xc])
                for t in range(NT):
                    p = pPre.next()
                    kb.tr(p[:, 0, :], xc[:, t * 128:(t + 1) * 128], kb.identf[:], [xc, kb.identf], [p])
                    kb.cp("act" if t % 2 else "dve", vtm[:, t, :], p[:, 0, :], [p], [vtm])
                continue
            kb.act(yc[:], yc[:], AF.Silu, [yc], [yc])
            for blk in range((T + 511) // 512):
                a, b = blk * 512, min(T, blk * 512 + 512)
                n = b - a
                sq = sc_ring.next()
                kb.tt("pool", sq[:, 0:n], yc[:, a:b], yc[:, a:b], ALU.mult, [yc], [sq])
                p = pPre.next()
                pv = p[:].rearrange("p a b -> p (a b)")
                kb.mm(pv[:, 0:n], ones[:], sq[:, 0:n], [ones, sq], [p])
                kb.act(sq[:, 0:n], pv[:, 0:n], AF.Ln, [p, kb.epsc], [sq], bias=kb.epsc[:, 0:1])
                kb.act(sq[:, 0:n], sq[:, 0:n], AF.Exp, [sq], [sq], scale=-0.5)
                if which == "q":
                    kb.fw.op(kb.fw.dve, [yc, sq], [dst], lambda: nc.vector.scalar_tensor_tensor(
                        out=dst[:, a:b], in0=yc[:, a:b], scalar=128.0 ** -0.5, in1=sq[:, 0:n], op0=ALU.mult, op1=ALU.mult))
                else:
                    kb.tt("dve", dst[:, a:b], yc[:, a:b], sq[:, 0:n], ALU.mult, [yc, sq], [dst])
            if which == "k":
                for t in range(NT):
                    p = pPre.next()
                    kb.tr(p[:, 0, :], kT[:, t * 128:(t + 1) * 128], kb.identf[:], [kT, kb.identf], [p])
                    kb.cp("act" if t % 2 else "dve", ktm[:, t, :], p[:, 0, :], [p], [ktm])
        kb.dma("sp", sgt[:], kb.SG[:, h * 128:(h + 1) * 128].rearrange("(t p) e -> p t e", p=128), [kb.SG], [sgt])
        import os
        CUTC = os.environ.get("CUTC", "")
        if CUTC == "1":
            break
        for d in range(2):
            dh = d * 4 + h
            p = pPre.next()
            kb.mm(p[:, 0, 0:NT], tri[:, d, :], gg[:, :, dh], [tri, gg], [p])
            kb.mm(p[:, 1, 0:NT], ones[:], gg[:, :, dh], [ones, gg], [p])
            kb.cp("dve", sm["Gcol"][:, d, :], p[:, 0, 0:NT], [p], [sm["Gcol"]])
            kb.cp("dve", sm["glast"][:, d, :], p[:, 1, 0:NT], [p], [sm["glast"]])
            kb.act(sm["expG"][:, d, :], p[:, 0, 0:NT], AF.Exp, [p], [sm["expG"]])
            kb.act(sm["eglast"][:, d, :], p[:, 1, 0:NT], AF.Exp, [p], [sm["eglast"]])
            kb.tt("dve", sm["bg"][:, d, :], sm["expG"][:, d, :], beta[:, :, dh], ALU.mult, [sm["expG"], beta], [sm["bg"]])
            kb.tt("dve", sm["etail"][:, d, :], sm["glast"][:, d, :], sm["Gcol"][:, d, :], ALU.subtract, [sm["glast"], sm["Gcol"]], [sm["etail"]])
            kb.act(sm["etail"][:, d, :], sm["etail"][:, d, :], AF.Exp, [sm["etail"]], [sm["etail"]])
            kb.memset("dve", S[d][:], 0.0, [S[d]])
        kb.memset("pool", otot[:], 0.0, [otot])
        order = [list(range(NT)), [1, 0] + list(range(NT - 1, 1, -1))]
        if CUTC == "2":
            break
        for step in range(NT if not CUTC.startswith("3") else int(CUTC[1:])):
            jobs = [(d, order[d][step]) for d in range(2)]
            st = {}
            for ji, (d, c) in enumerate(jobs):
                dh = d * 4 + h
                cs = slice(c * 128, (c + 1) * 128)
                rhsA, rhsB = W["rhsA"].next(), W["rhsB"].next()
                kb.ts("pool", rhsA[:], tri[:, d, :], gg[:, c, dh:dh + 1], None, ALU.mult, None, [tri, gg], [rhsA])
                kb.ts("pool", rhsB[:], kb.identf[:], beta[:, c, dh:dh + 1], None, ALU.mult, None, [kb.identf, beta], [rhsB])
                p = pPre.next()
                kb.mm(p[:, 0, :], ones[:], rhsA[:], [ones, rhsA], [p])
                kb.mm(p[:, 1, :], ones[:], rhsB[:], [ones, rhsB], [p])
                kb.mm(p[:, 2, :], kT[:, cs], kT[:, cs], [kT], [p])
                kb.mm(p[:, 3, :], kT[:, cs], qT[:, cs], [kT, qT], [p])
                E = W["E"].next()
                kb.ts("dve", E[:], p[:, 0, :], sm["Gcol"][:, d, c:c + 1], 0.0, ALU.subtract, ALU.min, [p, sm["Gcol"]], [E])
                kb.act(E[:], E[:], AF.Exp, [E], [E])
                DT = W["DT"].next()
                kb.tt("pool", DT[:], E[:], tri[:, d, :], ALU.mult, [E, tri], [DT])
                DTb = W["DTb"].next()
                kb.tt("dve", DTb[:], p[:, 1, :], DT[:], ALU.mult, [p, DT], [DTb])
                M = W["M"].next()
                kb.tt("dve", M[:], p[:, 2, :], DTb[:], ALU.mult, [p, DTb], [M])
                Qt = W["Qt"].next()
                kb.tt("pool", Qt[:], M[:], nst[:, d, :], ALU.mult, [M, nst], [Qt])
                QKD = W["QKD"].next()
                kb.tt("dve", QKD[:], p[:, 3, :], DT[:], ALU.mult, [p, DT], [QKD])
                ps = pSol[ji]
                kb.tr(ps[:, 0, :], Qt[:], kb.identf[:], [Qt, kb.identf], [ps])
                Q = W["Q"].next()
                kb.cp("act", Q[:], ps[:, 0, :], [ps], [Q])
                Tt = W["Tt"].next()
                kb.tt("pool", Tt[:], Qt[:], kb.identf[:], ALU.add, [Qt, kb.identf], [Tt])
                st[ji] = dict(Q=Q, Qt=Qt, Tt=Tt, QKD=QKD, ps=ps)
            CUTS = os.environ.get("CUTS", "")
            if CUTS == "a":
                continue
            for k in range(1, 7):
                for ji in range(2):
                    s_ = st[ji]
                    ps = s_["ps"]
                    kb.mm(ps[:, 0, :], s_["Qt"][:], s_["Q"][:], [s_["Qt"], s_["Q"]], [ps])
                    if k < 6:
                        kb.mm(ps[:, 1, :], s_["Q"][:], s_["Qt"][:], [s_["Qt"], s_["Q"]], [ps])
                    Qn = W["Q"].next()
                    kb.cp("act", Qn[:], ps[:, 0, :], [ps], [Qn])
                    if k < 6:
                        Qtn = W["Qt"].next()
                        kb.cp("dve", Qtn[:], ps[:, 1, :], [ps], [Qtn])
                        s_["Qt"] = Qtn
                    s_["Q"] = Qn
                    kb.mm(ps[:, 2, :], Qn[:], s_["Tt"][:], [Qn, s_["Tt"]], [ps])
                    Tn = W["Tt"].next()
                    kb.tt("dve", Tn[:], ps[:, 2, :], s_["Tt"][:], ALU.add, [ps, s_["Tt"]], [Tn])
                    s_["Tt"] = Tn
            if CUTS == "b":
                continue
            for ji, (d, c) in enumerate(jobs):
                dh = d * 4 + h
                cs = slice(c * 128, (c + 1) * 128)
                s_ = st[ji]
                ps = s_["ps"]
                Rv, Rk, kt_ = W["Rv"].next(), W["Rk"].next(), W["kt"].next()
                kb.ts("pool", Rv[:], vtm[:, c, :], beta[:, c, dh:dh + 1], None, ALU.mult, None, [vtm, beta], [Rv])
                kb.ts("pool", Rk[:], ktm[:, c, :], sm["bg"][:, d, c:c + 1], None, ALU.mult, None, [ktm, sm["bg"]], [Rk])
                kb.ts("pool", kt_[:], ktm[:, c, :], sm["etail"][:, d, c:c + 1], None, ALU.mult, None, [ktm, sm["etail"]], [kt_])
                kb.mm(ps[:, 0, :], s_["Tt"][:], Rv[:], [s_["Tt"], Rv], [ps])
                kb.mm(ps[:, 1, :], Rk[:], s_["Tt"][:], [s_["Tt"], Rk], [ps])
                u, wT = W["u"].next(), W["wT"].next()
                kb.cp("act", u[:], ps[:, 0, :], [ps], [u])
                kb.cp("dve", wT[:], ps[:, 1, :], [ps], [wT])
                pr = pRec.next()
                kb.mm(pr[:, 0, :], wT[:], S[d][:], [wT, S[d]], [pr])
                kb.mm(pr[:, 1, :], qT[:, cs], S[d][:], [qT, S[d]], [pr])
                vnew = W["vnew"].next()
                kb.tt("dve", vnew[:], u[:], pr[:, 0, :], ALU.subtract, [u, pr], [vnew])
                kb.mm(pr[:, 2, :], s_["QKD"][:], vnew[:], [s_["QKD"], vnew], [pr])
                kb.mm(pr[:, 3, :], kt_[:], vnew[:], [kt_, vnew], [pr])
                tmpo = W["tmpo"].next()
                kb.act(tmpo[:], pr[:, 1, :], AF.Copy, [pr, sm["expG"]], [tmpo], scale=sm["expG"][:, d, c:c + 1])
                kb.tt("dve", tmpo[:], tmpo[:], pr[:, 2, :], ALU.add, [tmpo, pr], [tmpo])
                kb.tt("pool", otot[:, c, :], otot[:, c, :], tmpo[:], ALU.add, [tmpo, otot], [otot])
                kb.fw.op(kb.fw.dve, [S[d], sm["eglast"], pr], [S[d]], lambda: nc.vector.scalar_tensor_tensor(
                    out=S[d][:], in0=S[d][:], scalar=sm["eglast"][:, d, c:c + 1], in1=pr[:, 3, :], op0=ALU.mult, op1=ALU.add))
        if CUTC:
            break
        if "odn" in kb.dbg:
            kb.dma("sp", kb.dbg["odn"][h], otot[:].rearrange("p t e -> p (t e)"), [otot], [])
        for g4 in range((NT + 3) // 4):
            tiles = list(range(4 * g4, min(4 * g4 + 4, NT)))
            ost = ostage.next()
            p = pPre.next()
            pb = p[:].rearrange("p a b -> p (a b)").bitcast(BF16)
            for ti, t in enumerate(tiles):
                ssq = W["ssq"].next()
                on = W["on"].next()
                kb.act(on[:], otot[:, t, :], AF.Square, [otot], [on, ssq], accum_out=ssq[:, 0:1])
                kb.rsqrt_inplace(ssq[:, 0:1], ssq, 1.0 / 128)
                kb.fw.op(kb.fw.dve, [otot, ssq, og], [on], lambda: nc.vector.scalar_tensor_tensor(
                    out=on[:], in0=otot[:, t, :], scalar=ssq[:, 0:1], in1=og[:], op0=ALU.mult, op1=ALU.mult))
                onb = W["onb"].next()
                kb.tt("pool", onb[:].bitcast(BF16)[:, 0:128], on[:], sgt[:, t, :], ALU.mult, [on, sgt], [onb])
                kb.tr(pb[:, ti * 128:(ti + 1) * 128], onb[:].bitcast(BF16)[:, 0:128], kb.identb[:], [onb, kb.identb], [p])
            n = len(tiles) * 128
            kb.cp("dve", ost[:, 0:n], pb[:, 0:n], [p], [ost])
            kb.dma("sp", kb.OT[12 + h, :, tiles[0] * 128:tiles[0] * 128 + n], ost[:, 0:n], [ost], [kb.OT])
    if "OTC" in kb.dbg:
        kb.dma("sp", kb.dbg["OTC"], kb.OT[12:16], [kb.OT], [])
    kb.end_phase()


_PROG = {}


def _get_prog(n_layers):
    if n_layers not in _PROG:
        _PROG[n_layers] = build_program(n_layers, debug=(), phases="0ABCDE")
    return _PROG[n_layers]


def kernel(**inputs):
    inp = {k: np.asarray(v) for k, v in inputs.items()}
    B = inp["x"].shape[0]
    depth = inp["w_in"].shape[0]
    nc = _get_prog(1)
    xs = [np.ascontiguousarray(np.concatenate([inp["ctx"][b], inp["x"][b]], axis=0)) for b in range(B)]
    for l in range(depth):
        base = core_inputs(inp, 0, [l])
        in_maps = []
        for b in range(B):
            d = dict(base)
            d["xin"] = xs[b]
            cv = np.stack([inp["c"][b], inp["c_ctx"]], axis=0)
            d["cT"] = np.ascontiguousarray(cv.reshape(2, 16, 128).transpose(2, 1, 0))
            in_maps.append(d)
        res = run_bass_kernel_spmd(nc, in_maps, core_ids=list(range(B)))
        xs = [np.asarray(res.results[b]["xout"]) for b in range(B)]
    return np.stack([xs[b][NCTX:] for b in range(B)], axis=0).astype(np.float32)
```

```python
import math
import numpy as np
from contextlib import ExitStack
import concourse.bass as bass
import concourse.mybir as mybir
from concourse.bass_utils import run_bass_kernel_spmd

F32 = mybir.dt.float32
BF16 = mybir.dt.bfloat16
I32 = mybir.dt.int32
U32 = mybir.dt.uint32
AF = mybir.ActivationFunctionType
ALU = mybir.AluOpType
AX = mybir.AxisListType

SEM_ROLL = 30000

D = 2048
NCTX = 256
NLAT = 4096
T = NCTX + NLAT
NT = T // 128
DIN = 5136
NE = 16
DEXP = 1024
EPS = 1e-6


class Tk:
    __slots__ = ("w", "r")

    def __init__(self):
        self.w = None
        self.r = {}


class Tile:
    def __init__(self, t, is_psum=False):
        self.t = t
        self.k = Tk()
        self.is_psum = is_psum

    def __getitem__(self, idx):
        return self.t[idx]


class Eng:
    def __init__(self, name, e):
        self.name = name
        self.e = e
        self.sem = None
        self.cnt = 0
        self.seen = {}
        self.same_sync = name in ("act", "dve", "pool")
        self.last_tok = None
        self.pending = False


class FW:
    def __init__(self, nc, stack):
        self.nc = nc
        self.stack = stack
        self.nsem = 0
        self.pe = Eng("pe", nc.tensor)
        self.act = Eng("act", nc.scalar)
        self.dve = Eng("dve", nc.vector)
        self.pool = Eng("pool", nc.gpsimd)
        self.sp = Eng("sp", nc.sync)
        self.engs = [self.pe, self.act, self.dve, self.pool, self.sp]
        for e in self.engs:
            e.sem = self.new_sem(e.name)
        self.dma_rings = {"sp": [[self.new_sem("dmah%d" % i), 0] for i in range(32)],
                          "pool": [[self.new_sem("dmas%d" % i), 0] for i in range(24)]}
        self.dma_is = {"sp": 0, "pool": 0}
        self.n_inst = 0

    def new_sem(self, name):
        self.nsem += 1
        return self.stack.enter_context(self.nc.semaphore("s_%s_%d" % (name, self.nsem)))

    def _wait(self, eng, tok):
        if tok is None:
            return
        sem, val = tok
        if eng.seen.get(sem.num, 0) >= val:
            return
        if sem is eng.sem and not eng.same_sync:
            return
        eng.e.wait_ge(sem, val)
        self.n_inst += 1
        eng.seen[sem.num] = val

    def deps(self, eng, reads, writes):
        for t in reads:
            self._wait(eng, t.k.w)
            if getattr(t, "is_psum", False):
                for tok in t.k.r.values():
                    if tok[0] is not eng.sem:
                        self._wait(eng, tok)
        for t in writes:
            self._wait(eng, t.k.w)
            for tok in t.k.r.values():
                self._wait(eng, tok)

    def commit(self, tok, reads, writes):
        for t in reads:
            t.k.r[tok[0].num] = tok
        for t in writes:
            t.k.w = tok
            t.k.r = {}

    def op(self, eng, reads, writes, fn, signal=True):
        if signal and eng.cnt >= SEM_ROLL and not eng.pending:
            eng.sem = self.new_sem(eng.name)
            eng.cnt = 0
        self.deps(eng, reads, writes)
        ins = fn()
        self.n_inst += 1
        eng.pending = not signal
        if signal:
            eng.cnt += 1
            ins.then_inc(eng.sem, 1)
            tok = (eng.sem, eng.cnt)
            eng.last_tok = tok
        else:
            tok = (eng.sem, eng.cnt + 1)
        self.commit(tok, reads, writes)
        return ins

    def dma(self, q, reads, writes, fn):
        ring = self.dma_rings[q.name]
        slot = ring[self.dma_is[q.name]]
        self.dma_is[q.name] = (self.dma_is[q.name] + 1) % len(ring)
        sem = slot[0]
        if slot[1] > 0:
            self._wait(q, (sem, slot[1]))
        self.deps(q, reads, writes)
        ins = fn(q.e)
        self.n_inst += 1
        slot[1] += 16
        ins.then_inc(sem, 16)
        tok = (sem, slot[1])
        self.commit(tok, reads, writes)
        return tok

    def barrier(self):
        toks = [e.last_tok for e in self.engs if e.last_tok is not None]
        toks += [(s[0], s[1]) for ring in self.dma_rings.values() for s in ring if s[1] > 0]
        for e in self.engs:
            for tok in toks:
                if tok[0] is e.sem:
                    continue
                self._wait(e, tok)


class Ring:
    def __init__(self, tiles):
        self.tiles = tiles
        self.i = -1

    def next(self):
        self.i = (self.i + 1) % len(self.tiles)
        return self.tiles[self.i]


class KB:
    def __init__(self, nc, stack, n_layers, debug=()):
        self.nc = nc
        self.top = stack
        self.fw = FW(nc, stack)
        self.L = n_layers
        self.debug = set(debug)
        self.uid = 0
        self.ph = None

    def _name(self, p):
        self.uid += 1
        return "%s_%d" % (p, self.uid)

    def sb(self, shape, dt, name="sb", perm=False):
        st = self.top if perm else self.ph
        return Tile(st.enter_context(self.nc.sbuf_tensor(self._name(name), list(shape), dt)))

    def ps(self, shape, dt, name="ps"):
        return Tile(self.ph.enter_context(self.nc.psum_tensor(self._name(name), list(shape), dt)), is_psum=True)

    def dram(self, name, shape, dt, kind="Internal"):
        return Tile(self.nc.dram_tensor(name, list(shape), dt, kind=kind).ap())

    def begin_phase(self):
        self.ph = ExitStack()

    def end_phase(self):
        self.fw.barrier()
        self.ph.close()
        self.ph = None

    def mm(self, out, lhsT, rhs, R, W, start=True, stop=True, signal=None):
        if signal is None:
            signal = stop
        return self.fw.op(self.fw.pe, R, W, lambda: self.nc.tensor.matmul(out, lhsT=lhsT, rhs=rhs, start=start, stop=stop), signal=signal)

    def tr(self, out, in_, ident, R, W, signal=True):
        return self.fw.op(self.fw.pe, R, W, lambda: self.nc.tensor.transpose(out=out, in_=in_, identity=ident), signal=signal)

    def _e(self, eng):
        return {"act": self.fw.act, "dve": self.fw.dve, "pool": self.fw.pool}[eng]

    def _ne(self, eng):
        return {"act": self.nc.scalar, "dve": self.nc.vector, "pool": self.nc.gpsimd}[eng]

    def act(self, out, in_, func, R, W, **kw):
        return self.fw.op(self.fw.act, R, W, lambda: self.nc.scalar.activation(out=out, in_=in_, func=func, **kw))

    def tt(self, eng, out, in0, in1, op, R, W):
        return self.fw.op(self._e(eng), R, W, lambda: self._ne(eng).tensor_tensor(out=out, in0=in0, in1=in1, op=op))

    def ts(self, eng, out, in0, s1, s2, op0, op1, R, W):
        if s2 is None:
            return self.fw.op(self._e(eng), R, W, lambda: self._ne(eng).tensor_scalar(out=out, in0=in0, scalar1=s1, scalar2=None, op0=op0))
        return self.fw.op(self._e(eng), R, W, lambda: self._ne(eng).tensor_scalar(out=out, in0=in0, scalar1=s1, scalar2=s2, op0=op0, op1=op1))

    def cp(self, eng, out, in_, R, W):
        if eng == "act":
            return self.act(out, in_, AF.Copy, R, W)
        return self.fw.op(self._e(eng), R, W, lambda: self._ne(eng).tensor_copy(out=out, in_=in_))

    def memset(self, eng, out, val, W):
        return self.fw.op(self._e(eng), [], W, lambda: self._ne(eng).memset(out, val))

    def dma(self, q, out, in_, R, W):
        qe = {"sp": self.fw.sp, "pool": self.fw.pool}[q]
        return self.fw.dma(qe, R, W, lambda e: e.dma_start(out=out, in_=in_))

    def rsqrt_inplace(self, ap, tile, mul):
        self.act(ap, ap, AF.Ln, [tile, self.epsc], [tile], scale=mul, bias=self.epsc[:ap.shape[0], 0:1])
        self.act(ap, ap, AF.Exp, [tile], [tile], scale=-0.5)


class Sub:
    def __init__(self):
        self.k = Tk()


IN_BLOCKS = [
    (0, 512, "qk", dict(H=4, gain="na_q", rope=False, dst="QT", h0=0)),
    (512, 512, "qk", dict(H=4, gain="na_k", rope=False, dst="KT", h0=0)),
    (1024, 512, "v", dict(h0=0)),
    (1536, 512, "qk", dict(H=4, gain="gq_q", rope=True, dst="QT", h0=4)),
    (2048, 512, "qk", dict(H=4, gain="gq_q", rope=True, dst="QT", h0=8)),
    (2560, 512, "kvb", dict()),
    (3072, 512, "fm", dict(c0=0)),
    (3584, 512, "fm", dict(c0=4)),
    (4096, 512, "fm", dict(c0=8)),
    (4608, 512, "gate", dict()),
    (5120, 16, "bd", dict()),
]


def declare_io(kb):
    nc = kb.nc
    L = kb.L
    I = {}

    def inp(name, shape, dt=F32):
        I[name] = nc.dram_tensor(name, list(shape), dt, kind="ExternalInput").ap()

    inp("xin", [T, D])
    inp("cT", [128, 16, 2])
    inp("ident", [128, 128])
    inp("ropec", [128, NT, 64])
    inp("ropes", [128, NT, 64])
    inp("ada_w", [L, D, 6 * D])
    inp("ada_b", [L, 6 * D])
    inp("ada_b_fm", [L, 128, 96])
    inp("gmix_fm", [L, 128, 16])
    inp("gffn_fm", [L, 128, 16])
    inp("w_in", [L, D, DIN])
    inp("w_out", [L, D, D])
    inp("na_gain", [L, 2, 128])
    inp("gq_gain", [L, 2, 128])
    kb.I = I
    kb.xout = nc.dram_tensor("xout", [T, D], F32, kind="ExternalOutput").ap()
    kb.xs_k = [Sub() for _ in range(NT)]
    kb.QT = kb.dram("QT", [12, 128, T], BF16)
    kb.KT = kb.dram("KT", [6, 128, T], BF16)
    kb.V = kb.dram("V", [T, 6, 128], BF16)
    kb.CQ = kb.dram("CQ", [12, 128, T], F32)
    kb.SG = kb.dram("SG", [T, 512], BF16)
    kb.dbg = {}

    def dbg(name, shape, dt=F32):
        if name in kb.debug:
            kb.dbg[name] = nc.dram_tensor("dbg_" + name, list(shape), dt, kind="ExternalOutput").ap()

    dbg("modfm", [128, 2 * 4 * 16])
    dbg("gates", [4, 128, D])
    dbg("QT", [12, 128, T], BF16)
    dbg("KT", [6, 128, T], BF16)
    dbg("V", [T, 6, 128], BF16)
    dbg("CQ", [12, 128, T])
    dbg("SG", [T, 512], BF16)
    dbg("BD", [128, NT * 16])


def setup_consts(kb):
    I = kb.I
    kb.begin_phase()
    kb.epsc = kb.sb([128, 1], F32, "eps", perm=True)
    kb.memset("dve", kb.epsc[:], EPS, [kb.epsc])
    kb.identf = kb.sb([128, 128], F32, "identf", perm=True)
    kb.identb = kb.sb([128, 128], BF16, "identb", perm=True)
    kb.dma("sp", kb.identf[:], I["ident"], [], [kb.identf])
    kb.cp("dve", kb.identb[:], kb.identf[:], [kb.identf], [kb.identb])
    kb.modfm = kb.sb([128, 2, 4, 16], F32, "modfm", perm=True)
    kb.gs = kb.sb([128, 2, 2, 16], F32, "gs", perm=True)
    kb.GTS = kb.dram("GTS", [4, 128, D], F32)
    kb.AFFT = kb.dram("AFFT", [NE, T], F32)
    kb.BD = kb.sb([128, NT, 16], F32, "BD", perm=True)
    kb.end_phase()


def phase0(kb, l):
    I = kb.I
    kb.begin_phase()
    cT = kb.sb([128, 16, 2], F32)
    kb.dma("sp", cT[:], I["cT"], [], [cT])
    scT = kb.sb([128, 16, 2], F32)
    kb.act(scT[:], cT[:], AF.Silu, [cT], [scT])
    screp = kb.sb([128, 16, 2, 128], F32)
    for r in range(2):
        kb.cp("dve", screp[:, :, r, :], scT[:, :, r:r + 1].to_broadcast([128, 16, 128]), [scT], [screp])
    adabfm = kb.sb([128, 96], F32)
    kb.dma("sp", adabfm[:], I["ada_b_fm"][l], [], [adabfm])
    gfm = kb.sb([128, 2, 16], F32)
    kb.dma("sp", gfm[:, 0, :], I["gmix_fm"][l], [], [gfm])
    kb.dma("sp", gfm[:, 1, :], I["gffn_fm"][l], [], [gfm])
    kb.gtb = [[kb.sb([128, D], F32, "gtb") for _ in range(2)] for _ in range(2)]
    wring = Ring([kb.sb([128, 16, 512], F32) for _ in range(2)])
    bring = Ring([kb.sb([128, 512], F32) for _ in range(2)])
    pg = Ring([kb.ps([128, 512], F32) for _ in range(2)])
    pf = Ring([kb.ps([128, 512], F32) for _ in range(2)])

    def load(blk):
        w = wring.next()
        kb.dma("sp", w[:], I["ada_w"][l, :, blk * 512:(blk + 1) * 512].rearrange("(k p) c -> p k c", p=128), [], [w])
        return w

    import os
    CUT = os.environ.get("CUT", "")
    nblk = 0 if CUT == "pre" else 24
    wn = load(0)
    for blk in range(nblk):
        w = wn
        if blk + 1 < 24:
            wn = load(blk + 1)
        m, cb = blk // 4, blk % 4
        if (CUT == "fm" and m in (2, 5)) or (CUT == "gate" and m not in (2, 5)):
            continue
        if m in (2, 5):
            gi = 0 if m == 2 else 1
            bb = bring.next()
            kb.dma("sp", bb[:], I["ada_b"][l, blk * 512:(blk + 1) * 512].partition_broadcast(128), [], [bb])
            for r in range(2):
                p = pg.next()
                for k in range(16):
                    kb.mm(p[:], screp[:, k, r, :], w[:, k, :], [screp, w], [p], start=(k == 0), stop=(k == 15))
                kb.tt("dve", kb.gtb[r][gi][:, cb * 512:(cb + 1) * 512], p[:], bb[:], ALU.add, [p, bb], [kb.gtb[r][gi]])
        else:
            mi = {0: 0, 1: 1, 3: 2, 4: 3}[m]
            p = pf.next()
            pv = p[:, 0:8].rearrange("p (j r) -> p j r", r=2)
            for j in range(4):
                for k in range(16):
                    kb.mm(pv[:, j, :], w[:, k, j * 128:(j + 1) * 128], scT[:, k, :], [scT, w], [p], start=(k == 0), stop=(k == 15))
            c0 = m * 16 + cb * 4
            kb.tt("dve", kb.modfm[:, :, mi, cb * 4:cb * 4 + 4], pv.rearrange("p j r -> p r j"),
                  adabfm[:, c0:c0 + 4].unsqueeze(1).to_broadcast([128, 2, 4]), ALU.add, [p, adabfm], [kb.modfm])
    for r in range(2):
        for wi, mi in ((0, 1), (1, 3)):
            kb.ts("dve", kb.gs[:, r, wi, :], kb.modfm[:, r, mi, :], 1.0, None, ALU.add, None, [kb.modfm], [kb.gs])
            kb.tt("dve", kb.gs[:, r, wi, :], kb.gs[:, r, wi, :], gfm[:, wi, :], ALU.mult, [kb.gs, gfm], [kb.gs])
    for r in range(2):
        for gi in range(2):
            kb.dma("sp", kb.GTS[r * 2 + gi], kb.gtb[r][gi][:], [kb.gtb[r][gi]], [kb.GTS])
    if "modfm" in kb.dbg:
        kb.dma("sp", kb.dbg["modfm"], kb.modfm[:].rearrange("p a b c -> p (a b c)"), [kb.modfm], [])
    if "gates" in kb.dbg:
        for r in range(2):
            for gi in range(2):
                kb.dma("sp", kb.dbg["gates"][r * 2 + gi], kb.gtb[r][gi][:], [kb.gtb[r][gi]], [])
    kb.end_phase()


def phaseA(kb, l):
    I = kb.I
    nc = kb.nc
    kb.begin_phase()
    xsrc = I["xin"] if l == 0 else kb.xout
    kb.ropec = kb.sb([128, NT, 64], F32, "ropec")
    kb.ropes = kb.sb([128, NT, 64], F32, "ropes")
    kb.dma("sp", kb.ropec[:], I["ropec"], [], [kb.ropec])
    kb.dma("sp", kb.ropes[:], I["ropes"], [], [kb.ropes])
    gains = {}
    gq = kb.sb([128, 4, 128], F32)
    kb.dma("sp", gq[:, 0, :], I["na_gain"][l, 0].partition_broadcast(128), [], [gq])
    kb.dma("sp", gq[:, 1, :], I["na_gain"][l, 1].partition_broadcast(128), [], [gq])
    kb.dma("sp", gq[:, 2, :], I["gq_gain"][l, 0].partition_broadcast(128), [], [gq])
    kb.dma("sp", gq[:, 3, :], I["gq_gain"][l, 1].partition_broadcast(128), [], [gq])
    qs = 128.0 ** -0.5
    kb.ts("dve", gq[:, 0, :], gq[:, 0, :], qs, None, ALU.mult, None, [gq], [gq])
    kb.ts("dve", gq[:, 2, :], gq[:, 2, :], qs, None, ALU.mult, None, [gq], [gq])
    gidx = {"na_q": 0, "na_k": 1, "gq_q": 2, "gq_k": 3}

    xt_ring = Ring([kb.sb([128, D], F32) for _ in range(2)])
    xs_ring = Ring([kb.sb([128, D], BF16) for _ in range(2)])
    sqj = kb.sb([128, D], BF16)
    ssq_ring = Ring([kb.sb([128, 1], F32) for _ in range(2)])
    xnT_ring = Ring([kb.sb([128, 16, 512], BF16) for _ in range(2)])
    tmpf_ring = Ring([kb.sb([128, 8, 128], F32) for _ in range(2)])
    w_ring = Ring([kb.sb([128, 16, 512], BF16) for _ in range(2)])
    psT = Ring([kb.ps([128, 8, 128], BF16) for _ in range(2)])
    pP = Ring([kb.ps([128, 512], F32) for _ in range(2)])
    pQ = Ring([kb.ps([128, 8, 128], BF16) for _ in range(2)])
    sq4 = kb.sb([128, 512], F32)
    ssq4_ring = Ring([kb.sb([128, 4], F32) for _ in range(2)])
    xn4_ring = Ring([kb.sb([128, 512], F32) for _ in range(2)])
    xg4_ring = Ring([kb.sb([128, 512], F32) for _ in range(2)])
    xr4_ring = Ring([kb.sb([128, 512], BF16) for _ in range(2)])
    rt_ring = Ring([kb.sb([128, 4, 2, 32], F32) for _ in range(4)])
    qst_ring = Ring([kb.sb([128, 4, 512], BF16) for _ in range(3)])
    vst_ring = Ring([kb.sb([128, 512], BF16) for _ in range(3)])
    cst_ring = Ring([kb.sb([128, 512], F32) for _ in range(3)])

    flip = [0]

    def alt(a="act", b="dve"):
        flip[0] ^= 1
        return a if flip[0] else b

    def qk_post(P, pcols, H, gname, rope, t, stage, h_off, ti):
        Pv = P[:, pcols:pcols + H * 128]
        kb.act(sq4[:, 0:H * 128], Pv, AF.Square, [P], [sq4])
        s4 = ssq4_ring.next()
        kb.fw.op(kb.fw.dve, [sq4], [s4], lambda: nc.vector.tensor_reduce(
            out=s4[:, 0:H], in_=sq4[:, 0:H * 128].rearrange("p (h d) -> p h d", h=H), axis=AX.X, op=ALU.add))
        kb.rsqrt_inplace(s4[:, 0:H], s4, 1.0 / 128)
        xn = xn4_ring.next()
        kb.tt("dve", xn[:, 0:H * 128].rearrange("p (h d) -> p h d", h=H), Pv.rearrange("p (h d) -> p h d", h=H),
              s4[:, 0:H].unsqueeze(2).to_broadcast([128, H, 128]), ALU.mult, [P, s4], [xn])
        xr = xr4_ring.next()
        gv = gq[:, gidx[gname], :].unsqueeze(1).to_broadcast([128, H, 128])
        if not (rope and t >= 2):
            kb.tt("pool", xr[:, 0:H * 128].rearrange("p (h d) -> p h d", h=H),
                  xn[:, 0:H * 128].rearrange("p (h d) -> p h d", h=H), gv, ALU.mult, [xn, gq], [xr])
        else:
            xg = xg4_ring.next()
            kb.tt("pool", xg[:, 0:H * 128].rearrange("p (h d) -> p h d", h=H),
                  xn[:, 0:H * 128].rearrange("p (h d) -> p h d", h=H), gv, ALU.mult, [xn, gq], [xg])
            v5 = xg[:, 0:H * 128].rearrange("p (h a b f) -> p h a b f", h=H, a=2, b=2)
            o5 = xr[:, 0:H * 128].rearrange("p (h a b f) -> p h a b f", h=H, a=2, b=2)
            x1, x2 = v5[:, :, :, 0, :], v5[:, :, :, 1, :]
            cb = kb.ropec[:, t, :].rearrange("p (a f) -> p a f", a=2).unsqueeze(1).to_broadcast([128, H, 2, 32])
            sb_ = kb.ropes[:, t, :].rearrange("p (a f) -> p a f", a=2).unsqueeze(1).to_broadcast([128, H, 2, 32])
            t1, t2, t3, t4 = rt_ring.next(), rt_ring.next(), rt_ring.next(), rt_ring.next()
            kb.tt("dve", t1[:, 0:H], x1, cb, ALU.mult, [xg, kb.ropec], [t1])
            kb.tt("pool", t2[:, 0:H], x2, sb_, ALU.mult, [xg, kb.ropes], [t2])
            kb.tt("dve", o5[:, :, :, 0, :], t1[:, 0:H], t2[:, 0:H], ALU.subtract, [t1, t2], [xr])
            kb.tt("pool", t3[:, 0:H], x1, sb_, ALU.mult, [xg, kb.ropes], [t3])
            kb.tt("dve", t4[:, 0:H], x2, cb, ALU.mult, [xg, kb.ropec], [t4])
            kb.tt("pool", o5[:, :, :, 1, :], t3[:, 0:H], t4[:, 0:H], ALU.add, [t3, t4], [xr])
        pq = pQ.next()
        for h in range(H):
            kb.tr(pq[:, h, :], xr[:, h * 128:(h + 1) * 128], kb.identb[:], [xr, kb.identb], [pq], signal=(h == H - 1))
        kb.cp(alt(), stage[:, h_off:h_off + H, ti * 128:(ti + 1) * 128], pq[:, 0:H, :], [pq], [stage])

    def load_x(t):
        xt = xt_ring.next()
        kb.dma("sp", xt[:], xsrc[t * 128:(t + 1) * 128, :], [kb.xs_k[t]], [xt])
        return xt

    def load_w(bi):
        c0, n, kind, info = IN_BLOCKS[bi]
        w = w_ring.next()
        kb.dma("pool", w[:, :, 0:n], I["w_in"][l, :, c0:c0 + n].rearrange("(k p) c -> p k c", p=128), [], [w])
        return w

    ngroups = (NT + 3) // 4
    xt_next = load_x(0)
    for g in range(ngroups):
        tiles = list(range(4 * g, min(4 * g + 4, NT)))
        nt = len(tiles)
        ntok = nt * 128
        tok0 = tiles[0] * 128
        xnT = xnT_ring.next()
        for ti, t in enumerate(tiles):
            r = 1 if t < 2 else 0
            xt = xt_next
            if t + 1 < NT:
                xt_next = load_x(t + 1)
            ssq = ssq_ring.next()
            kb.act(sqj[:], xt[:], AF.Square, [xt], [sqj, ssq], accum_out=ssq[:, 0:1])
            kb.rsqrt_inplace(ssq[:, 0:1], ssq, 1.0 / D)
            xs = xs_ring.next()
            kb.ts("dve", xs[:], xt[:], ssq[:, 0:1], None, ALU.mult, None, [xt, ssq], [xs])
            for hf in range(2):
                p = psT.next()
                for k8 in range(8):
                    k = hf * 8 + k8
                    kb.tr(p[:, k8, :], xs[:, k * 128:(k + 1) * 128], kb.identb[:], [xs, kb.identb], [p], signal=(k8 == 7))
                tf = tmpf_ring.next()
                kb.tt("dve", tf[:], p[:], kb.gs[:, r, 0, hf * 8:hf * 8 + 8].unsqueeze(2).to_broadcast([128, 8, 128]), ALU.mult, [p, kb.gs], [tf])
                kb.tt("pool", xnT[:, hf * 8:hf * 8 + 8, ti * 128:(ti + 1) * 128], tf[:],
                      kb.modfm[:, r, 0, hf * 8:hf * 8 + 8].unsqueeze(2).to_broadcast([128, 8, 128]), ALU.add, [tf, kb.modfm], [xnT])
        import os
        CUTA = os.environ.get("CUTA", "")
        if CUTA == "A1":
            continue
        w_next = load_w(0)
        for bi, (c0, n, kind, info) in enumerate(IN_BLOCKS):
            w = w_next
            if bi + 1 < len(IN_BLOCKS):
                w_next = load_w(bi + 1)
            if CUTA and kind not in CUTA.split(","):
                continue
            if kind == "fm":
                for j in range(4):
                    P = pP.next()
                    for k in range(16):
                        kb.mm(P[:, 0:ntok], w[:, k, j * 128:(j + 1) * 128], xnT[:, k, 0:ntok], [w, xnT], [P], start=(k == 0), stop=(k == 15))
                    cst = cst_ring.next()
                    kb.cp(alt(), cst[:, 0:ntok], P[:, 0:ntok], [P], [cst])
                    kb.dma("sp", kb.CQ[info["c0"] + j, :, tok0:tok0 + ntok], cst[:, 0:ntok], [cst], [kb.CQ])
                continue
            stage = None
            if kind in ("qk", "kvb"):
                stage = qst_ring.next()
            for ti, t in enumerate(tiles):
                P = pP.next()
                for k in range(16):
                    kb.mm(P[:, 0:n], xnT[:, k, ti * 128:(ti + 1) * 128], w[:, k, 0:n], [w, xnT], [P], start=(k == 0), stop=(k == 15))
                if kind == "qk":
                    qk_post(P, 0, info["H"], info["gain"], info["rope"], t, stage, 0, ti)
                elif kind == "kvb":
                    KVB = os.environ.get("KVB", "ab")
                    if "a" in KVB:
                        qk_post(P, 0, 2, "gq_k", True, t, stage, 0, ti)
                    if "b" in KVB:
                        vst = vst_ring.next()
                        kb.cp("dve", vst[:, 0:256], P[:, 256:512], [P], [vst])
                        kb.dma("sp", kb.V[t * 128:(t + 1) * 128, 4:6, :], vst[:, 0:256].rearrange("p (h d) -> p h d", h=2), [vst], [kb.V])
                elif kind == "v":
                    vst = vst_ring.next()
                    kb.cp("act", vst[:], P[:], [P], [vst])
                    kb.dma("sp", kb.V[t * 128:(t + 1) * 128, 0:4, :], vst[:].rearrange("p (h d) -> p h d", h=4), [vst], [kb.V])
                elif kind == "gate":
                    vst = vst_ring.next()
                    kb.act(vst[:], P[:], AF.Silu, [P], [vst])
                    kb.dma("sp", kb.SG[t * 128:(t + 1) * 128, :], vst[:], [vst], [kb.SG])
                elif kind == "bd":
                    kb.cp("dve", kb.BD[:, t, :], P[:, 0:16], [P], [kb.BD])
            if kind == "qk":
                dst = kb.QT if info["dst"] == "QT" else kb.KT
                h0 = info["h0"]
                kb.dma("sp", dst[h0:h0 + 4, :, tok0:tok0 + ntok].rearrange("h d t -> d h t"), stage[:, 0:4, 0:ntok], [stage], [dst])
            elif kind == "kvb":
                kb.dma("sp", kb.KT[4:6, :, tok0:tok0 + ntok].rearrange("h d t -> d h t"), stage[:, 0:2, 0:ntok], [stage], [kb.KT])
    for nm, tl in (("QT", kb.QT), ("KT", kb.KT), ("V", kb.V), ("CQ", kb.CQ), ("SG", kb.SG)):
        if nm in kb.dbg:
            kb.dma("sp", kb.dbg[nm], tl[:], [tl], [])
    if "BD" in kb.dbg:
        kb.dma("sp", kb.dbg["BD"], kb.BD[:].rearrange("p t c -> p (t c)"), [kb.BD], [])
    kb.end_phase()


def host_consts():
    t = np.arange(NLAT)
    pos = np.stack([t // 64, t % 64], axis=-1).astype(np.float32)
    nf = 32
    freqs = (10000.0 ** (-np.arange(nf, dtype=np.float32) / nf)).astype(np.float32)
    ang = pos[:, :, None] * freqs
    cos = np.zeros((T, 64), np.float32)
    sin = np.zeros((T, 64), np.float32)
    cos[NCTX:] = np.cos(ang).reshape(NLAT, 64)
    sin[NCTX:] = np.sin(ang).reshape(NLAT, 64)
    cos[:NCTX] = 1.0
    out = {
        "ident": np.eye(128, dtype=np.float32),
        "ropec": np.ascontiguousarray(cos.reshape(NT, 128, 64).transpose(1, 0, 2)),
        "ropes": np.ascontiguousarray(sin.reshape(NT, 128, 64).transpose(1, 0, 2)),
    }
    ii = np.arange(128)
    trif = (ii[:, None] <= ii[None, :]).astype(np.float32)
    trib = (ii[:, None] >= ii[None, :]).astype(np.float32)
    out["tri"] = np.stack([trif, trib])
    out["negstrict"] = -(out["tri"] - np.eye(128, dtype=np.float32)[None])
    return out


def fm(v):
    s = v.shape
    return np.ascontiguousarray(np.swapaxes(v.reshape(s[:-1] + (s[-1] // 128, 128)), -1, -2))


def core_inputs(inp, b, layers):
    ls = list(layers)
    d = dict(host_consts())
    d["xin"] = np.ascontiguousarray(np.concatenate([inp["ctx"][b], inp["x"][b]], axis=0))
    cv = np.stack([inp["c"][b], inp["c_ctx"]], axis=0)
    d["cT"] = np.ascontiguousarray(cv.reshape(2, 16, 128).transpose(2, 1, 0))
    d["ada_w"] = inp["ada_w"][ls]
    d["ada_b"] = inp["ada_b"][ls]
    d["ada_b_fm"] = fm(inp["ada_b"][ls])
    d["gmix_fm"] = fm(inp["norm_mix"][ls])
    d["gffn_fm"] = fm(inp["norm_ffn"][ls])
    d["w_in"] = inp["w_in"][ls]
    d["w_out"] = inp["w_out"][ls]
    d["na_gain"] = inp["na_qk_gain"][ls]
    d["gq_gain"] = inp["gqa_qk_gain"][ls]
    d["nab"] = na_bias_tables(inp["na_rpb"][ls])
    cw = inp["dn_conv"][ls]
    d["convw_fm"] = np.ascontiguousarray(cw.reshape(len(ls), 5, 12, 128).transpose(0, 3, 2, 1))
    d["alog_bc"] = np.ascontiguousarray(np.broadcast_to(inp["dn_a_log"][ls].reshape(len(ls), 1, 8), (len(ls), 128, 8)))
    d["dtb_bc"] = np.ascontiguousarray(np.broadcast_to(inp["dn_dt_bias"][ls].reshape(len(ls), 1, 8), (len(ls), 128, 8)))
    d["ogain"] = inp["dn_out_gain"][ls]
    d["router_fm"] = np.ascontiguousarray(inp["router_w"][ls].reshape(len(ls), 16, 128, NE).transpose(0, 2, 1, 3))
    d["w_gate"] = inp["exp_w_gate"][ls]
    d["w_up"] = inp["exp_w_up"][ls]
    d["w_down"] = inp["exp_w_down"][ls]
    return d


def build_program(n_layers, debug=(), phases="0A"):
    nc = bass.Bass("TRN2", target_bir_lowering=False)
    with ExitStack() as st:
        kb = KB(nc, st, n_layers, debug)
        declare_io(kb)
        declare_io_B(kb)
        declare_io_C(kb)
        declare_io_DE(kb)
        setup_consts(kb)
        for l in range(n_layers):
            if "0" in phases:
                phase0(kb, l)
            if "A" in phases:
                phaseA(kb, l)
            if "B" in phases or "a" in phases or "b" in phases:
                phaseB(kb, l, do_a=("B" in phases or "a" in phases), do_b=("B" in phases or "b" in phases))
            if "C" in phases:
                phaseC(kb, l)
            if "D" in phases:
                phaseD(kb, l)
            if "E" in phases:
                phaseE(kb, l)
        kb.fw.barrier()
        print("n_inst", kb.fw.n_inst, "nsem", kb.fw.nsem)
    return nc


_NA_CACHE = {}


def na_patterns():
    if "p" in _NA_CACHE:
        return _NA_CACHE["p"]
    rows, W = 64, 64
    pats = {}
    plist = []
    pairs = {}
    for qt in range(32):
        qr = np.repeat(np.arange(2 * qt, 2 * qt + 2), W)
        qc = np.tile(np.arange(W), 2)
        rstart = np.clip(qr - 4, 0, rows - 8)
        cstart = np.clip(qc - 8, 0, W - 16)
        lst = []
        for kt in range(32):
            kr = np.repeat(np.arange(2 * kt, 2 * kt + 2), W)[:, None]
            kc = np.tile(np.arange(W), 2)[:, None]
            valid = (kr >= rstart[None]) & (kr < rstart[None] + 8) & (kc >= cstart[None]) & (kc < cstart[None] + 16)
            if not valid.any():
                continue
            dr = np.clip(kr - qr[None] + 7, 0, 14)
            dc = np.clip(kc - qc[None] + 15, 0, 30)
            dr = np.where(valid, dr, 0).astype(np.int16)
            dc = np.where(valid, dc, 0).astype(np.int16)
            key = (valid.tobytes(), dr.tobytes(), dc.tobytes())
            if key not in pats:
                pats[key] = len(plist)
                plist.append((valid, dr, dc))
            lst.append((kt, pats[key]))
        pairs[qt] = lst
    _NA_CACHE["p"] = (pairs, plist)
    return pairs, plist


def na_bias_tables(rpb):
    pairs, plist = na_patterns()
    L, H = rpb.shape[0], rpb.shape[1]
    out = np.empty((L, H, len(plist), 128, 128), np.float32)
    for pi, (valid, dr, dc) in enumerate(plist):
        g = rpb[:, :, dr, dc]
        out[:, :, pi] = np.where(valid[None, None], g, np.float32(-30000.0))
    return out


def declare_io_B(kb):
    nc = kb.nc
    pairs, plist = na_patterns()
    kb.NP = len(plist)
    kb.I["nab"] = nc.dram_tensor("nab", [kb.L, 4, kb.NP, 128, 128], F32, kind="ExternalInput").ap()
    kb.OT = kb.dram("OT", [16, 128, T], BF16)
    if "OT" in kb.debug:
        kb.dbg["OT"] = nc.dram_tensor("dbg_OT", [16, 128, T], BF16, kind="ExternalOutput").ap()


def phaseB(kb, l, do_a=True, do_b=True):
    I = kb.I
    nc = kb.nc
    pairs, plist = na_patterns()
    kb.begin_phase()
    kT_ring = Ring([kb.sb([128, T], BF16) for _ in range(2)])
    v1_ring = Ring([kb.sb([128, NT, 130], BF16) for _ in range(2)])
    for v1 in v1_ring.tiles:
        kb.memset("pool", v1[:, :, 128:130], 1.0, [v1])
    qT_ring = Ring([kb.sb([128, 512], BF16) for _ in range(3)])
    pT_ring = Ring([kb.sb([128, 512], BF16) for _ in range(3)])
    sb_ring = Ring([kb.sb([128, 128], F32) for _ in range(3)])
    pS = Ring([kb.ps([128, 512], F32) for _ in range(2)])
    pO = Ring([kb.ps([128, 512], F32) for _ in range(4)])
    pTr = Ring([kb.ps([128, 8, 128], BF16) for _ in range(1)])
    rinv_ring = Ring([kb.sb([128, 4], F32) for _ in range(2)])
    on_ring = Ring([kb.sb([128, 128], BF16) for _ in range(3)])
    ost_ring = Ring([kb.sb([128, 512], BF16) for _ in range(3)])
    nab = None
    if do_a:
        nab = kb.sb([128, kb.NP, 128], F32)

    def load_kv(kvh):
        kT = kT_ring.next()
        v1 = v1_ring.next()
        kb.dma("sp", kT[:], kb.KT[kvh], [kb.KT], [kT])
        kb.dma("sp", v1[:, :, 0:128], kb.V[:, kvh, :].rearrange("(t p) d -> p t d", p=128), [kb.V], [v1])
        return kT, v1

    def attn(qh, kT, v1, chunk, qtiles, keys):
        nq = len(qtiles)
        nqt = nq * 128
        q0 = qtiles[0] * 128
        qT = qT_ring.next()
        kb.dma("sp", qT[:, 0:nqt], kb.QT[qh, :, q0:q0 + nqt], [kb.QT], [qT])
        acc = [pO.next() for _ in range(nq)]
        nk = len(keys)
        for ki, (kt, bias) in enumerate(keys):
            S = pS.next()
            kb.mm(S[:, 0:nqt], kT[:, kt * 128:(kt + 1) * 128], qT[:, 0:nqt], [kT, qT], [S])
            pT = pT_ring.next()
            if bias is None:
                kb.act(pT[:, 0:nqt], S[:, 0:nqt], AF.Exp, [S], [pT])
            else:
                sbt = sb_ring.next()
                kb.tt("dve", sbt[:, 0:nqt], S[:, 0:nqt], bias, ALU.add, [S, nab], [sbt])
                kb.act(pT[:, 0:nqt], sbt[:, 0:nqt], AF.Exp, [sbt], [pT])
            for j in range(nq):
                a = acc[j]
                kb.mm(a[:, 0:129], pT[:, j * 128:(j + 1) * 128], v1[:, kt, 0:129], [pT, v1], [a],
                      start=(ki == 0), stop=(ki == nk - 1), signal=(ki == nk - 1))
        ost = ost_ring.next()
        ptr = pTr.next()
        rinv = rinv_ring.next()
        for j in range(nq):
            a = acc[j]
            off = 0
            kb.fw.op(kb.fw.dve, [a], [rinv], lambda: nc.vector.reciprocal(out=rinv[:, j:j + 1], in_=a[:, off + 128:off + 129]))
            on = on_ring.next()
            kb.act(on[:], a[:, off:off + 128], AF.Copy, [a, rinv], [on], scale=rinv[:, j:j + 1])
            kb.tr(ptr[:, j, :], on[:], kb.identb[:], [on, kb.identb], [ptr])
        kb.cp("dve", ost[:, 0:nqt], ptr[:, 0:nq, :].rearrange("p j q -> p (j q)"), [ptr], [ost])
        kb.dma("sp", kb.OT[chunk, :, q0:q0 + nqt], ost[:, 0:nqt], [ost], [kb.OT])

    if do_a:
        for h in range(4):
            kT, v1 = load_kv(h)
            kb.dma("sp", nab[:], I["nab"][l, h].rearrange("n k q -> k n q"), [], [nab])
            attn(h, kT, v1, h, [0, 1], [(0, None), (1, None)])
            for qt in range(32):
                keys = [(0, None), (1, None)] + [(kt + 2, nab[:, pid, :]) for kt, pid in pairs[qt]]
                attn(h, kT, v1, h, [qt + 2], keys)
    if do_b:
        for kvh in range(2):
            kT, v1 = load_kv(4 + kvh)
            for qi in range(4):
                qh = 4 + kvh * 4 + qi
                attn(qh, kT, v1, qh, [0, 1], [(0, None), (1, None)])
                for g in range(8):
                    attn(qh, kT, v1, qh, [2 + 4 * g + j for j in range(4)], [(kt, None) for kt in range(NT)])
    if "OT" in kb.dbg:
        kb.dma("sp", kb.dbg["OT"][0:12], kb.OT[0:12], [kb.OT], [])
    kb.end_phase()


def declare_io_DE(kb):
    nc = kb.nc
    L = kb.L
    kb.I["router_fm"] = nc.dram_tensor("router_fm", [L, 128, 16, NE], F32, kind="ExternalInput").ap()
    kb.I["w_gate"] = nc.dram_tensor("w_gate", [L, NE, D, DEXP], F32, kind="ExternalInput").ap()
    kb.I["w_up"] = nc.dram_tensor("w_up", [L, NE, D, DEXP], F32, kind="ExternalInput").ap()
    kb.I["w_down"] = nc.dram_tensor("w_down", [L, NE, DEXP, D], F32, kind="ExternalInput").ap()
    kb.H2 = kb.dram("H2", [T, D], BF16)
    kb.xs_c = [Sub() for _ in range(4)]
    for nm, shape, dt in (("xmid", [T, D], F32), ("H2", [T, D], BF16), ("affT", [NE, T], F32), ("idx", [128, 5 * NE], I32), ("gate", [128, 5 * NE], F32)):
        if nm in kb.debug:
            kb.dbg[nm] = nc.dram_tensor("dbg_" + nm, shape, dt, kind="ExternalOutput").ap()


def phaseD(kb, l):
    I = kb.I
    nc = kb.nc
    kb.begin_phase()
    xsrc = I["xin"] if l == 0 else kb.xout
    kb.affT = kb.sb([NE, T], F32, "affT")
    kb.gtb = [[None, None], [None, None]]
    for r in range(2):
        kb.gtb[r][0] = kb.sb([128, D], F32, "gtb")
        kb.dma("sp", kb.gtb[r][0][:], kb.GTS[r * 2 + 0], [kb.GTS], [kb.gtb[r][0]])
    wo = kb.sb([128, 16, D], BF16)
    for q in range(4):
        kb.dma("pool", wo[:, :, q * 512:(q + 1) * 512], I["w_out"][l, :, q * 512:(q + 1) * 512].rearrange("(k p) c -> p k c", p=128), [], [wo])
    rw = kb.sb([128, 16, NE], F32)
    kb.dma("sp", rw[:], I["router_fm"][l], [], [rw])
    ot_ring = Ring([kb.sb([128, 16, 512], BF16) for _ in range(2)])
    xt_ring = Ring([kb.sb([128, D], F32) for _ in range(2)])
    xn_ring = Ring([kb.sb([128, D], F32) for _ in range(1)])
    tmp_ring = Ring([kb.sb([128, 512], F32) for _ in range(3)])
    sqj = kb.sb([128, D], BF16)
    ssq_ring = Ring([kb.sb([128, 1], F32) for _ in range(2)])
    xs2_ring = Ring([kb.sb([128, D], F32) for _ in range(1)])
    xs2b_ring = Ring([kb.sb([128, D], BF16) for _ in range(1)])
    h2T_ring = Ring([kb.sb([128, 16, 128], F32) for _ in range(1)])
    tf_ring = Ring([kb.sb([128, 4, 128], F32) for _ in range(2)])
    sm_ring = Ring([kb.sb([128, 4], F32) for _ in range(2)])
    lg_ring = Ring([kb.sb([128, NE], F32) for _ in range(2)])
    pP = Ring([kb.ps([128, 512], F32) for _ in range(3)])
    pT = Ring([kb.ps([128, 4, 128], F32) for _ in range(2)])
    pL = Ring([kb.ps([128, 512], F32) for _ in range(2)])

    def load_ot(g):
        tiles = list(range(4 * g, min(4 * g + 4, NT)))
        ot = ot_ring.next()
        n = len(tiles) * 128
        kb.dma("sp", ot[:, :, 0:n], kb.OT[:, :, tiles[0] * 128:tiles[0] * 128 + n].rearrange("k d t -> d k t"), [kb.OT], [ot])
        return ot

    def load_x(t):
        xt = xt_ring.next()
        kb.dma("sp", xt[:], xsrc[t * 128:(t + 1) * 128, :], [kb.xs_k[t]], [xt])
        return xt

    ngroups = (NT + 3) // 4
    ot_next = load_ot(0)
    xt_next = load_x(0)
    for g in range(ngroups):
        tiles = list(range(4 * g, min(4 * g + 4, NT)))
        ot = ot_next
        if g + 1 < ngroups:
            ot_next = load_ot(g + 1)
        for ti, t in enumerate(tiles):
            r = 1 if t < 2 else 0
            xt = xt_next
            if t + 1 < NT:
                xt_next = load_x(t + 1)
            xn = xn_ring.next()
            for cb in range(4):
                P = pP.next()
                for k in range(16):
                    kb.mm(P[:], ot[:, k, ti * 128:(ti + 1) * 128], wo[:, k, cb * 512:(cb + 1) * 512], [ot, wo], [P], start=(k == 0), stop=(k == 15))
                tmp = tmp_ring.next()
                kb.tt("dve", tmp[:], P[:], kb.gtb[r][0][:, cb * 512:(cb + 1) * 512], ALU.mult, [P, kb.gtb[r][0]], [tmp])
                kb.tt("pool", xn[:, cb * 512:(cb + 1) * 512], tmp[:], xt[:, cb * 512:(cb + 1) * 512], ALU.add, [tmp, xt], [xn])
            kb.dma("sp", kb.xout[t * 128:(t + 1) * 128, :], xn[:], [xn], [kb.xs_k[t]])
            ssq = ssq_ring.next()
            kb.act(sqj[:], xn[:], AF.Square, [xn], [sqj, ssq], accum_out=ssq[:, 0:1])
            kb.rsqrt_inplace(ssq[:, 0:1], ssq, 1.0 / D)
            xs2 = xs2_ring.next()
            kb.ts("dve", xs2[:], xn[:], ssq[:, 0:1], None, ALU.mult, None, [xn, ssq], [xs2])
            xs2b = xs2b_ring.next()
            kb.cp("pool", xs2b[:], xs2[:], [xs2], [xs2b])
            kb.dma("sp", kb.H2[t * 128:(t + 1) * 128, :], xs2b[:], [xs2b], [kb.H2])
            h2T = h2T_ring.next()
            for q in range(4):
                p = pT.next()
                for j in range(4):
                    k = q * 4 + j
                    kb.tr(p[:, j, :], xs2[:, k * 128:(k + 1) * 128], kb.identf[:], [xs2, kb.identf], [p], signal=(j == 3))
                tf = tf_ring.next()
                kb.tt("dve", tf[:], p[:], kb.gs[:, r, 1, q * 4:q * 4 + 4].unsqueeze(2).to_broadcast([128, 4, 128]), ALU.mult, [p, kb.gs], [tf])
                kb.tt("pool", h2T[:, q * 4:q * 4 + 4, :], tf[:], kb.modfm[:, r, 2, q * 4:q * 4 + 4].unsqueeze(2).to_broadcast([128, 4, 128]), ALU.add, [tf, kb.modfm], [h2T])
            PL = pL.next()
            for k in range(16):
                kb.mm(PL[:, 0:NE], h2T[:, k, :], rw[:, k, :], [h2T, rw], [PL], start=(k == 0), stop=(k == 15))
            sm = sm_ring.next()
            kb.fw.op(kb.fw.dve, [PL], [sm], lambda: nc.vector.tensor_reduce(out=sm[:, 0:1], in_=PL[:, 0:NE], axis=AX.X, op=ALU.max))
            kb.ts("dve", sm[:, 1:2], sm[:, 0:1], -1.0, None, ALU.mult, None, [sm], [sm])
            lg = lg_ring.next()
            kb.act(lg[:], PL[:, 0:NE], AF.Exp, [PL, sm], [lg, sm], bias=sm[:, 1:2], accum_out=sm[:, 2:3])
            kb.fw.op(kb.fw.dve, [sm], [sm], lambda: nc.vector.reciprocal(out=sm[:, 3:4], in_=sm[:, 2:3]))
            kb.ts("dve", lg[:], lg[:], sm[:, 3:4], None, ALU.mult, None, [lg, sm], [lg])
            kb.tr(PL[0:NE, 128:256], lg[:], kb.identf[:], [lg, kb.identf], [PL])
            kb.cp("act", kb.affT[:, t * 128:(t + 1) * 128], PL[0:NE, 128:256], [PL], [kb.affT])
    kb.dma("sp", kb.AFFT[:], kb.affT[:], [kb.affT], [kb.AFFT])
    if "xmid" in kb.dbg:
        kb.dma("sp", kb.dbg["xmid"], kb.xout, [], kb.xs_k)
    if "H2" in kb.dbg:
        kb.dma("sp", kb.dbg["H2"], kb.H2[:], [kb.H2], [])
    if "affT" in kb.dbg:
        kb.dma("sp", kb.dbg["affT"], kb.affT[:], [kb.affT], [])
    kb.end_phase()


def phaseE(kb, l):
    I = kb.I
    nc = kb.nc
    kb.begin_phase()
    NS = 544
    kb.affT = kb.sb([NE, T], F32, "affT")
    kb.dma("sp", kb.affT[:], kb.AFFT[:], [kb.AFFT], [kb.affT])
    kb.gtb = [[None, None], [None, None]]
    for r in range(2):
        kb.gtb[r][1] = kb.sb([128, D], F32, "gtb")
        kb.dma("sp", kb.gtb[r][1][:], kb.GTS[r * 2 + 1], [kb.GTS], [kb.gtb[r][1]])
    work = kb.sb([NE, NLAT], F32)
    workc = kb.sb([NE, NCTX], F32)
    vals = kb.sb([NE, NS], F32)
    idxu = kb.sb([NE, NS], U32)
    idxf = kb.sb([NE, NS], F32)
    kb.cp("dve", work[:], kb.affT[:, NCTX:T], [kb.affT], [work])
    kb.cp("pool", workc[:], kb.affT[:, 0:NCTX], [kb.affT], [workc])
    for (wk, base, nround) in ((work, 0, 64), (workc, 512, 4)):
        for i in range(nround):
            c0 = base + i * 8
            kb.fw.op(kb.fw.dve, [wk], [vals], lambda: nc.vector.max(out=vals[:, c0:c0 + 8], in_=wk[:]))
            kb.fw.op(kb.fw.dve, [wk, vals], [idxu], lambda: nc.vector.max_index(out=idxu[:, c0:c0 + 8], in_max=vals[:, c0:c0 + 8], in_values=wk[:]))
            if i + 1 < nround:
                kb.fw.op(kb.fw.dve, [vals, wk], [wk], lambda: nc.vector.match_replace(out=wk[:], in_to_replace=vals[:, c0:c0 + 8], in_values=wk[:], imm_value=-1.0))
    kb.cp("dve", idxf[:], idxu[:], [idxu], [idxf])
    kb.ts("dve", idxf[:, 0:512], idxf[:, 0:512], float(NCTX), None, ALU.add, None, [idxf], [idxf])
    idxT = kb.sb([128, 5, NE], I32)
    gateT = kb.sb([128, 5, NE], F32)
    kb.memset("dve", idxT[:], 0, [idxT])
    kb.memset("dve", gateT[:], 0.0, [gateT])
    pX = kb.ps([128, 512], F32)
    for s in range(5):
        n = 128 if s < 4 else 32
        kb.tr(pX[0:n, 0:NE], idxf[:, s * 128:s * 128 + n], kb.identf[0:NE, 0:NE], [idxf, kb.identf], [pX])
        kb.cp("dve", idxT[0:n, s, :], pX[0:n, 0:NE], [pX], [idxT])
        kb.tr(pX[0:n, 64:64 + NE], vals[:, s * 128:s * 128 + n], kb.identf[0:NE, 0:NE], [vals, kb.identf], [pX])
        kb.cp("dve", gateT[0:n, s, :], pX[0:n, 64:64 + NE], [pX], [gateT])
    if "idx" in kb.dbg:
        kb.dma("sp", kb.dbg["idx"], idxT[:].rearrange("p s e -> p (s e)"), [idxT], [])
        kb.dma("sp", kb.dbg["gate"], gateT[:].rearrange("p s e -> p (s e)"), [gateT], [])
    xe_ring = Ring([kb.sb([128, D], BF16) for _ in range(3)])
    xeT = kb.sb([128, 16, NS], BF16)
    tf_ring = Ring([kb.sb([128, 8, 128], F32) for _ in range(2)])
    wg_ring = Ring([kb.sb([128, 16, 256], BF16) for _ in range(2)])
    wu_ring = Ring([kb.sb([128, 16, 256], BF16) for _ in range(2)])
    wd_ring = Ring([kb.sb([128, 8, 512], BF16) for _ in range(2)])
    hidT = kb.sb([128, 8, NS], BF16)
    sg_ring = Ring([kb.sb([128, NS], F32) for _ in range(2)])
    yes = [kb.sb([128, D], F32) for _ in range(5)]
    pTr = Ring([kb.ps([128, 8, 128], BF16) for _ in range(2)])
    pG = Ring([kb.ps([128, 512], F32) for _ in range(1)])
    pGc = Ring([kb.ps([128, 512], F32) for _ in range(1)])
    pU = Ring([kb.ps([128, 512], F32) for _ in range(1)])
    pY = Ring([kb.ps([128, 512], F32) for _ in range(2)])
    for e in range(NE):
        for s in range(5):
            n = 128 if s < 4 else 32
            r = 0 if s < 4 else 1
            xe = xe_ring.next()
            kb.fw.dma(kb.fw.pool, [idxT, kb.H2], [xe], lambda q: q.indirect_dma_start(
                out=xe[0:n, :], out_offset=None, in_=kb.H2[:], in_offset=bass.IndirectOffsetOnAxis(ap=idxT[0:n, s, e:e + 1], axis=0)))
            for hf in range(2):
                p = pTr.next()
                for k8 in range(8):
                    k = hf * 8 + k8
                    kb.tr(p[:, k8, 0:n], xe[0:n, k * 128:(k + 1) * 128], kb.identb[0:n, 0:n], [xe, kb.identb], [p], signal=(k8 == 7))
                tf = tf_ring.next()
                kb.tt("dve", tf[:, :, 0:n], p[:, :, 0:n], kb.gs[:, r, 1, hf * 8:hf * 8 + 8].unsqueeze(2).to_broadcast([128, 8, n]), ALU.mult, [p, kb.gs], [tf])
                kb.tt("pool", xeT[:, hf * 8:hf * 8 + 8, s * 128:s * 128 + n], tf[:, :, 0:n],
                      kb.modfm[:, r, 2, hf * 8:hf * 8 + 8].unsqueeze(2).to_broadcast([128, 8, n]), ALU.add, [tf, kb.modfm], [xeT])
        for cq in range(4):
            wg = wg_ring.next()
            wu = wu_ring.next()
            kb.dma("pool", wg[:], I["w_gate"][l, e, :, cq * 256:(cq + 1) * 256].rearrange("(k p) c -> p k c", p=128), [], [wg])
            kb.dma("pool", wu[:], I["w_up"][l, e, :, cq * 256:(cq + 1) * 256].rearrange("(k p) c -> p k c", p=128), [], [wu])
            for cj in range(2):
                c = cq * 2 + cj
                G, U, Gc = pG.next(), pU.next(), pGc.next()
                for k in range(16):
                    kb.mm(G[:], wg[:, k, cj * 128:(cj + 1) * 128], xeT[:, k, 0:512], [wg, xeT], [G], start=(k == 0), stop=(k == 15))
                for k in range(16):
                    kb.mm(Gc[:, 0:32], wg[:, k, cj * 128:(cj + 1) * 128], xeT[:, k, 512:544], [wg, xeT], [Gc], start=(k == 0), stop=(k == 15))
                for k in range(16):
                    kb.mm(U[:], wu[:, k, cj * 128:(cj + 1) * 128], xeT[:, k, 0:512], [wu, xeT], [U], start=(k == 0), stop=(k == 15))
                for k in range(16):
                    kb.mm(Gc[:, 32:64], wu[:, k, cj * 128:(cj + 1) * 128], xeT[:, k, 512:544], [wu, xeT], [Gc], start=(k == 0), stop=(k == 15))
                sg = sg_ring.next()
                kb.act(sg[:, 0:512], G[:], AF.Silu, [G], [sg])
                kb.act(sg[:, 512:544], Gc[:, 0:32], AF.Silu, [Gc], [sg])
                kb.tt("dve", hidT[:, c, 0:512], sg[:, 0:512], U[:], ALU.mult, [sg, U], [hidT])
                kb.tt("dve", hidT[:, c, 512:544], sg[:, 512:544], Gc[:, 32:64], ALU.mult, [sg, Gc], [hidT])
        for cb in range(4):
            wd = wd_ring.next()
            kb.dma("pool", wd[:], I["w_down"][l, e, :, cb * 512:(cb + 1) * 512].rearrange("(c p) n -> p c n", p=128), [], [wd])
            for s in range(5):
                n = 128 if s < 4 else 32
                r = 0 if s < 4 else 1
                Y = pY.next()
                for c in range(8):
                    kb.mm(Y[0:n, :], hidT[:, c, s * 128:s * 128 + n], wd[:, c, :], [hidT, wd], [Y], start=(c == 0), stop=(c == 7))
                ye = yes[s]
                kb.fw.op(kb.fw.dve, [Y, gateT, kb.gtb[r][1]], [ye], lambda: nc.vector.scalar_tensor_tensor(
                    out=ye[0:n, cb * 512:(cb + 1) * 512], in0=Y[0:n, :], scalar=gateT[0:n, s, e:e + 1], in1=kb.gtb[r][1][0:n, cb * 512:(cb + 1) * 512],
                    op0=ALU.mult, op1=ALU.mult))
        for s in range(5):
            n = 128 if s < 4 else 32
            ye = yes[s]
            kb.fw.dma(kb.fw.pool, [ye, idxT], [kb.xs_c[0]], lambda q: q.indirect_dma_start(
                out=kb.xout, out_offset=bass.IndirectOffsetOnAxis(ap=idxT[0:n, s, e:e + 1], axis=0),
                in_=ye[0:n, :], in_offset=None, compute_op=ALU.add))
    kb.end_phase()


def declare_io_C(kb):
    nc = kb.nc
    L = kb.L
    for nm, shape in (("convw_fm", [L, 128, 12, 5]), ("alog_bc", [L, 128, 8]), ("dtb_bc", [L, 128, 8]),
                      ("ogain", [L, 128]), ("tri", [2, 128, 128]), ("negstrict", [2, 128, 128])):
        kb.I[nm] = nc.dram_tensor(nm, shape, F32, kind="ExternalInput").ap()
    if "OTC" in kb.debug:
        kb.dbg["OTC"] = nc.dram_tensor("dbg_OTC", [4, 128, T], BF16, kind="ExternalOutput").ap()
    if "odn" in kb.debug:
        kb.dbg["odn"] = nc.dram_tensor("dbg_odn", [4, 128, NT * 128], F32, kind="ExternalOutput").ap()


def phaseC(kb, l):
    I = kb.I
    nc = kb.nc
    kb.begin_phase()
    SEGS = ((0, NCTX), (NCTX, T))
    tri = kb.sb([128, 2, 128], F32)
    nst = kb.sb([128, 2, 128], F32)
    for d in range(2):
        kb.dma("sp", tri[:, d, :], I["tri"][d], [], [tri])
        kb.dma("sp", nst[:, d, :], I["negstrict"][d], [], [nst])
    ones = kb.sb([128, 128], F32)
    kb.memset("dve", ones[:], 1.0, [ones])
    cw = kb.sb([128, 12, 5], F32)
    kb.dma("sp", cw[:], I["convw_fm"][l], [], [cw])
    ab = kb.sb([128, 2, 8], F32)
    kb.dma("sp", ab[:, 0, :], I["alog_bc"][l], [], [ab])
    kb.dma("sp", ab[:, 1, :], I["dtb_bc"][l], [], [ab])
    og = kb.sb([128, 128], F32)
    kb.dma("sp", og[:], I["ogain"][l].partition_broadcast(128), [], [og])
    beta = kb.sb([128, NT, 8], F32)
    gg = kb.sb([128, NT, 8], F32)
    tmp8 = kb.sb([128, NT, 8], F32)
    kb.act(beta[:], kb.BD[:, :, 0:8], AF.Sigmoid, [kb.BD], [beta])
    kb.tt("dve", tmp8[:], kb.BD[:, :, 8:16], ab[:, 1, :].unsqueeze(1).to_broadcast([128, NT, 8]), ALU.add, [kb.BD, ab], [tmp8])
    kb.act(tmp8[:], tmp8[:], AF.Exp, [tmp8], [tmp8])
    kb.act(tmp8[:], tmp8[:], AF.Ln, [tmp8], [tmp8], bias=1.0)
    kb.act(ab[:, 0, :], ab[:, 0, :], AF.Exp, [ab], [ab])
    kb.tt("dve", gg[:], tmp8[:], ab[:, 0, :].unsqueeze(1).to_broadcast([128, NT, 8]), ALU.mult, [tmp8, ab], [gg])
    kb.ts("dve", gg[:], gg[:], -1.0, None, ALU.mult, None, [gg], [gg])

    qT = kb.sb([128, T], F32)
    kT = kb.sb([128, T], F32)
    ktm = kb.sb([128, NT, 128], F32)
    vtm = kb.sb([128, NT, 128], F32)
    otot = kb.sb([128, NT, 128], F32)
    sgt = kb.sb([128, NT, 128], BF16)
    xc_ring = Ring([kb.sb([128, T], F32) for _ in range(1)])
    yc = kb.sb([128, T], F32)
    sc_ring = Ring([kb.sb([128, 512], F32) for _ in range(2)])
    sm = {nm: kb.sb([128, 2, NT], F32) for nm in ("Gcol", "glast", "expG", "bg", "etail", "eglast")}
    S = [kb.sb([128, 128], F32) for _ in range(2)]
    pPre = Ring([kb.ps([128, 4, 128], F32) for _ in range(2)])
    pSol = [kb.ps([128, 4, 128], F32) for _ in range(4)]
    pRec = Ring([kb.ps([128, 4, 128], F32) for _ in range(2)])
    W = {}

    def wt(name, n, shape=(128, 128), dt=F32):
        W[name] = Ring([kb.sb(list(shape), dt) for _ in range(n)])

    for nm in ("rhsA", "rhsB", "E", "DT", "DTb", "M", "Rv", "Rk", "kt", "vnew", "tmpo", "on", "onb"):
        wt(nm, 2)
    for nm in ("Q", "Qt", "Tt", "QKD", "wT", "u"):
        wt(nm, 4)
    wt("ssq", 4, (128, 1))
    ostage = Ring([kb.sb([128, 512], BF16) for _ in range(2)])
    pTrb = None

    for h in range(4):
        for which, c in (("q", h), ("k", 4 + h), ("v", 8 + h)):
            xc = xc_ring.next()
            kb.dma("sp", xc[:], kb.CQ[c], [kb.CQ], [xc])
            kb.ts("dve", yc[:], xc[:], cw[:, c, 2:3], None, ALU.mult, None, [xc, cw], [yc])
            for j in (0, 1, 3, 4):
                s = j - 2
                for (lo, hi) in SEGS:
                    a, b = max(lo, lo - s), min(hi, hi - s)
                    eng = "dve"
                    kb.fw.op(kb._e(eng), [xc, cw, yc], [yc], lambda: kb._ne(eng).scalar_tensor_tensor(
                        out=yc[:, a:b], in0=xc[:, a + s:b + s], scalar=cw[:, c, j:j + 1], in1=yc[:, a:b], op0=ALU.mult, op1=ALU.add))
            dst = {"q": qT, "k": kT, "v": xc}[which]
            if which == "v":
                kb.act(xc[:], yc[:], AF.Silu, [yc], [xc])
                for t in range(NT):
                    p = pPre.next()
                    kb.tr(p[:, 0, :], xc[:, t * 128:(t + 1) * 128], kb.identf[:], [xc, kb.identf], [p])
                    kb.cp("act" if t % 2 else "dve", vtm[:, t, :], p[:, 0, :], [p], [vtm])
                continue
            kb.act(yc[:], yc[:], AF.Silu, [yc], [yc])
            for blk in range((T + 511) // 512):
                a, b = blk * 512, min(T, blk * 512 + 512)
                n = b - a
                sq = sc_ring.next()
                kb.tt("pool", sq[:, 0:n], yc[:, a:b], yc[:, a:b], ALU.mult, [yc], [sq])
                p = pPre.next()
                pv = p[:].rearrange("p a b -> p (a b)")
                kb.mm(pv[:, 0:n], ones[:], sq[:, 0:n], [ones, sq], [p])
                kb.act(sq[:, 0:n], pv[:, 0:n], AF.Ln, [p, kb.epsc], [sq], bias=kb.epsc[:, 0:1])
                kb.act(sq[:, 0:n], sq[:, 0:n], AF.Exp, [sq], [sq], scale=-0.5)
                if which == "q":
                    kb.fw.op(kb.fw.dve, [yc, sq], [dst], lambda: nc.vector.scalar_tensor_tensor(
                        out=dst[:, a:b], in0=yc[:, a:b], scalar=128.0 ** -0.5, in1=sq[:, 0:n], op0=ALU.mult, op1=ALU.mult))
                else:
                    kb.tt("dve", dst[:, a:b], yc[:, a:b], sq[:, 0:n], ALU.mult, [yc, sq], [dst])
            if which == "k":
                for t in range(NT):
                    p = pPre.next()
                    kb.tr(p[:, 0, :], kT[:, t * 128:(t + 1) * 128], kb.identf[:], [kT, kb.identf], [p])
                    kb.cp("act" if t % 2 else "dve", ktm[:, t, :], p[:, 0, :], [p], [ktm])
        kb.dma("sp", sgt[:], kb.SG[:, h * 128:(h + 1) * 128].rearrange("(t p) e -> p t e", p=128), [kb.SG], [sgt])
        import os
        CUTC = os.environ.get("CUTC", "")
        if CUTC == "1":
            break
        for d in range(2):
            dh = d * 4 + h
            p = pPre.next()
            kb.mm(p[:, 0, 0:NT], tri[:, d, :], gg[:, :, dh], [tri, gg], [p])
            kb.mm(p[:, 1, 0:NT], ones[:], gg[:, :, dh], [ones, gg], [p])
            kb.cp("dve", sm["Gcol"][:, d, :], p[:, 0, 0:NT], [p], [sm["Gcol"]])
            kb.cp("dve", sm["glast"][:, d, :], p[:, 1, 0:NT], [p], [sm["glast"]])
            kb.act(sm["expG"][:, d, :], p[:, 0, 0:NT], AF.Exp, [p], [sm["expG"]])
            kb.act(sm["eglast"][:, d, :], p[:, 1, 0:NT], AF.Exp, [p], [sm["eglast"]])
            kb.tt("dve", sm["bg"][:, d, :], sm["expG"][:, d, :], beta[:, :, dh], ALU.mult, [sm["expG"], beta], [sm["bg"]])
            kb.tt("dve", sm["etail"][:, d, :], sm["glast"][:, d, :], sm["Gcol"][:, d, :], ALU.subtract, [sm["glast"], sm["Gcol"]], [sm["etail"]])
            kb.act(sm["etail"][:, d, :], sm["etail"][:, d, :], AF.Exp, [sm["etail"]], [sm["etail"]])
            kb.memset("dve", S[d][:], 0.0, [S[d]])
        kb.memset("pool", otot[:], 0.0, [otot])
        order = [list(range(NT)), [1, 0] + list(range(NT - 1, 1, -1))]
        if CUTC == "2":
            break
        for step in range(NT if not CUTC.startswith("3") else int(CUTC[1:])):
            jobs = [(d, order[d][step]) for d in range(2)]
            st = {}
            for ji, (d, c) in enumerate(jobs):
                dh = d * 4 + h
                cs = slice(c * 128, (c + 1) * 128)
                rhsA, rhsB = W["rhsA"].next(), W["rhsB"].next()
                kb.ts("pool", rhsA[:], tri[:, d, :], gg[:, c, dh:dh + 1], None, ALU.mult, None, [tri, gg], [rhsA])
                kb.ts("pool", rhsB[:], kb.identf[:], beta[:, c, dh:dh + 1], None, ALU.mult, None, [kb.identf, beta], [rhsB])
                p = pPre.next()
                kb.mm(p[:, 0, :], ones[:], rhsA[:], [ones, rhsA], [p])
                kb.mm(p[:, 1, :], ones[:], rhsB[:], [ones, rhsB], [p])
                kb.mm(p[:, 2, :], kT[:, cs], kT[:, cs], [kT], [p])
                kb.mm(p[:, 3, :], kT[:, cs], qT[:, cs], [kT, qT], [p])
                E = W["E"].next()
                kb.ts("dve", E[:], p[:, 0, :], sm["Gcol"][:, d, c:c + 1], 0.0, ALU.subtract, ALU.min, [p, sm["Gcol"]], [E])
                kb.act(E[:], E[:], AF.Exp, [E], [E])
                DT = W["DT"].next()
                kb.tt("pool", DT[:], E[:], tri[:, d, :], ALU.mult, [E, tri], [DT])
                DTb = W["DTb"].next()
                kb.tt("dve", DTb[:], p[:, 1, :], DT[:], ALU.mult, [p, DT], [DTb])
                M = W["M"].next()
                kb.tt("dve", M[:], p[:, 2, :], DTb[:], ALU.mult, [p, DTb], [M])
                Qt = W["Qt"].next()
                kb.tt("pool", Qt[:], M[:], nst[:, d, :], ALU.mult, [M, nst], [Qt])
                QKD = W["QKD"].next()
                kb.tt("dve", QKD[:], p[:, 3, :], DT[:], ALU.mult, [p, DT], [QKD])
                ps = pSol[ji]
                kb.tr(ps[:, 0, :], Qt[:], kb.identf[:], [Qt, kb.identf], [ps])
                Q = W["Q"].next()
                kb.cp("act", Q[:], ps[:, 0, :], [ps], [Q])
                Tt = W["Tt"].next()
                kb.tt("pool", Tt[:], Qt[:], kb.identf[:], ALU.add, [Qt, kb.identf], [Tt])
                st[ji] = dict(Q=Q, Qt=Qt, Tt=Tt, QKD=QKD, ps=ps)
            CUTS = os.environ.get("CUTS", "")
            if CUTS == "a":
                continue
            for k in range(1, 7):
                for ji in range(2):
                    s_ = st[ji]
                    ps = s_["ps"]
                    kb.mm(ps[:, 0, :], s_["Qt"][:], s_["Q"][:], [s_["Qt"], s_["Q"]], [ps])
                    if k < 6:
                        kb.mm(ps[:, 1, :], s_["Q"][:], s_["Qt"][:], [s_["Qt"], s_["Q"]], [ps])
                    Qn = W["Q"].next()
                    kb.cp("act", Qn[:], ps[:, 0, :], [ps], [Qn])
                    if k < 6:
                        Qtn = W["Qt"].next()
                        kb.cp("dve", Qtn[:], ps[:, 1, :], [ps], [Qtn])
                        s_["Qt"] = Qtn
                    s_["Q"] = Qn
                    kb.mm(ps[:, 2, :], Qn[:], s_["Tt"][:], [Qn, s_["Tt"]], [ps])
                    Tn = W["Tt"].next()
                    kb.tt("dve", Tn[:], ps[:, 2, :], s_["Tt"][:], ALU.add, [ps, s_["Tt"]], [Tn])
                    s_["Tt"] = Tn
            if CUTS == "b":
                continue
            for ji, (d, c) in enumerate(jobs):
                dh = d * 4 + h
                cs = slice(c * 128, (c + 1) * 128)
                s_ = st[ji]
                ps = s_["ps"]
                Rv, Rk, kt_ = W["Rv"].next(), W["Rk"].next(), W["kt"].next()
                kb.ts("pool", Rv[:], vtm[:, c, :], beta[:, c, dh:dh + 1], None, ALU.mult, None, [vtm, beta], [Rv])
                kb.ts("pool", Rk[:], ktm[:, c, :], sm["bg"][:, d, c:c + 1], None, ALU.mult, None, [ktm, sm["bg"]], [Rk])
                kb.ts("pool", kt_[:], ktm[:, c, :], sm["etail"][:, d, c:c + 1], None, ALU.mult, None, [ktm, sm["etail"]], [kt_])
                kb.mm(ps[:, 0, :], s_["Tt"][:], Rv[:], [s_["Tt"], Rv], [ps])
                kb.mm(ps[:, 1, :], Rk[:], s_["Tt"][:], [s_["Tt"], Rk], [ps])
                u, wT = W["u"].next(), W["wT"].next()
                kb.cp("act", u[:], ps[:, 0, :], [ps], [u])
                kb.cp("dve", wT[:], ps[:, 1, :], [ps], [wT])
                pr = pRec.next()
                kb.mm(pr[:, 0, :], wT[:], S[d][:], [wT, S[d]], [pr])
                kb.mm(pr[:, 1, :], qT[:, cs], S[d][:], [qT, S[d]], [pr])
                vnew = W["vnew"].next()
                kb.tt("dve", vnew[:], u[:], pr[:, 0, :], ALU.subtract, [u, pr], [vnew])
                kb.mm(pr[:, 2, :], s_["QKD"][:], vnew[:], [s_["QKD"], vnew], [pr])
                kb.mm(pr[:, 3, :], kt_[:], vnew[:], [kt_, vnew], [pr])
                tmpo = W["tmpo"].next()
                kb.act(tmpo[:], pr[:, 1, :], AF.Copy, [pr, sm["expG"]], [tmpo], scale=sm["expG"][:, d, c:c + 1])
                kb.tt("dve", tmpo[:], tmpo[:], pr[:, 2, :], ALU.add, [tmpo, pr], [tmpo])
                kb.tt("pool", otot[:, c, :], otot[:, c, :], tmpo[:], ALU.add, [tmpo, otot], [otot])
                kb.fw.op(kb.fw.dve, [S[d], sm["eglast"], pr], [S[d]], lambda: nc.vector.scalar_tensor_tensor(
                    out=S[d][:], in0=S[d][:], scalar=sm["eglast"][:, d, c:c + 1], in1=pr[:, 3, :], op0=ALU.mult, op1=ALU.add))
        if CUTC:
            break
        if "odn" in kb.dbg:
            kb.dma("sp", kb.dbg["odn"][h], otot[:].rearrange("p t e -> p (t e)"), [otot], [])
        for g4 in range((NT + 3) // 4):
            tiles = list(range(4 * g4, min(4 * g4 + 4, NT)))
            ost = ostage.next()
            p = pPre.next()
            pb = p[:].rearrange("p a b -> p (a b)").bitcast(BF16)
            for ti, t in enumerate(tiles):
                ssq = W["ssq"].next()
                on = W["on"].next()
                kb.act(on[:], otot[:, t, :], AF.Square, [otot], [on, ssq], accum_out=ssq[:, 0:1])
                kb.rsqrt_inplace(ssq[:, 0:1], ssq, 1.0 / 128)
                kb.fw.op(kb.fw.dve, [otot, ssq, og], [on], lambda: nc.vector.scalar_tensor_tensor(
                    out=on[:], in0=otot[:, t, :], scalar=ssq[:, 0:1], in1=og[:], op0=ALU.mult, op1=ALU.mult))
                onb = W["onb"].next()
                kb.tt("pool", onb[:].bitcast(BF16)[:, 0:128], on[:], sgt[:, t, :], ALU.mult, [on, sgt], [onb])
                kb.tr(pb[:, ti * 128:(ti + 1) * 128], onb[:].bitcast(BF16)[:, 0:128], kb.identb[:], [onb, kb.identb], [p])
            n = len(tiles) * 128
            kb.cp("dve", ost[:, 0:n], pb[:, 0:n], [p], [ost])
            kb.dma("sp", kb.OT[12 + h, :, tiles[0] * 128:tiles[0] * 128 + n], ost[:, 0:n], [ost], [kb.OT])
    if "OTC" in kb.dbg:
        kb.dma("sp", kb.dbg["OTC"], kb.OT[12:16], [kb.OT], [])
    kb.end_phase()


_PROG = {}


def _get_prog(n_layers):
    if n_layers not in _PROG:
        _PROG[n_layers] = build_program(n_layers, debug=(), phases="0ABCDE")
    return _PROG[n_layers]


def kernel(**inputs):
    inp = {k: np.asarray(v) for k, v in inputs.items()}
    B = inp["x"].shape[0]
    depth = inp["w_in"].shape[0]
    nc = _get_prog(depth)
    base = core_inputs(inp, 0, range(depth))
    in_maps = []
    for b in range(B):
        d = dict(base)
        d["xin"] = np.ascontiguousarray(np.concatenate([inp["ctx"][b], inp["x"][b]], axis=0))
        cv = np.stack([inp["c"][b], inp["c_ctx"]], axis=0)
        d["cT"] = np.ascontiguousarray(cv.reshape(2, 16, 128).transpose(2, 1, 0))
        in_maps.append(d)
    res = run_bass_kernel_spmd(nc, in_maps, core_ids=list(range(B)))
    return np.stack([np.asarray(res.results[b]["xout"])[NCTX:] for b in range(B)], axis=0).astype(np.float32)
```

```python
import math
import numpy as np
from contextlib import ExitStack
import concourse.bass as bass
import concourse.mybir as mybir
from concourse.bass_utils import run_bass_kernel_spmd

F32 = mybir.dt.float32
BF16 = mybir.dt.bfloat16
I32 = mybir.dt.int32
U32 = mybir.dt.uint32
AF = mybir.ActivationFunctionType
ALU = mybir.AluOpType
AX = mybir.AxisListType

SEM_ROLL = 30000

D = 2048
NCTX = 256
NLAT = 4096
T = NCTX + NLAT
NT = T // 128
DIN = 5136
NE = 16
DEXP = 1024
EPS = 1e-6


class Tk:
    __slots__ = ("w", "r")

    def __init__(self):
        self.w = None
        self.r = {}


class Tile:
    def __init__(self, t, is_psum=False):
        self.t = t
        self.k = Tk()
        self.is_psum = is_psum

    def __getitem__(self, idx):
        return self.t[idx]


class Eng:
    def __init__(self, name, e):
        self.name = name
        self.e = e
        self.sem = None
        self.cnt = 0
        self.seen = {}
        self.same_sync = name in ("act", "dve", "pool")
        self.last_tok = None
        self.pending = False


class FW:
    def __init__(self, nc, stack):
        self.nc = nc
        self.stack = stack
        self.nsem = 0
        self.pe = Eng("pe", nc.tensor)
        self.act = Eng("act", nc.scalar)
        self.dve = Eng("dve", nc.vector)
        self.pool = Eng("pool", nc.gpsimd)
        self.sp = Eng("sp", nc.sync)
        self.engs = [self.pe, self.act, self.dve, self.pool, self.sp]
        for e in self.engs:
            e.sem = self.new_sem(e.name)
        self.dma_rings = {"sp": [[self.new_sem("dmah%d" % i), 0] for i in range(32)],
                          "pool": [[self.new_sem("dmas%d" % i), 0] for i in range(24)]}
        self.dma_is = {"sp": 0, "pool": 0}
        self.n_inst = 0

    def new_sem(self, name):
        self.nsem += 1
        return self.stack.enter_context(self.nc.semaphore("s_%s_%d" % (name, self.nsem)))

    def _wait(self, eng, tok):
        if tok is None:
            return
        sem, val = tok
        if eng.seen.get(sem.num, 0) >= val:
            return
        if sem is eng.sem and not eng.same_sync:
            return
        eng.e.wait_ge(sem, val)
        self.n_inst += 1
        eng.seen[sem.num] = val

    def deps(self, eng, reads, writes):
        for t in reads:
            self._wait(eng, t.k.w)
            if getattr(t, "is_psum", False):
                for tok in t.k.r.values():
                    if tok[0] is not eng.sem:
                        self._wait(eng, tok)
        for t in writes:
            self._wait(eng, t.k.w)
            for tok in t.k.r.values():
                self._wait(eng, tok)

    def commit(self, tok, reads, writes):
        for t in reads:
            t.k.r[tok[0].num] = tok
        for t in writes:
            t.k.w = tok
            t.k.r = {}

    def op(self, eng, reads, writes, fn, signal=True):
        if signal and eng.cnt >= SEM_ROLL and not eng.pending:
            eng.sem = self.new_sem(eng.name)
            eng.cnt = 0
        self.deps(eng, reads, writes)
        ins = fn()
        self.n_inst += 1
        eng.pending = not signal
        if signal:
            eng.cnt += 1
            ins.then_inc(eng.sem, 1)
            tok = (eng.sem, eng.cnt)
            eng.last_tok = tok
        else:
            tok = (eng.sem, eng.cnt + 1)
        self.commit(tok, reads, writes)
        return ins

    def dma(self, q, reads, writes, fn):
        ring = self.dma_rings[q.name]
        slot = ring[self.dma_is[q.name]]
        self.dma_is[q.name] = (self.dma_is[q.name] + 1) % len(ring)
        sem = slot[0]
        if slot[1] > 0:
            self._wait(q, (sem, slot[1]))
        self.deps(q, reads, writes)
        ins = fn(q.e)
        self.n_inst += 1
        slot[1] += 16
        ins.then_inc(sem, 16)
        tok = (sem, slot[1])
        self.commit(tok, reads, writes)
        return tok

    def barrier(self):
        toks = [e.last_tok for e in self.engs if e.last_tok is not None]
        toks += [(s[0], s[1]) for ring in self.dma_rings.values() for s in ring if s[1] > 0]
        for e in self.engs:
            for tok in toks:
                if tok[0] is e.sem:
                    continue
                self._wait(e, tok)


class Ring:
    def __init__(self, tiles):
        self.tiles = tiles
        self.i = -1

    def next(self):
        self.i = (self.i + 1) % len(self.tiles)
        return self.tiles[self.i]


class KB:
    def __init__(self, nc, stack, n_layers, debug=()):
        self.nc = nc
        self.top = stack
        self.fw = FW(nc, stack)
        self.L = n_layers
        self.debug = set(debug)
        self.uid = 0
        self.ph = None

    def _name(self, p):
        self.uid += 1
        return "%s_%d" % (p, self.uid)

    def sb(self, shape, dt, name="sb", perm=False):
        st = self.top if perm else self.ph
        return Tile(st.enter_context(self.nc.sbuf_tensor(self._name(name), list(shape), dt)))

    def ps(self, shape, dt, name="ps"):
        return Tile(self.ph.enter_context(self.nc.psum_tensor(self._name(name), list(shape), dt)), is_psum=True)

    def dram(self, name, shape, dt, kind="Internal"):
        return Tile(self.nc.dram_tensor(name, list(shape), dt, kind=kind).ap())

    def begin_phase(self):
        self.ph = ExitStack()

    def end_phase(self):
        self.fw.barrier()
        self.ph.close()
        self.ph = None

    def mm(self, out, lhsT, rhs, R, W, start=True, stop=True, signal=None):
        if signal is None:
            signal = stop
        return self.fw.op(self.fw.pe, R, W, lambda: self.nc.tensor.matmul(out, lhsT=lhsT, rhs=rhs, start=start, stop=stop), signal=signal)

    def tr(self, out, in_, ident, R, W, signal=True):
        return self.fw.op(self.fw.pe, R, W, lambda: self.nc.tensor.transpose(out=out, in_=in_, identity=ident), signal=signal)

    def _e(self, eng):
        return {"act": self.fw.act, "dve": self.fw.dve, "pool": self.fw.pool}[eng]

    def _ne(self, eng):
        return {"act": self.nc.scalar, "dve": self.nc.vector, "pool": self.nc.gpsimd}[eng]

    def act(self, out, in_, func, R, W, **kw):
        return self.fw.op(self.fw.act, R, W, lambda: self.nc.scalar.activation(out=out, in_=in_, func=func, **kw))

    def tt(self, eng, out, in0, in1, op, R, W):
        return self.fw.op(self._e(eng), R, W, lambda: self._ne(eng).tensor_tensor(out=out, in0=in0, in1=in1, op=op))

    def ts(self, eng, out, in0, s1, s2, op0, op1, R, W):
        if s2 is None:
            return self.fw.op(self._e(eng), R, W, lambda: self._ne(eng).tensor_scalar(out=out, in0=in0, scalar1=s1, scalar2=None, op0=op0))
        return self.fw.op(self._e(eng), R, W, lambda: self._ne(eng).tensor_scalar(out=out, in0=in0, scalar1=s1, scalar2=s2, op0=op0, op1=op1))

    def cp(self, eng, out, in_, R, W):
        if eng == "act":
            return self.act(out, in_, AF.Copy, R, W)
        return self.fw.op(self._e(eng), R, W, lambda: self._ne(eng).tensor_copy(out=out, in_=in_))

    def memset(self, eng, out, val, W):
        return self.fw.op(self._e(eng), [], W, lambda: self._ne(eng).memset(out, val))

    def dma(self, q, out, in_, R, W):
        qe = {"sp": self.fw.sp, "pool": self.fw.pool}[q]
        return self.fw.dma(qe, R, W, lambda e: e.dma_start(out=out, in_=in_))

    def rsqrt_inplace(self, ap, tile, mul):
        self.act(ap, ap, AF.Ln, [tile, self.epsc], [tile], scale=mul, bias=self.epsc[:ap.shape[0], 0:1])
        self.act(ap, ap, AF.Exp, [tile], [tile], scale=-0.5)


class Sub:
    def __init__(self):
        self.k = Tk()


IN_BLOCKS = [
    (0, 512, "qk", dict(H=4, gain="na_q", rope=False, dst="QT", h0=0)),
    (512, 512, "qk", dict(H=4, gain="na_k", rope=False, dst="KT", h0=0)),
    (1024, 512, "v", dict(h0=0)),
    (1536, 512, "qk", dict(H=4, gain="gq_q", rope=True, dst="QT", h0=4)),
    (2048, 512, "qk", dict(H=4, gain="gq_q", rope=True, dst="QT", h0=8)),
    (2560, 512, "kvb", dict()),
    (3072, 512, "fm", dict(c0=0)),
    (3584, 512, "fm", dict(c0=4)),
    (4096, 512, "fm", dict(c0=8)),
    (4608, 512, "gate", dict()),
    (5120, 16, "bd", dict()),
]


def declare_io(kb):
    nc = kb.nc
    L = kb.L
    I = {}

    def inp(name, shape, dt=F32):
        I[name] = nc.dram_tensor(name, list(shape), dt, kind="ExternalInput").ap()

    inp("xin", [T, D])
    inp("cT", [128, 16, 2])
    inp("ident", [128, 128])
    inp("ropec", [128, NT, 64])
    inp("ropes", [128, NT, 64])
    inp("ada_w", [L, D, 6 * D])
    inp("ada_b", [L, 6 * D])
    inp("ada_b_fm", [L, 128, 96])
    inp("gmix_fm", [L, 128, 16])
    inp("gffn_fm", [L, 128, 16])
    inp("w_in", [L, D, DIN])
    inp("w_out", [L, D, D])
    inp("na_gain", [L, 2, 128])
    inp("gq_gain", [L, 2, 128])
    kb.I = I
    kb.xout = nc.dram_tensor("xout", [T, D], F32, kind="ExternalOutput").ap()
    kb.xs_k = [Sub() for _ in range(NT)]
    kb.QT = kb.dram("QT", [12, 128, T], BF16)
    kb.KT = kb.dram("KT", [6, 128, T], BF16)
    kb.V = kb.dram("V", [T, 6, 128], BF16)
    kb.CQ = kb.dram("CQ", [12, 128, T], F32)
    kb.SG = kb.dram("SG", [T, 512], BF16)
    kb.dbg = {}

    def dbg(name, shape, dt=F32):
        if name in kb.debug:
            kb.dbg[name] = nc.dram_tensor("dbg_" + name, list(shape), dt, kind="ExternalOutput").ap()

    dbg("modfm", [128, 2 * 4 * 16])
    dbg("gates", [4, 128, D])
    dbg("QT", [12, 128, T], BF16)
    dbg("KT", [6, 128, T], BF16)
    dbg("V", [T, 6, 128], BF16)
    dbg("CQ", [12, 128, T])
    dbg("SG", [T, 512], BF16)
    dbg("BD", [128, NT * 16])


def setup_consts(kb):
    I = kb.I
    kb.begin_phase()
    kb.epsc = kb.sb([128, 1], F32, "eps", perm=True)
    kb.memset("dve", kb.epsc[:], EPS, [kb.epsc])
    kb.identf = kb.sb([128, 128], F32, "identf", perm=True)
    kb.identb = kb.sb([128, 128], BF16, "identb", perm=True)
    kb.dma("sp", kb.identf[:], I["ident"], [], [kb.identf])
    kb.cp("dve", kb.identb[:], kb.identf[:], [kb.identf], [kb.identb])
    kb.modfm = kb.sb([128, 2, 4, 16], F32, "modfm", perm=True)
    kb.gs = kb.sb([128, 2, 2, 16], F32, "gs", perm=True)
    kb.GTS = kb.dram("GTS", [4, 128, D], F32)
    kb.AFFT = kb.dram("AFFT", [NE, T], F32)
    kb.BD = kb.sb([128, NT, 16], F32, "BD", perm=True)
    kb.end_phase()


def phase0(kb, l):
    I = kb.I
    kb.begin_phase()
    cT = kb.sb([128, 16, 2], F32)
    kb.dma("sp", cT[:], I["cT"], [], [cT])
    scT = kb.sb([128, 16, 2], F32)
    kb.act(scT[:], cT[:], AF.Silu, [cT], [scT])
    screp = kb.sb([128, 16, 2, 128], F32)
    for r in range(2):
        kb.cp("dve", screp[:, :, r, :], scT[:, :, r:r + 1].to_broadcast([128, 16, 128]), [scT], [screp])
    adabfm = kb.sb([128, 96], F32)
    kb.dma("sp", adabfm[:], I["ada_b_fm"][l], [], [adabfm])
    gfm = kb.sb([128, 2, 16], F32)
    kb.dma("sp", gfm[:, 0, :], I["gmix_fm"][l], [], [gfm])
    kb.dma("sp", gfm[:, 1, :], I["gffn_fm"][l], [], [gfm])
    kb.gtb = [[kb.sb([128, D], F32, "gtb") for _ in range(2)] for _ in range(2)]
    wring = Ring([kb.sb([128, 16, 512], F32) for _ in range(2)])
    bring = Ring([kb.sb([128, 512], F32) for _ in range(2)])
    pg = Ring([kb.ps([128, 512], F32) for _ in range(2)])
    pf = Ring([kb.ps([128, 512], F32) for _ in range(2)])

    def load(blk):
        w = wring.next()
        kb.dma("sp", w[:], I["ada_w"][l, :, blk * 512:(blk + 1) * 512].rearrange("(k p) c -> p k c", p=128), [], [w])
        return w

    import os
    CUT = os.environ.get("CUT", "")
    nblk = 0 if CUT == "pre" else 24
    wn = load(0)
    for blk in range(nblk):
        w = wn
        if blk + 1 < 24:
            wn = load(blk + 1)
        m, cb = blk // 4, blk % 4
        if (CUT == "fm" and m in (2, 5)) or (CUT == "gate" and m not in (2, 5)):
            continue
        if m in (2, 5):
            gi = 0 if m == 2 else 1
            bb = bring.next()
            kb.dma("sp", bb[:], I["ada_b"][l, blk * 512:(blk + 1) * 512].partition_broadcast(128), [], [bb])
            for r in range(2):
                p = pg.next()
                for k in range(16):
                    kb.mm(p[:], screp[:, k, r, :], w[:, k, :], [screp, w], [p], start=(k == 0), stop=(k == 15))
                kb.tt("dve", kb.gtb[r][gi][:, cb * 512:(cb + 1) * 512], p[:], bb[:], ALU.add, [p, bb], [kb.gtb[r][gi]])
        else:
            mi = {0: 0, 1: 1, 3: 2, 4: 3}[m]
            p = pf.next()
            pv = p[:, 0:8].rearrange("p (j r) -> p j r", r=2)
            for j in range(4):
                for k in range(16):
                    kb.mm(pv[:, j, :], w[:, k, j * 128:(j + 1) * 128], scT[:, k, :], [scT, w], [p], start=(k == 0), stop=(k == 15))
            c0 = m * 16 + cb * 4
            kb.tt("dve", kb.modfm[:, :, mi, cb * 4:cb * 4 + 4], pv.rearrange("p j r -> p r j"),
                  adabfm[:, c0:c0 + 4].unsqueeze(1).to_broadcast([128, 2, 4]), ALU.add, [p, adabfm], [kb.modfm])
    for r in range(2):
        for wi, mi in ((0, 1), (1, 3)):
            kb.ts("dve", kb.gs[:, r, wi, :], kb.modfm[:, r, mi, :], 1.0, None, ALU.add, None, [kb.modfm], [kb.gs])
            kb.tt("dve", kb.gs[:, r, wi, :], kb.gs[:, r, wi, :], gfm[:, wi, :], ALU.mult, [kb.gs, gfm], [kb.gs])
    for r in range(2):
        for gi in range(2):
            kb.dma("sp", kb.GTS[r * 2 + gi], kb.gtb[r][gi][:], [kb.gtb[r][gi]], [kb.GTS])
    if "modfm" in kb.dbg:
        kb.dma("sp", kb.dbg["modfm"], kb.modfm[:].rearrange("p a b c -> p (a b c)"), [kb.modfm], [])
    if "gates" in kb.dbg:
        for r in range(2):
            for gi in range(2):
                kb.dma("sp", kb.dbg["gates"][r * 2 + gi], kb.gtb[r][gi][:], [kb.gtb[r][gi]], [])
    kb.end_phase()


def phaseA(kb, l):
    I = kb.I
    nc = kb.nc
    kb.begin_phase()
    xsrc = I["xin"] if l == 0 else kb.xout
    kb.ropec = kb.sb([128, NT, 64], F32, "ropec")
    kb.ropes = kb.sb([128, NT, 64], F32, "ropes")
    kb.dma("sp", kb.ropec[:], I["ropec"], [], [kb.ropec])
    kb.dma("sp", kb.ropes[:], I["ropes"], [], [kb.ropes])
    gains = {}
    gq = kb.sb([128, 4, 128], F32)
    kb.dma("sp", gq[:, 0, :], I["na_gain"][l, 0].partition_broadcast(128), [], [gq])
    kb.dma("sp", gq[:, 1, :], I["na_gain"][l, 1].partition_broadcast(128), [], [gq])
    kb.dma("sp", gq[:, 2, :], I["gq_gain"][l, 0].partition_broadcast(128), [], [gq])
    kb.dma("sp", gq[:, 3, :], I["gq_gain"][l, 1].partition_broadcast(128), [], [gq])
    qs = 128.0 ** -0.5
    kb.ts("dve", gq[:, 0, :], gq[:, 0, :], qs, None, ALU.mult, None, [gq], [gq])
    kb.ts("dve", gq[:, 2, :], gq[:, 2, :], qs, None, ALU.mult, None, [gq], [gq])
    gidx = {"na_q": 0, "na_k": 1, "gq_q": 2, "gq_k": 3}

    xt_ring = Ring([kb.sb([128, D], F32) for _ in range(2)])
    xs_ring = Ring([kb.sb([128, D], BF16) for _ in range(2)])
    sqj = kb.sb([128, D], BF16)
    ssq_ring = Ring([kb.sb([128, 1], F32) for _ in range(2)])
    xnT_ring = Ring([kb.sb([128, 16, 512], BF16) for _ in range(2)])
    tmpf_ring = Ring([kb.sb([128, 8, 128], F32) for _ in range(2)])
    w_ring = Ring([kb.sb([128, 16, 512], BF16) for _ in range(2)])
    psT = Ring([kb.ps([128, 8, 128], BF16) for _ in range(2)])
    pP = Ring([kb.ps([128, 512], F32) for _ in range(2)])
    pQ = Ring([kb.ps([128, 8, 128], BF16) for _ in range(2)])
    sq4 = kb.sb([128, 512], F32)
    ssq4_ring = Ring([kb.sb([128, 4], F32) for _ in range(2)])
    xn4_ring = Ring([kb.sb([128, 512], F32) for _ in range(2)])
    xg4_ring = Ring([kb.sb([128, 512], F32) for _ in range(2)])
    xr4_ring = Ring([kb.sb([128, 512], BF16) for _ in range(2)])
    rt_ring = Ring([kb.sb([128, 4, 2, 32], F32) for _ in range(4)])
    qst_ring = Ring([kb.sb([128, 4, 512], BF16) for _ in range(3)])
    vst_ring = Ring([kb.sb([128, 512], BF16) for _ in range(3)])
    cst_ring = Ring([kb.sb([128, 512], F32) for _ in range(3)])

    flip = [0]

    def alt(a="act", b="dve"):
        flip[0] ^= 1
        return a if flip[0] else b

    def qk_post(P, pcols, H, gname, rope, t, stage, h_off, ti):
        Pv = P[:, pcols:pcols + H * 128]
        kb.act(sq4[:, 0:H * 128], Pv, AF.Square, [P], [sq4])
        s4 = ssq4_ring.next()
        kb.fw.op(kb.fw.dve, [sq4], [s4], lambda: nc.vector.tensor_reduce(
            out=s4[:, 0:H], in_=sq4[:, 0:H * 128].rearrange("p (h d) -> p h d", h=H), axis=AX.X, op=ALU.add))
        kb.rsqrt_inplace(s4[:, 0:H], s4, 1.0 / 128)
        xn = xn4_ring.next()
        kb.tt("dve", xn[:, 0:H * 128].rearrange("p (h d) -> p h d", h=H), Pv.rearrange("p (h d) -> p h d", h=H),
              s4[:, 0:H].unsqueeze(2).to_broadcast([128, H, 128]), ALU.mult, [P, s4], [xn])
        xr = xr4_ring.next()
        gv = gq[:, gidx[gname], :].unsqueeze(1).to_broadcast([128, H, 128])
        if not (rope and t >= 2):
            kb.tt("pool", xr[:, 0:H * 128].rearrange("p (h d) -> p h d", h=H),
                  xn[:, 0:H * 128].rearrange("p (h d) -> p h d", h=H), gv, ALU.mult, [xn, gq], [xr])
        else:
            xg = xg4_ring.next()
            kb.tt("pool", xg[:, 0:H * 128].rearrange("p (h d) -> p h d", h=H),
                  xn[:, 0:H * 128].rearrange("p (h d) -> p h d", h=H), gv, ALU.mult, [xn, gq], [xg])
            v5 = xg[:, 0:H * 128].rearrange("p (h a b f) -> p h a b f", h=H, a=2, b=2)
            o5 = xr[:, 0:H * 128].rearrange("p (h a b f) -> p h a b f", h=H, a=2, b=2)
            x1, x2 = v5[:, :, :, 0, :], v5[:, :, :, 1, :]
            cb = kb.ropec[:, t, :].rearrange("p (a f) -> p a f", a=2).unsqueeze(1).to_broadcast([128, H, 2, 32])
            sb_ = kb.ropes[:, t, :].rearrange("p (a f) -> p a f", a=2).unsqueeze(1).to_broadcast([128, H, 2, 32])
            t1, t2, t3, t4 = rt_ring.next(), rt_ring.next(), rt_ring.next(), rt_ring.next()
            kb.tt("dve", t1[:, 0:H], x1, cb, ALU.mult, [xg, kb.ropec], [t1])
            kb.tt("pool", t2[:, 0:H], x2, sb_, ALU.mult, [xg, kb.ropes], [t2])
            kb.tt("dve", o5[:, :, :, 0, :], t1[:, 0:H], t2[:, 0:H], ALU.subtract, [t1, t2], [xr])
            kb.tt("pool", t3[:, 0:H], x1, sb_, ALU.mult, [xg, kb.ropes], [t3])
            kb.tt("dve", t4[:, 0:H], x2, cb, ALU.mult, [xg, kb.ropec], [t4])
            kb.tt("pool", o5[:, :, :, 1, :], t3[:, 0:H], t4[:, 0:H], ALU.add, [t3, t4], [xr])
        pq = pQ.next()
        for h in range(H):
            kb.tr(pq[:, h, :], xr[:, h * 128:(h + 1) * 128], kb.identb[:], [xr, kb.identb], [pq], signal=(h == H - 1))
        kb.cp(alt(), stage[:, h_off:h_off + H, ti * 128:(ti + 1) * 128], pq[:, 0:H, :], [pq], [stage])

    def load_x(t):
        xt = xt_ring.next()
        kb.dma("sp", xt[:], xsrc[t * 128:(t + 1) * 128, :], [kb.xs_k[t]], [xt])
        return xt

    def load_w(bi):
        c0, n, kind, info = IN_BLOCKS[bi]
        w = w_ring.next()
        kb.dma("pool", w[:, :, 0:n], I["w_in"][l, :, c0:c0 + n].rearrange("(k p) c -> p k c", p=128), [], [w])
        return w

    ngroups = (NT + 3) // 4
    xt_next = load_x(0)
    for g in range(ngroups):
        tiles = list(range(4 * g, min(4 * g + 4, NT)))
        nt = len(tiles)
        ntok = nt * 128
        tok0 = tiles[0] * 128
        xnT = xnT_ring.next()
        for ti, t in enumerate(tiles):
            r = 1 if t < 2 else 0
            xt = xt_next
            if t + 1 < NT:
                xt_next = load_x(t + 1)
            ssq = ssq_ring.next()
            kb.act(sqj[:], xt[:], AF.Square, [xt], [sqj, ssq], accum_out=ssq[:, 0:1])
            kb.rsqrt_inplace(ssq[:, 0:1], ssq, 1.0 / D)
            xs = xs_ring.next()
            kb.ts("dve", xs[:], xt[:], ssq[:, 0:1], None, ALU.mult, None, [xt, ssq], [xs])
            for hf in range(2):
                p = psT.next()
                for k8 in range(8):
                    k = hf * 8 + k8
                    kb.tr(p[:, k8, :], xs[:, k * 128:(k + 1) * 128], kb.identb[:], [xs, kb.identb], [p], signal=(k8 == 7))
                tf = tmpf_ring.next()
                kb.tt("dve", tf[:], p[:], kb.gs[:, r, 0, hf * 8:hf * 8 + 8].unsqueeze(2).to_broadcast([128, 8, 128]), ALU.mult, [p, kb.gs], [tf])
                kb.tt("pool", xnT[:, hf * 8:hf * 8 + 8, ti * 128:(ti + 1) * 128], tf[:],
                      kb.modfm[:, r, 0, hf * 8:hf * 8 + 8].unsqueeze(2).to_broadcast([128, 8, 128]), ALU.add, [tf, kb.modfm], [xnT])
        import os
        CUTA = os.environ.get("CUTA", "")
        if CUTA == "A1":
            continue
        w_next = load_w(0)
        for bi, (c0, n, kind, info) in enumerate(IN_BLOCKS):
            w = w_next
            if bi + 1 < len(IN_BLOCKS):
                w_next = load_w(bi + 1)
            if CUTA and kind not in CUTA.split(","):
                continue
            if kind == "fm":
                for j in range(4):
                    P = pP.next()
                    for k in range(16):
                        kb.mm(P[:, 0:ntok], w[:, k, j * 128:(j + 1) * 128], xnT[:, k, 0:ntok], [w, xnT], [P], start=(k == 0), stop=(k == 15))
                    cst = cst_ring.next()
                    kb.cp(alt(), cst[:, 0:ntok], P[:, 0:ntok], [P], [cst])
                    kb.dma("sp", kb.CQ[info["c0"] + j, :, tok0:tok0 + ntok], cst[:, 0:ntok], [cst], [kb.CQ])
                continue
            stage = None
            if kind in ("qk", "kvb"):
                stage = qst_ring.next()
            for ti, t in enumerate(tiles):
                P = pP.next()
                for k in range(16):
                    kb.mm(P[:, 0:n], xnT[:, k, ti * 128:(ti + 1) * 128], w[:, k, 0:n], [w, xnT], [P], start=(k == 0), stop=(k == 15))
                if kind == "qk":
                    qk_post(P, 0, info["H"], info["gain"], info["rope"], t, stage, 0, ti)
                elif kind == "kvb":
                    KVB = os.environ.get("KVB", "ab")
                    if "a" in KVB:
                        qk_post(P, 0, 2, "gq_k", True, t, stage, 0, ti)
                    if "b" in KVB:
                        vst = vst_ring.next()
                        kb.cp("dve", vst[:, 0:256], P[:, 256:512], [P], [vst])
                        kb.dma("sp", kb.V[t * 128:(t + 1) * 128, 4:6, :], vst[:, 0:256].rearrange("p (h d) -> p h d", h=2), [vst], [kb.V])
                elif kind == "v":
                    vst = vst_ring.next()
                    kb.cp("act", vst[:], P[:], [P], [vst])
                    kb.dma("sp", kb.V[t * 128:(t + 1) * 128, 0:4, :], vst[:].rearrange("p (h d) -> p h d", h=4), [vst], [kb.V])
                elif kind == "gate":
                    vst = vst_ring.next()
                    kb.act(vst[:], P[:], AF.Silu, [P], [vst])
                    kb.dma("sp", kb.SG[t * 128:(t + 1) * 128, :], vst[:], [vst], [kb.SG])
                elif kind == "bd":
                    kb.cp("dve", kb.BD[:, t, :], P[:, 0:16], [P], [kb.BD])
            if kind == "qk":
                dst = kb.QT if info["dst"] == "QT" else kb.KT
                h0 = info["h0"]
                kb.dma("sp", dst[h0:h0 + 4, :, tok0:tok0 + ntok].rearrange("h d t -> d h t"), stage[:, 0:4, 0:ntok], [stage], [dst])
            elif kind == "kvb":
                kb.dma("sp", kb.KT[4:6, :, tok0:tok0 + ntok].rearrange("h d t -> d h t"), stage[:, 0:2, 0:ntok], [stage], [kb.KT])
    for nm, tl in (("QT", kb.QT), ("KT", kb.KT), ("V", kb.V), ("CQ", kb.CQ), ("SG", kb.SG)):
        if nm in kb.dbg:
            kb.dma("sp", kb.dbg[nm], tl[:], [tl], [])
    if "BD" in kb.dbg:
        kb.dma("sp", kb.dbg["BD"], kb.BD[:].rearrange("p t c -> p (t c)"), [kb.BD], [])
    kb.end_phase()


def host_consts():
    t = np.arange(NLAT)
    pos = np.stack([t // 64, t % 64], axis=-1).astype(np.float32)
    nf = 32
    freqs = (10000.0 ** (-np.arange(nf, dtype=np.float32) / nf)).astype(np.float32)
    ang = pos[:, :, None] * freqs
    cos = np.zeros((T, 64), np.float32)
    sin = np.zeros((T, 64), np.float32)
    cos[NCTX:] = np.cos(ang).reshape(NLAT, 64)
    sin[NCTX:] = np.sin(ang).reshape(NLAT, 64)
    cos[:NCTX] = 1.0
    out = {
        "ident": np.eye(128, dtype=np.float32),
        "ropec": np.ascontiguousarray(cos.reshape(NT, 128, 64).transpose(1, 0, 2)),
        "ropes": np.ascontiguousarray(sin.reshape(NT, 128, 64).transpose(1, 0, 2)),
    }
    ii = np.arange(128)
    trif = (ii[:, None] <= ii[None, :]).astype(np.float32)
    trib = (ii[:, None] >= ii[None, :]).astype(np.float32)
    out["tri"] = np.stack([trif, trib])
    out["negstrict"] = -(out["tri"] - np.eye(128, dtype=np.float32)[None])
    return out


def fm(v):
    s = v.shape
    return np.ascontiguousarray(np.swapaxes(v.reshape(s[:-1] + (s[-1] // 128, 128)), -1, -2))


def core_inputs(inp, b, layers):
    ls = list(layers)
    d = dict(host_consts())
    d["xin"] = np.ascontiguousarray(np.concatenate([inp["ctx"][b], inp["x"][b]], axis=0))
    cv = np.stack([inp["c"][b], inp["c_ctx"]], axis=0)
    d["cT"] = np.ascontiguousarray(cv.reshape(2, 16, 128).transpose(2, 1, 0))
    d["ada_w"] = inp["ada_w"][ls]
    d["ada_b"] = inp["ada_b"][ls]
    d["ada_b_fm"] = fm(inp["ada_b"][ls])
    d["gmix_fm"] = fm(inp["norm_mix"][ls])
    d["gffn_fm"] = fm(inp["norm_ffn"][ls])
    d["w_in"] = inp["w_in"][ls]
    d["w_out"] = inp["w_out"][ls]
    d["na_gain"] = inp["na_qk_gain"][ls]
    d["gq_gain"] = inp["gqa_qk_gain"][ls]
    d["nab"] = na_bias_tables(inp["na_rpb"][ls])
    cw = inp["dn_conv"][ls]
    d["convw_fm"] = np.ascontiguousarray(cw.reshape(len(ls), 5, 12, 128).transpose(0, 3, 2, 1))
    d["alog_bc"] = np.ascontiguousarray(np.broadcast_to(inp["dn_a_log"][ls].reshape(len(ls), 1, 8), (len(ls), 128, 8)))
    d["dtb_bc"] = np.ascontiguousarray(np.broadcast_to(inp["dn_dt_bias"][ls].reshape(len(ls), 1, 8), (len(ls), 128, 8)))
    d["ogain"] = inp["dn_out_gain"][ls]
    d["router_fm"] = np.ascontiguousarray(inp["router_w"][ls].reshape(len(ls), 16, 128, NE).transpose(0, 2, 1, 3))
    d["w_gate"] = inp["exp_w_gate"][ls]
    d["w_up"] = inp["exp_w_up"][ls]
    d["w_down"] = inp["exp_w_down"][ls]
    return d


def build_program(n_layers, debug=(), phases="0A"):
    nc = bass.Bass("TRN2", target_bir_lowering=False)
    with ExitStack() as st:
        kb = KB(nc, st, n_layers, debug)
        declare_io(kb)
        declare_io_B(kb)
        declare_io_C(kb)
        declare_io_DE(kb)
        setup_consts(kb)
        for l in range(n_layers):
            if "0" in phases:
                phase0(kb, l)
            if "A" in phases:
                phaseA(kb, l)
            if "B" in phases or "a" in phases or "b" in phases:
                phaseB(kb, l, do_a=("B" in phases or "a" in phases), do_b=("B" in phases or "b" in phases))
            if "C" in phases:
                phaseC(kb, l)
            if "D" in phases:
                phaseD(kb, l)
            if "E" in phases:
                phaseE(kb, l)
        kb.fw.barrier()
        print("n_inst", kb.fw.n_inst, "nsem", kb.fw.nsem)
    return nc


_NA_CACHE = {}


def na_patterns():
    if "p" in _NA_CACHE:
        return _NA_CACHE["p"]
    rows, W = 64, 64
    pats = {}
    plist = []
    pairs = {}
    for qt in range(32):
        qr = np.repeat(np.arange(2 * qt, 2 * qt + 2), W)
        qc = np.tile(np.arange(W), 2)
        rstart = np.clip(qr - 4, 0, rows - 8)
        cstart = np.clip(qc - 8, 0, W - 16)
        lst = []
        for kt in range(32):
            kr = np.repeat(np.arange(2 * kt, 2 * kt + 2), W)[:, None]
            kc = np.tile(np.arange(W), 2)[:, None]
            valid = (kr >= rstart[None]) & (kr < rstart[None] + 8) & (kc >= cstart[None]) & (kc < cstart[None] + 16)
            if not valid.any():
                continue
            dr = np.clip(kr - qr[None] + 7, 0, 14)
            dc = np.clip(kc - qc[None] + 15, 0, 30)
            dr = np.where(valid, dr, 0).astype(np.int16)
            dc = np.where(valid, dc, 0).astype(np.int16)
            key = (valid.tobytes(), dr.tobytes(), dc.tobytes())
            if key not in pats:
                pats[key] = len(plist)
                plist.append((valid, dr, dc))
            lst.append((kt, pats[key]))
        pairs[qt] = lst
    _NA_CACHE["p"] = (pairs, plist)
    return pairs, plist


def na_bias_tables(rpb):
    pairs, plist = na_patterns()
    L, H = rpb.shape[0], rpb.shape[1]
    out = np.empty((L, H, len(plist), 128, 128), np.float32)
    for pi, (valid, dr, dc) in enumerate(plist):
        g = rpb[:, :, dr, dc]
        out[:, :, pi] = np.where(valid[None, None], g, np.float32(-30000.0))
    return out


def declare_io_B(kb):
    nc = kb.nc
    pairs, plist = na_patterns()
    kb.NP = len(plist)
    kb.I["nab"] = nc.dram_tensor("nab", [kb.L, 4, kb.NP, 128, 128], F32, kind="ExternalInput").ap()
    kb.OT = kb.dram("OT", [16, 128, T], BF16)
    if "OT" in kb.debug:
        kb.dbg["OT"] = nc.dram_tensor("dbg_OT", [16, 128, T], BF16, kind="ExternalOutput").ap()


def phaseB(kb, l, do_a=True, do_b=True):
    I = kb.I
    nc = kb.nc
    pairs, plist = na_patterns()
    kb.begin_phase()
    kT_ring = Ring([kb.sb([128, T], BF16) for _ in range(2)])
    v1_ring = Ring([kb.sb([128, NT, 130], BF16) for _ in range(2)])
    for v1 in v1_ring.tiles:
        kb.memset("pool", v1[:, :, 128:130], 1.0, [v1])
    qT_ring = Ring([kb.sb([128, 512], BF16) for _ in range(3)])
    pT_ring = Ring([kb.sb([128, 512], BF16) for _ in range(3)])
    sb_ring = Ring([kb.sb([128, 128], F32) for _ in range(3)])
    pS = Ring([kb.ps([128, 512], F32) for _ in range(3)])
    pO = Ring([kb.ps([128, 512], F32) for _ in range(4)])
    pTr = Ring([kb.ps([128, 8, 128], BF16) for _ in range(1)])
    rinv_ring = Ring([kb.sb([128, 4], F32) for _ in range(2)])
    on_ring = Ring([kb.sb([128, 128], BF16) for _ in range(3)])
    ost_ring = Ring([kb.sb([128, 512], BF16) for _ in range(3)])
    nab = None
    if do_a:
        nab = kb.sb([128, kb.NP, 128], F32)

    def load_kv(kvh):
        kT = kT_ring.next()
        v1 = v1_ring.next()
        kb.dma("sp", kT[:], kb.KT[kvh], [kb.KT], [kT])
        kb.dma("sp", v1[:, :, 0:128], kb.V[:, kvh, :].rearrange("(t p) d -> p t d", p=128), [kb.V], [v1])
        return kT, v1

    def attn(qh, kT, v1, chunk, qtiles, keys):
        nq = len(qtiles)
        nqt = nq * 128
        q0 = qtiles[0] * 128
        qT = qT_ring.next()
        kb.dma("sp", qT[:, 0:nqt], kb.QT[qh, :, q0:q0 + nqt], [kb.QT], [qT])
        acc = [pO.next() for _ in range(nq)]
        nk = len(keys)

        def s_mm(ki):
            S_ = pS.next()
            kt_ = keys[ki][0]
            kb.mm(S_[:, 0:nqt], kT[:, kt_ * 128:(kt_ + 1) * 128], qT[:, 0:nqt], [kT, qT], [S_])
            return S_

        S_next = s_mm(0)
        for ki, (kt, bias) in enumerate(keys):
            S = S_next
            if ki + 1 < nk:
                S_next = s_mm(ki + 1)
            pT = pT_ring.next()
            if bias is None:
                kb.act(pT[:, 0:nqt], S[:, 0:nqt], AF.Exp, [S], [pT])
            else:
                sbt = sb_ring.next()
                kb.tt("dve", sbt[:, 0:nqt], S[:, 0:nqt], bias, ALU.add, [S, nab], [sbt])
                kb.act(pT[:, 0:nqt], sbt[:, 0:nqt], AF.Exp, [sbt], [pT])
            for j in range(nq):
                a = acc[j]
                kb.mm(a[:, 0:129], pT[:, j * 128:(j + 1) * 128], v1[:, kt, 0:129], [pT, v1], [a],
                      start=(ki == 0), stop=(ki == nk - 1), signal=(ki == nk - 1))
        ost = ost_ring.next()
        ptr = pTr.next()
        rinv = rinv_ring.next()
        for j in range(nq):
            a = acc[j]
            off = 0
            kb.fw.op(kb.fw.dve, [a], [rinv], lambda: nc.vector.reciprocal(out=rinv[:, j:j + 1], in_=a[:, off + 128:off + 129]))
            on = on_ring.next()
            kb.act(on[:], a[:, off:off + 128], AF.Copy, [a, rinv], [on], scale=rinv[:, j:j + 1])
            kb.tr(ptr[:, j, :], on[:], kb.identb[:], [on, kb.identb], [ptr])
        kb.cp("dve", ost[:, 0:nqt], ptr[:, 0:nq, :].rearrange("p j q -> p (j q)"), [ptr], [ost])
        kb.dma("sp", kb.OT[chunk, :, q0:q0 + nqt], ost[:, 0:nqt], [ost], [kb.OT])

    if do_a:
        for h in range(4):
            kT, v1 = load_kv(h)
            kb.dma("sp", nab[:], I["nab"][l, h].rearrange("n k q -> k n q"), [], [nab])
            attn(h, kT, v1, h, [0, 1], [(0, None), (1, None)])
            for qt in range(32):
                keys = [(0, None), (1, None)] + [(kt + 2, nab[:, pid, :]) for kt, pid in pairs[qt]]
                attn(h, kT, v1, h, [qt + 2], keys)
    if do_b:
        for kvh in range(2):
            kT, v1 = load_kv(4 + kvh)
            for qi in range(4):
                qh = 4 + kvh * 4 + qi
                attn(qh, kT, v1, qh, [0, 1], [(0, None), (1, None)])
                for g in range(8):
                    attn(qh, kT, v1, qh, [2 + 4 * g + j for j in range(4)], [(kt, None) for kt in range(NT)])
    if "OT" in kb.dbg:
        kb.dma("sp", kb.dbg["OT"][0:12], kb.OT[0:12], [kb.OT], [])
    kb.end_phase()


def declare_io_DE(kb):
    nc = kb.nc
    L = kb.L
    kb.I["router_fm"] = nc.dram_tensor("router_fm", [L, 128, 16, NE], F32, kind="ExternalInput").ap()
    kb.I["w_gate"] = nc.dram_tensor("w_gate", [L, NE, D, DEXP], F32, kind="ExternalInput").ap()
    kb.I["w_up"] = nc.dram_tensor("w_up", [L, NE, D, DEXP], F32, kind="ExternalInput").ap()
    kb.I["w_down"] = nc.dram_tensor("w_down", [L, NE, DEXP, D], F32, kind="ExternalInput").ap()
    kb.H2 = kb.dram("H2", [T, D], BF16)
    kb.xs_c = [Sub() for _ in range(4)]
    for nm, shape, dt in (("xmid", [T, D], F32), ("H2", [T, D], BF16), ("affT", [NE, T], F32), ("idx", [128, 5 * NE], I32), ("gate", [128, 5 * NE], F32)):
        if nm in kb.debug:
            kb.dbg[nm] = nc.dram_tensor("dbg_" + nm, shape, dt, kind="ExternalOutput").ap()


def phaseD(kb, l):
    I = kb.I
    nc = kb.nc
    kb.begin_phase()
    xsrc = I["xin"] if l == 0 else kb.xout
    kb.affT = kb.sb([NE, T], F32, "affT")
    kb.gtb = [[None, None], [None, None]]
    for r in range(2):
        kb.gtb[r][0] = kb.sb([128, D], F32, "gtb")
        kb.dma("sp", kb.gtb[r][0][:], kb.GTS[r * 2 + 0], [kb.GTS], [kb.gtb[r][0]])
    wo = kb.sb([128, 16, D], BF16)
    for q in range(4):
        kb.dma("pool", wo[:, :, q * 512:(q + 1) * 512], I["w_out"][l, :, q * 512:(q + 1) * 512].rearrange("(k p) c -> p k c", p=128), [], [wo])
    rw = kb.sb([128, 16, NE], F32)
    kb.dma("sp", rw[:], I["router_fm"][l], [], [rw])
    ot_ring = Ring([kb.sb([128, 16, 512], BF16) for _ in range(2)])
    xt_ring = Ring([kb.sb([128, D], F32) for _ in range(2)])
    xn_ring = Ring([kb.sb([128, D], F32) for _ in range(1)])
    tmp_ring = Ring([kb.sb([128, 512], F32) for _ in range(3)])
    sqj = kb.sb([128, D], BF16)
    ssq_ring = Ring([kb.sb([128, 1], F32) for _ in range(2)])
    xs2_ring = Ring([kb.sb([128, D], F32) for _ in range(1)])
    xs2b_ring = Ring([kb.sb([128, D], BF16) for _ in range(1)])
    h2T_ring = Ring([kb.sb([128, 16, 128], F32) for _ in range(1)])
    tf_ring = Ring([kb.sb([128, 4, 128], F32) for _ in range(2)])
    sm_ring = Ring([kb.sb([128, 4], F32) for _ in range(2)])
    lg_ring = Ring([kb.sb([128, NE], F32) for _ in range(2)])
    pP = Ring([kb.ps([128, 512], F32) for _ in range(3)])
    pT = Ring([kb.ps([128, 4, 128], F32) for _ in range(2)])
    pL = Ring([kb.ps([128, 512], F32) for _ in range(2)])

    def load_ot(g):
        tiles = list(range(4 * g, min(4 * g + 4, NT)))
        ot = ot_ring.next()
        n = len(tiles) * 128
        kb.dma("sp", ot[:, :, 0:n], kb.OT[:, :, tiles[0] * 128:tiles[0] * 128 + n].rearrange("k d t -> d k t"), [kb.OT], [ot])
        return ot

    def load_x(t):
        xt = xt_ring.next()
        kb.dma("sp", xt[:], xsrc[t * 128:(t + 1) * 128, :], [kb.xs_k[t]], [xt])
        return xt

    ngroups = (NT + 3) // 4
    ot_next = load_ot(0)
    xt_next = load_x(0)
    for g in range(ngroups):
        tiles = list(range(4 * g, min(4 * g + 4, NT)))
        ot = ot_next
        if g + 1 < ngroups:
            ot_next = load_ot(g + 1)
        for ti, t in enumerate(tiles):
            r = 1 if t < 2 else 0
            xt = xt_next
            if t + 1 < NT:
                xt_next = load_x(t + 1)
            xn = xn_ring.next()
            for cb in range(4):
                P = pP.next()
                for k in range(16):
                    kb.mm(P[:], ot[:, k, ti * 128:(ti + 1) * 128], wo[:, k, cb * 512:(cb + 1) * 512], [ot, wo], [P], start=(k == 0), stop=(k == 15))
                tmp = tmp_ring.next()
                kb.tt("dve", tmp[:], P[:], kb.gtb[r][0][:, cb * 512:(cb + 1) * 512], ALU.mult, [P, kb.gtb[r][0]], [tmp])
                kb.tt("pool", xn[:, cb * 512:(cb + 1) * 512], tmp[:], xt[:, cb * 512:(cb + 1) * 512], ALU.add, [tmp, xt], [xn])
            kb.dma("sp", kb.xout[t * 128:(t + 1) * 128, :], xn[:], [xn], [kb.xs_k[t]])
            ssq = ssq_ring.next()
            kb.act(sqj[:], xn[:], AF.Square, [xn], [sqj, ssq], accum_out=ssq[:, 0:1])
            kb.rsqrt_inplace(ssq[:, 0:1], ssq, 1.0 / D)
            xs2 = xs2_ring.next()
            kb.ts("dve", xs2[:], xn[:], ssq[:, 0:1], None, ALU.mult, None, [xn, ssq], [xs2])
            xs2b = xs2b_ring.next()
            kb.cp("pool", xs2b[:], xs2[:], [xs2], [xs2b])
            kb.dma("sp", kb.H2[t * 128:(t + 1) * 128, :], xs2b[:], [xs2b], [kb.H2])
            h2T = h2T_ring.next()
            for q in range(4):
                p = pT.next()
                for j in range(4):
                    k = q * 4 + j
                    kb.tr(p[:, j, :], xs2[:, k * 128:(k + 1) * 128], kb.identf[:], [xs2, kb.identf], [p], signal=(j == 3))
                tf = tf_ring.next()
                kb.tt("dve", tf[:], p[:], kb.gs[:, r, 1, q * 4:q * 4 + 4].unsqueeze(2).to_broadcast([128, 4, 128]), ALU.mult, [p, kb.gs], [tf])
                kb.tt("pool", h2T[:, q * 4:q * 4 + 4, :], tf[:], kb.modfm[:, r, 2, q * 4:q * 4 + 4].unsqueeze(2).to_broadcast([128, 4, 128]), ALU.add, [tf, kb.modfm], [h2T])
            PL = pL.next()
            for k in range(16):
                kb.mm(PL[:, 0:NE], h2T[:, k, :], rw[:, k, :], [h2T, rw], [PL], start=(k == 0), stop=(k == 15))
            sm = sm_ring.next()
            kb.fw.op(kb.fw.dve, [PL], [sm], lambda: nc.vector.tensor_reduce(out=sm[:, 0:1], in_=PL[:, 0:NE], axis=AX.X, op=ALU.max))
            kb.ts("dve", sm[:, 1:2], sm[:, 0:1], -1.0, None, ALU.mult, None, [sm], [sm])
            lg = lg_ring.next()
            kb.act(lg[:], PL[:, 0:NE], AF.Exp, [PL, sm], [lg, sm], bias=sm[:, 1:2], accum_out=sm[:, 2:3])
            kb.fw.op(kb.fw.dve, [sm], [sm], lambda: nc.vector.reciprocal(out=sm[:, 3:4], in_=sm[:, 2:3]))
            kb.ts("dve", lg[:], lg[:], sm[:, 3:4], None, ALU.mult, None, [lg, sm], [lg])
            kb.tr(PL[0:NE, 128:256], lg[:], kb.identf[:], [lg, kb.identf], [PL])
            kb.cp("act", kb.affT[:, t * 128:(t + 1) * 128], PL[0:NE, 128:256], [PL], [kb.affT])
    kb.dma("sp", kb.AFFT[:], kb.affT[:], [kb.affT], [kb.AFFT])
    if "xmid" in kb.dbg:
        kb.dma("sp", kb.dbg["xmid"], kb.xout, [], kb.xs_k)
    if "H2" in kb.dbg:
        kb.dma("sp", kb.dbg["H2"], kb.H2[:], [kb.H2], [])
    if "affT" in kb.dbg:
        kb.dma("sp", kb.dbg["affT"], kb.affT[:], [kb.affT], [])
    kb.end_phase()


def phaseE(kb, l):
    I = kb.I
    nc = kb.nc
    kb.begin_phase()
    NS = 544
    kb.affT = kb.sb([NE, T], F32, "affT")
    kb.dma("sp", kb.affT[:], kb.AFFT[:], [kb.AFFT], [kb.affT])
    kb.gtb = [[None, None], [None, None]]
    for r in range(2):
        kb.gtb[r][1] = kb.sb([128, D], F32, "gtb")
        kb.dma("sp", kb.gtb[r][1][:], kb.GTS[r * 2 + 1], [kb.GTS], [kb.gtb[r][1]])
    work = kb.sb([NE, NLAT], F32)
    workc = kb.sb([NE, NCTX], F32)
    vals = kb.sb([NE, NS], F32)
    idxu = kb.sb([NE, NS], U32)
    idxf = kb.sb([NE, NS], F32)
    kb.cp("dve", work[:], kb.affT[:, NCTX:T], [kb.affT], [work])
    kb.cp("pool", workc[:], kb.affT[:, 0:NCTX], [kb.affT], [workc])
    for (wk, base, nround) in ((work, 0, 64), (workc, 512, 4)):
        for i in range(nround):
            c0 = base + i * 8
            kb.fw.op(kb.fw.dve, [wk], [vals], lambda: nc.vector.max(out=vals[:, c0:c0 + 8], in_=wk[:]))
            kb.fw.op(kb.fw.dve, [wk, vals], [idxu], lambda: nc.vector.max_index(out=idxu[:, c0:c0 + 8], in_max=vals[:, c0:c0 + 8], in_values=wk[:]))
            if i + 1 < nround:
                kb.fw.op(kb.fw.dve, [vals, wk], [wk], lambda: nc.vector.match_replace(out=wk[:], in_to_replace=vals[:, c0:c0 + 8], in_values=wk[:], imm_value=-1.0))
    kb.cp("dve", idxf[:], idxu[:], [idxu], [idxf])
    kb.ts("dve", idxf[:, 0:512], idxf[:, 0:512], float(NCTX), None, ALU.add, None, [idxf], [idxf])
    idxT = kb.sb([128, 5, NE], I32)
    gateT = kb.sb([128, 5, NE], F32)
    kb.memset("dve", idxT[:], 0, [idxT])
    kb.memset("dve", gateT[:], 0.0, [gateT])
    pX = kb.ps([128, 512], F32)
    for s in range(5):
        n = 128 if s < 4 else 32
        kb.tr(pX[0:n, 0:NE], idxf[:, s * 128:s * 128 + n], kb.identf[0:NE, 0:NE], [idxf, kb.identf], [pX])
        kb.cp("dve", idxT[0:n, s, :], pX[0:n, 0:NE], [pX], [idxT])
        kb.tr(pX[0:n, 64:64 + NE], vals[:, s * 128:s * 128 + n], kb.identf[0:NE, 0:NE], [vals, kb.identf], [pX])
        kb.cp("dve", gateT[0:n, s, :], pX[0:n, 64:64 + NE], [pX], [gateT])
    if "idx" in kb.dbg:
        kb.dma("sp", kb.dbg["idx"], idxT[:].rearrange("p s e -> p (s e)"), [idxT], [])
        kb.dma("sp", kb.dbg["gate"], gateT[:].rearrange("p s e -> p (s e)"), [gateT], [])
    xe_ring = Ring([kb.sb([128, D], BF16) for _ in range(3)])
    xeT = kb.sb([128, 16, NS], BF16)
    tf_ring = Ring([kb.sb([128, 8, 128], F32) for _ in range(2)])
    wg_ring = Ring([kb.sb([128, 16, 256], BF16) for _ in range(2)])
    wu_ring = Ring([kb.sb([128, 16, 256], BF16) for _ in range(2)])
    wd_ring = Ring([kb.sb([128, 8, 512], BF16) for _ in range(2)])
    hidT = kb.sb([128, 8, NS], BF16)
    sg_ring = Ring([kb.sb([128, NS], F32) for _ in range(2)])
    yes = [kb.sb([128, D], F32) for _ in range(5)]
    pTr = Ring([kb.ps([128, 8, 128], BF16) for _ in range(2)])
    pG = Ring([kb.ps([128, 512], F32) for _ in range(1)])
    pGc = Ring([kb.ps([128, 512], F32) for _ in range(1)])
    pU = Ring([kb.ps([128, 512], F32) for _ in range(1)])
    pY = Ring([kb.ps([128, 512], F32) for _ in range(2)])
    for e in range(NE):
        for s in range(5):
            n = 128 if s < 4 else 32
            r = 0 if s < 4 else 1
            xe = xe_ring.next()
            kb.fw.dma(kb.fw.pool, [idxT, kb.H2], [xe], lambda q: q.indirect_dma_start(
                out=xe[0:n, :], out_offset=None, in_=kb.H2[:], in_offset=bass.IndirectOffsetOnAxis(ap=idxT[0:n, s, e:e + 1], axis=0)))
            for hf in range(2):
                p = pTr.next()
                for k8 in range(8):
                    k = hf * 8 + k8
                    kb.tr(p[:, k8, 0:n], xe[0:n, k * 128:(k + 1) * 128], kb.identb[0:n, 0:n], [xe, kb.identb], [p], signal=(k8 == 7))
                tf = tf_ring.next()
                kb.tt("dve", tf[:, :, 0:n], p[:, :, 0:n], kb.gs[:, r, 1, hf * 8:hf * 8 + 8].unsqueeze(2).to_broadcast([128, 8, n]), ALU.mult, [p, kb.gs], [tf])
                kb.tt("pool", xeT[:, hf * 8:hf * 8 + 8, s * 128:s * 128 + n], tf[:, :, 0:n],
                      kb.modfm[:, r, 2, hf * 8:hf * 8 + 8].unsqueeze(2).to_broadcast([128, 8, n]), ALU.add, [tf, kb.modfm], [xeT])
        for cq in range(4):
            wg = wg_ring.next()
            wu = wu_ring.next()
            kb.dma("pool", wg[:], I["w_gate"][l, e, :, cq * 256:(cq + 1) * 256].rearrange("(k p) c -> p k c", p=128), [], [wg])
            kb.dma("pool", wu[:], I["w_up"][l, e, :, cq * 256:(cq + 1) * 256].rearrange("(k p) c -> p k c", p=128), [], [wu])
            for cj in range(2):
                c = cq * 2 + cj
                G, U, Gc = pG.next(), pU.next(), pGc.next()
                for k in range(16):
                    kb.mm(G[:], wg[:, k, cj * 128:(cj + 1) * 128], xeT[:, k, 0:512], [wg, xeT], [G], start=(k == 0), stop=(k == 15))
                for k in range(16):
                    kb.mm(Gc[:, 0:32], wg[:, k, cj * 128:(cj + 1) * 128], xeT[:, k, 512:544], [wg, xeT], [Gc], start=(k == 0), stop=(k == 15))
                for k in range(16):
                    kb.mm(U[:], wu[:, k, cj * 128:(cj + 1) * 128], xeT[:, k, 0:512], [wu, xeT], [U], start=(k == 0), stop=(k == 15))
                for k in range(16):
                    kb.mm(Gc[:, 32:64], wu[:, k, cj * 128:(cj + 1) * 128], xeT[:, k, 512:544], [wu, xeT], [Gc], start=(k == 0), stop=(k == 15))
                sg = sg_ring.next()
                kb.act(sg[:, 0:512], G[:], AF.Silu, [G], [sg])
                kb.act(sg[:, 512:544], Gc[:, 0:32], AF.Silu, [Gc], [sg])
                kb.tt("dve", hidT[:, c, 0:512], sg[:, 0:512], U[:], ALU.mult, [sg, U], [hidT])
                kb.tt("dve", hidT[:, c, 512:544], sg[:, 512:544], Gc[:, 32:64], ALU.mult, [sg, Gc], [hidT])
        for cb in range(4):
            wd = wd_ring.next()
            kb.dma("pool", wd[:], I["w_down"][l, e, :, cb * 512:(cb + 1) * 512].rearrange("(c p) n -> p c n", p=128), [], [wd])
            for s in range(5):
                n = 128 if s < 4 else 32
                r = 0 if s < 4 else 1
                Y = pY.next()
                for c in range(8):
                    kb.mm(Y[0:n, :], hidT[:, c, s * 128:s * 128 + n], wd[:, c, :], [hidT, wd], [Y], start=(c == 0), stop=(c == 7))
                ye = yes[s]
                kb.fw.op(kb.fw.dve, [Y, gateT, kb.gtb[r][1]], [ye], lambda: nc.vector.scalar_tensor_tensor(
                    out=ye[0:n, cb * 512:(cb + 1) * 512], in0=Y[0:n, :], scalar=gateT[0:n, s, e:e + 1], in1=kb.gtb[r][1][0:n, cb * 512:(cb + 1) * 512],
                    op0=ALU.mult, op1=ALU.mult))
        for s in range(5):
            n = 128 if s < 4 else 32
            ye = yes[s]
            kb.fw.dma(kb.fw.pool, [ye, idxT], [kb.xs_c[0]], lambda q: q.indirect_dma_start(
                out=kb.xout, out_offset=bass.IndirectOffsetOnAxis(ap=idxT[0:n, s, e:e + 1], axis=0),
                in_=ye[0:n, :], in_offset=None, compute_op=ALU.add))
    kb.end_phase()


def declare_io_C(kb):
    nc = kb.nc
    L = kb.L
    for nm, shape in (("convw_fm", [L, 128, 12, 5]), ("alog_bc", [L, 128, 8]), ("dtb_bc", [L, 128, 8]),
                      ("ogain", [L, 128]), ("tri", [2, 128, 128]), ("negstrict", [2, 128, 128])):
        kb.I[nm] = nc.dram_tensor(nm, shape, F32, kind="ExternalInput").ap()
    if "OTC" in kb.debug:
        kb.dbg["OTC"] = nc.dram_tensor("dbg_OTC", [4, 128, T], BF16, kind="ExternalOutput").ap()
    if "odn" in kb.debug:
        kb.dbg["odn"] = nc.dram_tensor("dbg_odn", [4, 128, NT * 128], F32, kind="ExternalOutput").ap()


def phaseC(kb, l):
    I = kb.I
    nc = kb.nc
    kb.begin_phase()
    SEGS = ((0, NCTX), (NCTX, T))
    tri = kb.sb([128, 2, 128], F32)
    nst = kb.sb([128, 2, 128], F32)
    for d in range(2):
        kb.dma("sp", tri[:, d, :], I["tri"][d], [], [tri])
        kb.dma("sp", nst[:, d, :], I["negstrict"][d], [], [nst])
    ones = kb.sb([128, 128], F32)
    kb.memset("dve", ones[:], 1.0, [ones])
    cw = kb.sb([128, 12, 5], F32)
    kb.dma("sp", cw[:], I["convw_fm"][l], [], [cw])
    ab = kb.sb([128, 2, 8], F32)
    kb.dma("sp", ab[:, 0, :], I["alog_bc"][l], [], [ab])
    kb.dma("sp", ab[:, 1, :], I["dtb_bc"][l], [], [ab])
    og = kb.sb([128, 128], F32)
    kb.dma("sp", og[:], I["ogain"][l].partition_broadcast(128), [], [og])
    beta = kb.sb([128, NT, 8], F32)
    gg = kb.sb([128, NT, 8], F32)
    tmp8 = kb.sb([128, NT, 8], F32)
    kb.act(beta[:], kb.BD[:, :, 0:8], AF.Sigmoid, [kb.BD], [beta])
    kb.tt("dve", tmp8[:], kb.BD[:, :, 8:16], ab[:, 1, :].unsqueeze(1).to_broadcast([128, NT, 8]), ALU.add, [kb.BD, ab], [tmp8])
    kb.act(tmp8[:], tmp8[:], AF.Exp, [tmp8], [tmp8])
    kb.act(tmp8[:], tmp8[:], AF.Ln, [tmp8], [tmp8], bias=1.0)
    kb.act(ab[:, 0, :], ab[:, 0, :], AF.Exp, [ab], [ab])
    kb.tt("dve", gg[:], tmp8[:], ab[:, 0, :].unsqueeze(1).to_broadcast([128, NT, 8]), ALU.mult, [tmp8, ab], [gg])
    kb.ts("dve", gg[:], gg[:], -1.0, None, ALU.mult, None, [gg], [gg])

    qT = kb.sb([128, T], F32)
    kT = kb.sb([128, T], F32)
    ktm = kb.sb([128, NT, 128], F32)
    vtm = kb.sb([128, NT, 128], F32)
    otot = kb.sb([128, NT, 128], F32)
    sgt = kb.sb([128, NT, 128], BF16)
    xc_ring = Ring([kb.sb([128, T], F32) for _ in range(1)])
    yc = kb.sb([128, T], F32)
    sc_ring = Ring([kb.sb([128, 512], F32) for _ in range(2)])
    sm = {nm: kb.sb([128, 2, NT], F32) for nm in ("Gcol", "glast", "expG", "bg", "etail", "eglast")}
    S = [kb.sb([128, 128], F32) for _ in range(2)]
    pPre = Ring([kb.ps([128, 4, 128], F32) for _ in range(2)])
    pSol = [kb.ps([128, 4, 128], F32) for _ in range(4)]
    pRec = Ring([kb.ps([128, 4, 128], F32) for _ in range(2)])
    W = {}

    def wt(name, n, shape=(128, 128), dt=F32):
        W[name] = Ring([kb.sb(list(shape), dt) for _ in range(n)])

    for nm in ("rhsA", "rhsB", "E", "DT", "DTb", "M"):
        wt(nm, 4)
    for nm in ("vnew", "tmpo", "on", "onb"):
        wt(nm, 3)
    for nm in ("Q", "Qt", "Tt"):
        wt(nm, 10)
    for nm in ("QKD", "wT", "u", "Rv", "Rk", "kt"):
        wt(nm, 8 if nm == "QKD" else 6)
    wt("ssq", 4, (128, 1))
    ostage = Ring([kb.sb([128, 512], BF16) for _ in range(2)])
    pTrb = None

    for h in range(4):
        for which, c in (("q", h), ("k", 4 + h), ("v", 8 + h)):
            xc = xc_ring.next()
            kb.dma("sp", xc[:], kb.CQ[c], [kb.CQ], [xc])
            kb.ts("dve", yc[:], xc[:], cw[:, c, 2:3], None, ALU.mult, None, [xc, cw], [yc])
            for j in (0, 1, 3, 4):
                s = j - 2
                for (lo, hi) in SEGS:
                    a, b = max(lo, lo - s), min(hi, hi - s)
                    eng = "dve"
                    kb.fw.op(kb._e(eng), [xc, cw, yc], [yc], lambda: kb._ne(eng).scalar_tensor_tensor(
                        out=yc[:, a:b], in0=xc[:, a + s:b + s], scalar=cw[:, c, j:j + 1], in1=yc[:, a:b], op0=ALU.mult, op1=ALU.add))
            dst = {"q": qT, "k": kT, "v": xc}[which]
            if which == "v":
                kb.act(xc[:], yc[:], AF.Silu, [yc], [xc])
                for t in range(NT):
                    p = pPre.next()
                    kb.tr(p[:, 0, :], xc[:, t * 128:(t + 1) * 128], kb.identf[:], [xc, kb.identf], [p])
                    kb.cp("act" if t % 2 else "dve", vtm[:, t, :], p[:, 0, :], [p], [vtm])
                continue
            kb.act(yc[:], yc[:], AF.Silu, [yc], [yc])
            for blk in range((T + 511) // 512):
                a, b = blk * 512, min(T, blk * 512 + 512)
                n = b - a
                sq = sc_ring.next()
                kb.tt("pool", sq[:, 0:n], yc[:, a:b], yc[:, a:b], ALU.mult, [yc], [sq])
                p = pPre.next()
                pv = p[:].rearrange("p a b -> p (a b)")
                kb.mm(pv[:, 0:n], ones[:], sq[:, 0:n], [ones, sq], [p])
                kb.act(sq[:, 0:n], pv[:, 0:n], AF.Ln, [p, kb.epsc], [sq], bias=kb.epsc[:, 0:1])
                kb.act(sq[:, 0:n], sq[:, 0:n], AF.Exp, [sq], [sq], scale=-0.5)
                if which == "q":
                    kb.fw.op(kb.fw.dve, [yc, sq], [dst], lambda: nc.vector.scalar_tensor_tensor(
                        out=dst[:, a:b], in0=yc[:, a:b], scalar=128.0 ** -0.5, in1=sq[:, 0:n], op0=ALU.mult, op1=ALU.mult))
                else:
                    kb.tt("dve", dst[:, a:b], yc[:, a:b], sq[:, 0:n], ALU.mult, [yc, sq], [dst])
            if which == "k":
                for t in range(NT):
                    p = pPre.next()
                    kb.tr(p[:, 0, :], kT[:, t * 128:(t + 1) * 128], kb.identf[:], [kT, kb.identf], [p])
                    kb.cp("act" if t % 2 else "dve", ktm[:, t, :], p[:, 0, :], [p], [ktm])
        kb.dma("sp", sgt[:], kb.SG[:, h * 128:(h + 1) * 128].rearrange("(t p) e -> p t e", p=128), [kb.SG], [sgt])
        import os
        CUTC = os.environ.get("CUTC", "")
        if CUTC == "1":
            break
        for d in range(2):
            dh = d * 4 + h
            p = pPre.next()
            kb.mm(p[:, 0, 0:NT], tri[:, d, :], gg[:, :, dh], [tri, gg], [p])
            kb.mm(p[:, 1, 0:NT], ones[:], gg[:, :, dh], [ones, gg], [p])
            kb.cp("dve", sm["Gcol"][:, d, :], p[:, 0, 0:NT], [p], [sm["Gcol"]])
            kb.cp("dve", sm["glast"][:, d, :], p[:, 1, 0:NT], [p], [sm["glast"]])
            kb.act(sm["expG"][:, d, :], p[:, 0, 0:NT], AF.Exp, [p], [sm["expG"]])
            kb.act(sm["eglast"][:, d, :], p[:, 1, 0:NT], AF.Exp, [p], [sm["eglast"]])
            kb.tt("dve", sm["bg"][:, d, :], sm["expG"][:, d, :], beta[:, :, dh], ALU.mult, [sm["expG"], beta], [sm["bg"]])
            kb.tt("dve", sm["etail"][:, d, :], sm["glast"][:, d, :], sm["Gcol"][:, d, :], ALU.subtract, [sm["glast"], sm["Gcol"]], [sm["etail"]])
            kb.act(sm["etail"][:, d, :], sm["etail"][:, d, :], AF.Exp, [sm["etail"]], [sm["etail"]])
            kb.memset("dve", S[d][:], 0.0, [S[d]])
        kb.memset("pool", otot[:], 0.0, [otot])
        order = [list(range(NT)), [1, 0] + list(range(NT - 1, 1, -1))]
        if CUTC == "2":
            break
        def run_rr(gens):
            active = list(gens)
            while active:
                for g in list(active):
                    try:
                        next(g)
                    except StopIteration:
                        active.remove(g)

        for step in range(0, NT, 2):
            jobs = [(d, order[d][sp_]) for sp_ in (step, step + 1) for d in range(2)]
            st = {}

            def pre_job(ji, d, c):
                dh = d * 4 + h
                cs = slice(c * 128, (c + 1) * 128)
                rhsA, rhsB = W["rhsA"].next(), W["rhsB"].next()
                kb.ts("pool", rhsA[:], tri[:, d, :], gg[:, c, dh:dh + 1], None, ALU.mult, None, [tri, gg], [rhsA])
                kb.ts("pool", rhsB[:], kb.identf[:], beta[:, c, dh:dh + 1], None, ALU.mult, None, [kb.identf, beta], [rhsB])
                Rv, Rk, kt_ = W["Rv"].next(), W["Rk"].next(), W["kt"].next()
                kb.ts("pool", Rv[:], vtm[:, c, :], beta[:, c, dh:dh + 1], None, ALU.mult, None, [vtm, beta], [Rv])
                kb.ts("pool", Rk[:], ktm[:, c, :], sm["bg"][:, d, c:c + 1], None, ALU.mult, None, [ktm, sm["bg"]], [Rk])
                kb.ts("pool", kt_[:], ktm[:, c, :], sm["etail"][:, d, c:c + 1], None, ALU.mult, None, [ktm, sm["etail"]], [kt_])
                yield
                p = pSol[ji]
                kb.mm(p[:, 0, :], ones[:], rhsA[:], [ones, rhsA], [p])
                kb.mm(p[:, 1, :], ones[:], rhsB[:], [ones, rhsB], [p])
                kb.mm(p[:, 2, :], kT[:, cs], kT[:, cs], [kT], [p])
                kb.mm(p[:, 3, :], kT[:, cs], qT[:, cs], [kT, qT], [p])
                yield
                E = W["E"].next()
                kb.ts("dve", E[:], p[:, 0, :], sm["Gcol"][:, d, c:c + 1], 0.0, ALU.subtract, ALU.min, [p, sm["Gcol"]], [E])
                yield
                kb.act(E[:], E[:], AF.Exp, [E], [E])
                yield
                DT = W["DT"].next()
                kb.tt("pool", DT[:], E[:], tri[:, d, :], ALU.mult, [E, tri], [DT])
                yield
                DTb = W["DTb"].next()
                kb.tt("dve", DTb[:], p[:, 1, :], DT[:], ALU.mult, [p, DT], [DTb])
                M = W["M"].next()
                kb.tt("dve", M[:], p[:, 2, :], DTb[:], ALU.mult, [p, DTb], [M])
                QKD = W["QKD"].next()
                kb.tt("dve", QKD[:], p[:, 3, :], DT[:], ALU.mult, [p, DT], [QKD])
                yield
                Qt = W["Qt"].next()
                kb.tt("pool", Qt[:], M[:], nst[:, d, :], ALU.mult, [M, nst], [Qt])
                Tt = W["Tt"].next()
                kb.tt("pool", Tt[:], Qt[:], kb.identf[:], ALU.add, [Qt, kb.identf], [Tt])
                yield
                kb.tr(p[:, 0, :], Qt[:], kb.identf[:], [Qt, kb.identf], [p])
                yield
                Q = W["Q"].next()
                kb.cp("act", Q[:], p[:, 0, :], [p], [Q])
                st[ji] = dict(Q=Q, Qt=Qt, Tt=Tt, QKD=QKD, ps=p, Rv=Rv, Rk=Rk, kt=kt_)

            run_rr([pre_job(ji, d, c) for ji, (d, c) in enumerate(jobs)])
            for k in range(1, 7):
                for ji in range(4):
                    s_ = st[ji]
                    ps = s_["ps"]
                    kb.mm(ps[:, 0, :], s_["Qt"][:], s_["Q"][:], [s_["Qt"], s_["Q"]], [ps])
                    if k < 6:
                        kb.mm(ps[:, 1, :], s_["Q"][:], s_["Qt"][:], [s_["Qt"], s_["Q"]], [ps])
                    Qn = W["Q"].next()
                    kb.cp("act", Qn[:], ps[:, 0, :], [ps], [Qn])
                    if k < 6:
                        Qtn = W["Qt"].next()
                        kb.cp("dve", Qtn[:], ps[:, 1, :], [ps], [Qtn])
                        s_["Qt"] = Qtn
                    s_["Q"] = Qn
                    kb.mm(ps[:, 2, :], Qn[:], s_["Tt"][:], [Qn, s_["Tt"]], [ps])
                    Tn = W["Tt"].next()
                    kb.tt("dve", Tn[:], ps[:, 2, :], s_["Tt"][:], ALU.add, [ps, s_["Tt"]], [Tn])
                    s_["Tt"] = Tn

            def uw_job(ji, d, c):
                s_ = st[ji]
                ps = s_["ps"]
                kb.mm(ps[:, 0, :], s_["Tt"][:], s_["Rv"][:], [s_["Tt"], s_["Rv"]], [ps])
                kb.mm(ps[:, 1, :], s_["Rk"][:], s_["Tt"][:], [s_["Tt"], s_["Rk"]], [ps])
                yield
                u, wT = W["u"].next(), W["wT"].next()
                kb.cp("act", u[:], ps[:, 0, :], [ps], [u])
                kb.cp("dve", wT[:], ps[:, 1, :], [ps], [wT])
                s_["u"], s_["wT"] = u, wT

            run_rr([uw_job(ji, d, c) for ji, (d, c) in enumerate(jobs)])

            def rec_job(ji, d, c):
                cs = slice(c * 128, (c + 1) * 128)
                s_ = st[ji]
                u, wT, kt_ = s_["u"], s_["wT"], s_["kt"]
                pr = pRec.next()
                kb.mm(pr[:, 0, :], wT[:], S[d][:], [wT, S[d]], [pr])
                kb.mm(pr[:, 1, :], qT[:, cs], S[d][:], [qT, S[d]], [pr])
                yield
                vnew = W["vnew"].next()
                kb.tt("dve", vnew[:], u[:], pr[:, 0, :], ALU.subtract, [u, pr], [vnew])
                yield
                kb.mm(pr[:, 2, :], s_["QKD"][:], vnew[:], [s_["QKD"], vnew], [pr])
                kb.mm(pr[:, 3, :], kt_[:], vnew[:], [kt_, vnew], [pr])
                yield
                kb.fw.op(kb.fw.dve, [S[d], sm["eglast"], pr], [S[d]], lambda: nc.vector.scalar_tensor_tensor(
                    out=S[d][:], in0=S[d][:], scalar=sm["eglast"][:, d, c:c + 1], in1=pr[:, 3, :], op0=ALU.mult, op1=ALU.add))
                yield
                tmpo = W["tmpo"].next()
                kb.act(tmpo[:], pr[:, 1, :], AF.Copy, [pr, sm["expG"]], [tmpo], scale=sm["expG"][:, d, c:c + 1])
                yield
                kb.tt("dve", tmpo[:], tmpo[:], pr[:, 2, :], ALU.add, [tmpo, pr], [tmpo])
                yield
                kb.tt("pool", otot[:, c, :], otot[:, c, :], tmpo[:], ALU.add, [tmpo, otot], [otot])

            run_rr([rec_job(ji, d, c) for ji, (d, c) in enumerate(jobs) if ji < 2])
            run_rr([rec_job(ji, d, c) for ji, (d, c) in enumerate(jobs) if ji >= 2])
        if CUTC:
            break
        if "odn" in kb.dbg:
            kb.dma("sp", kb.dbg["odn"][h], otot[:].rearrange("p t e -> p (t e)"), [otot], [])
        for g4 in range((NT + 3) // 4):
            tiles = list(range(4 * g4, min(4 * g4 + 4, NT)))
            ost = ostage.next()
            p = pPre.next()
            pb = p[:].rearrange("p a b -> p (a b)").bitcast(BF16)
            for ti, t in enumerate(tiles):
                ssq = W["ssq"].next()
                on = W["on"].next()
                kb.act(on[:], otot[:, t, :], AF.Square, [otot], [on, ssq], accum_out=ssq[:, 0:1])
                kb.rsqrt_inplace(ssq[:, 0:1], ssq, 1.0 / 128)
                kb.fw.op(kb.fw.dve, [otot, ssq, og], [on], lambda: nc.vector.scalar_tensor_tensor(
                    out=on[:], in0=otot[:, t, :], scalar=ssq[:, 0:1], in1=og[:], op0=ALU.mult, op1=ALU.mult))
                onb = W["onb"].next()
                kb.tt("pool", onb[:].bitcast(BF16)[:, 0:128], on[:], sgt[:, t, :], ALU.mult, [on, sgt], [onb])
                kb.tr(pb[:, ti * 128:(ti + 1) * 128], onb[:].bitcast(BF16)[:, 0:128], kb.identb[:], [onb, kb.identb], [p])
            n = len(tiles) * 128
            kb.cp("dve", ost[:, 0:n], pb[:, 0:n], [p], [ost])
            kb.dma("sp", kb.OT[12 + h, :, tiles[0] * 128:tiles[0] * 128 + n], ost[:, 0:n], [ost], [kb.OT])
    if "OTC" in kb.dbg:
        kb.dma("sp", kb.dbg["OTC"], kb.OT[12:16], [kb.OT], [])
    kb.end_phase()


_PROG = {}


def _get_prog(n_layers):
    if n_layers not in _PROG:
        _PROG[n_layers] = build_program(n_layers, debug=(), phases="0ABCDE")
    return _PROG[n_layers]


def kernel(**inputs):
    inp = {k: np.asarray(v) for k, v in inputs.items()}
    B = inp["x"].shape[0]
    depth = inp["w_in"].shape[0]
    nc = _get_prog(depth)
    base = core_inputs(inp, 0, range(depth))
    in_maps = []
    for b in range(B):
        d = dict(base)
        d["xin"] = np.ascontiguousarray(np.concatenate([inp["ctx"][b], inp["x"][b]], axis=0))
        cv = np.stack([inp["c"][b], inp["c_ctx"]], axis=0)
        d["cT"] = np.ascontiguousarray(cv.reshape(2, 16, 128).transpose(2, 1, 0))
        in_maps.append(d)
    res = run_bass_kernel_spmd(nc, in_maps, core_ids=list(range(B)))
    return np.stack([np.asarray(res.results[b]["xout"])[NCTX:] for b in range(B)], axis=0).astype(np.float32)
```
